# Optimizing a Trainium2 kernel written in Bass

```python
import jax, jax.numpy as jnp
from jax import lax
import numpy as np

D_MODEL = 1024
BATCH = 8
SEQ = 4096
DEPTH = 4

N_MIXERS = 2
SB_HEADS = 16
SB_HEAD_DIM = D_MODEL // SB_HEADS
Q_BLOCK = 128
RET_HEADS = 8
RET_QK_DIM = D_MODEL // RET_HEADS
RET_V_DIM = 2 * RET_QK_DIM
RET_CHUNK = 128
ROPE_BASE = 10000.0
N_EXPERTS = 16
N_GROUPS = 4
EXPERTS_PER_GROUP = N_EXPERTS // N_GROUPS
TOP_K = 2
D_EXPERT = D_MODEL // 2
N_SB = (DEPTH + 1) // 2
N_RET = DEPTH // 2
DEEPNORM_ALPHA = (2 * DEPTH) ** 0.25
DEEPNORM_BETA = (8 * DEPTH) ** -0.25
LN_EPS = 1e-5

kernel_name = "hybrid_stickbreak_retention_groupmoe"


def layer_norm(x, g, b):
    xf = x.astype(jnp.float32)
    mu = jnp.mean(xf, axis=-1, keepdims=True)
    var = jnp.mean(jnp.square(xf - mu), axis=-1, keepdims=True)
    y = (xf - mu) * lax.rsqrt(var + LN_EPS) * g.astype(jnp.float32) + b.astype(jnp.float32)
    return y.astype(x.dtype)


def adaln_params(c, w, b):
    m = jax.nn.silu(c) @ w + b
    shift, scale, gate = jnp.split(m, 3, axis=-1)
    return shift[:, None, :], scale[:, None, :], gate[:, None, :]


def stick_breaking_attention(h, w_in, w_out):
    B, S, _ = h.shape
    qkv = (h @ w_in).reshape(B, S, 3, SB_HEADS, SB_HEAD_DIM)
    q = qkv[:, :, 0].transpose(0, 2, 1, 3)
    k = qkv[:, :, 1].transpose(0, 2, 1, 3)
    v = qkv[:, :, 2].transpose(0, 2, 1, 3)
    n_blocks = S // Q_BLOCK
    q_blocks = q.reshape(B, SB_HEADS, n_blocks, Q_BLOCK, SB_HEAD_DIM).transpose(2, 0, 1, 3, 4)
    key_pos = jnp.arange(S)
    scale = SB_HEAD_DIM ** -0.5

    def block(args):
        qb, i = args
        z = jnp.einsum('bhqd,bhkd->bhqk', qb, k).astype(jnp.float32) * scale
        q_pos = i * Q_BLOCK + jnp.arange(Q_BLOCK)
        causal = key_pos[None, :] < q_pos[:, None]
        log_keep = jnp.where(causal, jax.nn.log_sigmoid(-z), 0.0)
        suffix = lax.cumsum(log_keep, axis=3, reverse=True) - log_keep
        a = jnp.where(causal, jnp.exp(jax.nn.log_sigmoid(z) + suffix), 0.0)
        return jnp.einsum('bhqk,bhkd->bhqd', a.astype(v.dtype), v)

    o = lax.map(block, (q_blocks, jnp.arange(n_blocks)))
    o = o.transpose(1, 0, 3, 2, 4).reshape(B, S, D_MODEL)
    return o @ w_out


def rotary(x, cos, sin):
    half = x.shape[-1] // 2
    x1, x2 = x[..., :half], x[..., half:]
    return jnp.concatenate([x1 * cos - x2 * sin, x2 * cos + x1 * sin], axis=-1)


def retention(h, positions, w_in, gn_g, w_out):
    B, S, _ = h.shape
    f32 = jnp.float32
    proj = h @ w_in
    q, k, v, g = jnp.split(proj, [D_MODEL, 2 * D_MODEL, 4 * D_MODEL], axis=-1)
    q = q.reshape(B, S, RET_HEADS, RET_QK_DIM).astype(f32)
    k = k.reshape(B, S, RET_HEADS, RET_QK_DIM).astype(f32) * (RET_QK_DIM ** -0.5)
    v = v.reshape(B, S, RET_HEADS, RET_V_DIM).astype(f32)
    inv_freq = 1.0 / (ROPE_BASE ** (jnp.arange(0, RET_QK_DIM, 2, dtype=f32) / RET_QK_DIM))
    ang = positions.astype(f32)[..., None] * inv_freq
    cos = jnp.cos(ang)[:, :, None, :]
    sin = jnp.sin(ang)[:, :, None, :]
    q = rotary(q, cos, sin)
    k = rotary(k, cos, sin)

    log_gamma = jnp.log(1.0 - jnp.exp2(-5.0 - jnp.arange(RET_HEADS, dtype=f32)))
    n = jnp.arange(RET_CHUNK, dtype=f32)
    diff = n[:, None] - n[None, :]
    dmask = jnp.where(diff >= 0, jnp.exp(jnp.maximum(diff, 0.0) * log_gamma[:, None, None]), 0.0)
    xi = jnp.exp((n + 1.0) * log_gamma[:, None])
    zeta = jnp.exp((RET_CHUNK - 1.0 - n) * log_gamma[:, None])
    chunk_decay = jnp.exp(RET_CHUNK * log_gamma)
    n_chunks = S // RET_CHUNK

    def chunks(t):
        return t.reshape(B, n_chunks, RET_CHUNK, RET_HEADS, t.shape[-1]).transpose(1, 0, 3, 2, 4)

    def step(state, xs):
        qc, kc, vc = xs
        qk = jnp.einsum('bhnd,bhmd->bhnm', qc, kc) * dmask
        inner = jnp.einsum('bhnm,bhme->bhne', qk, vc)
        cross = jnp.einsum('bhnd,bhde->bhne', qc, state) * xi[None, :, :, None]
        state = state * chunk_decay[None, :, None, None] + jnp.einsum(
            'bhmd,bhme->bhde', kc * zeta[None, :, :, None], vc)
        return state, inner + cross

    state0 = jnp.zeros((B, RET_HEADS, RET_QK_DIM, RET_V_DIM), f32)
    _, o = lax.scan(step, state0, (chunks(q), chunks(k), chunks(v)))
    o = o.transpose(1, 0, 3, 2, 4).reshape(B, S, RET_HEADS, RET_V_DIM)
    mu = jnp.mean(o, axis=-1, keepdims=True)
    var = jnp.mean(jnp.square(o - mu), axis=-1, keepdims=True)
    o = ((o - mu) * lax.rsqrt(var + LN_EPS)).reshape(B, S, 2 * D_MODEL) * gn_g.astype(f32)
    return (jax.nn.silu(g) * o.astype(h.dtype)) @ w_out


def grouped_moe(h, router_w, router_b, w_gate, w_up, w_down):
    B, S, Dm = h.shape
    t = h.reshape(B * S, Dm)
    logits = t.astype(jnp.float32) @ router_w.astype(jnp.float32) + router_b.astype(jnp.float32)
    probs = jax.nn.softmax(logits, axis=-1)
    grouped = probs.reshape(-1, N_GROUPS, EXPERTS_PER_GROUP)
    group_score = jnp.sum(lax.top_k(grouped, TOP_K)[0], axis=-1)
    sel_group = jnp.argmax(group_score, axis=-1)
    expert_group = jnp.arange(N_EXPERTS) // EXPERTS_PER_GROUP
    in_group = expert_group[None, :] == sel_group[:, None]
    masked = jnp.where(in_group, logits, -jnp.inf)
    top_val, top_idx = lax.top_k(masked, TOP_K)
    top_w = jax.nn.softmax(top_val, axis=-1)
    combine = jnp.sum(jax.nn.one_hot(top_idx, N_EXPERTS, dtype=jnp.float32) * top_w[..., None], axis=1)
    out = jnp.zeros(t.shape, jnp.float32)
    for e in range(N_EXPERTS):
        hidden = jax.nn.silu(t @ w_gate[e]) * (t @ w_up[e])
        out = out + combine[:, e:e + 1] * (hidden @ w_down[e]).astype(jnp.float32)
    return out.astype(h.dtype).reshape(B, S, Dm)


def setup_inputs(seed: int = 0) -> dict:
    key = jax.random.key(seed)
    ks = jax.random.split(key, 20)
    D, F = D_MODEL, D_EXPERT
    nrm = jax.random.normal
    x = nrm(ks[0], (BATCH, SEQ, D), jnp.float32)
    c = nrm(ks[1], (BATCH, D), jnp.float32)
    positions = jnp.broadcast_to(jnp.arange(SEQ, dtype=jnp.int32)[None, :], (BATCH, SEQ))
    ada_w = nrm(ks[2], (DEPTH, 2, D, 3 * D), jnp.float32) * (0.5 * D ** -0.5)
    ada_b = nrm(ks[3], (DEPTH, 2, 3 * D), jnp.float32) * 0.02
    ln_g = 1.0 + 0.02 * nrm(ks[4], (DEPTH, 2, D), jnp.float32)
    ln_b = 0.02 * nrm(ks[5], (DEPTH, 2, D), jnp.float32)
    sb_col = jnp.concatenate([jnp.ones((2 * D,), jnp.float32), jnp.full((D,), DEEPNORM_BETA, jnp.float32)])
    sb_w_in = nrm(ks[6], (N_SB, D, 3 * D), jnp.float32) * (D ** -0.5) * sb_col
    sb_w_out = nrm(ks[7], (N_SB, D, D), jnp.float32) * (D ** -0.5) * DEEPNORM_BETA
    ret_col = jnp.concatenate([jnp.ones((2 * D,), jnp.float32), jnp.full((2 * D,), DEEPNORM_BETA, jnp.float32),
                               jnp.ones((2 * D,), jnp.float32)])
    ret_w_in = nrm(ks[8], (N_RET, D, 6 * D), jnp.float32) * (D ** -0.5) * ret_col
    ret_gn_g = 1.0 + 0.02 * nrm(ks[9], (N_RET, 2 * D), jnp.float32)
    ret_w_out = nrm(ks[10], (N_RET, 2 * D, D), jnp.float32) * ((2 * D) ** -0.5) * DEEPNORM_BETA
    router_w = nrm(ks[11], (D, N_EXPERTS), jnp.float32) * (D ** -0.5)
    router_b = nrm(ks[12], (N_EXPERTS,), jnp.float32) * 0.01
    moe_w_gate = nrm(ks[13], (DEPTH, N_EXPERTS, D, F), jnp.float32) * (D ** -0.5)
    moe_w_up = nrm(ks[14], (DEPTH, N_EXPERTS, D, F), jnp.float32) * (D ** -0.5) * DEEPNORM_BETA
    moe_w_down = nrm(ks[15], (DEPTH, N_EXPERTS, F, D), jnp.float32) * (F ** -0.5) * DEEPNORM_BETA
    return {"x": x, "c": c, "positions": positions, "ada_w": ada_w, "ada_b": ada_b,
            "ln_g": ln_g, "ln_b": ln_b, "sb_w_in": sb_w_in, "sb_w_out": sb_w_out,
            "ret_w_in": ret_w_in, "ret_gn_g": ret_gn_g, "ret_w_out": ret_w_out,
            "router_w": router_w, "router_b": router_b, "moe_w_gate": moe_w_gate,
            "moe_w_up": moe_w_up, "moe_w_down": moe_w_down}


def reference(x, c, positions, ada_w, ada_b, ln_g, ln_b, sb_w_in, sb_w_out, ret_w_in, ret_gn_g,
              ret_w_out, router_w, router_b, moe_w_gate, moe_w_up, moe_w_down):
    for i in range(DEPTH):
        shift, scale, gate = adaln_params(c, ada_w[i, 0], ada_b[i, 0])
        h = x * (1.0 + scale) + shift
        if i % N_MIXERS == 0:
            y = stick_breaking_attention(h, sb_w_in[i // 2], sb_w_out[i // 2])
        else:
            y = retention(h, positions, ret_w_in[i // 2], ret_gn_g[i // 2], ret_w_out[i // 2])
        x = layer_norm(DEEPNORM_ALPHA * x + (1.0 + gate) * y, ln_g[i, 0], ln_b[i, 0])
        shift, scale, gate = adaln_params(c, ada_w[i, 1], ada_b[i, 1])
        h = x * (1.0 + scale) + shift
        y = grouped_moe(h, router_w, router_b, moe_w_gate[i], moe_w_up[i], moe_w_down[i])
        x = layer_norm(DEEPNORM_ALPHA * x + (1.0 + gate) * y, ln_g[i, 1], ln_b[i, 1])
    return x
```

```python
import numpy as np
import ml_dtypes
from contextlib import ExitStack
import concourse.bass as bass
import concourse.mybir as mybir
from concourse.bass_utils import run_bass_kernel_spmd

F32 = mybir.dt.float32
BF16 = mybir.dt.bfloat16
I32 = mybir.dt.int32
AF = mybir.ActivationFunctionType
ALU = mybir.AluOpType
AX = mybir.AxisListType

D = 1024
DEPTH = 4
ALPHA = (2 * DEPTH) ** 0.25
LN_EPS = 1e-5


class Aff:
    __slots__ = ("c", "k")

    def __init__(self, c=0, k=()):
        self.c = c
        self.k = tuple(k)

    def add(self, n):
        return Aff(self.c + n, self.k)

    def le(self, o):
        return self.k == o.k and self.c <= o.c


class Tile:
    def __init__(self, name, t):
        self.name = name
        self.t = t
        self.wr = None
        self.rd = {}

    def __getitem__(self, idx):
        return self.t[idx]


class K:
    ENG = ("pe", "act", "dve", "pool", "sp")

    def __init__(self, nc):
        self.nc = nc
        self.E = {"pe": nc.tensor, "act": nc.scalar, "dve": nc.vector,
                  "pool": nc.gpsimd, "sp": nc.sync}
        self.sems = {}
        self.cur = {}
        self.known = {e: {} for e in self.ENG}
        self.dirty = set()
        self.tiles = []
        self.dry = 0
        self.loops = []
        self.nloop = 0
        self.tregs = {}
        for e in ("pe", "act", "dve", "pool"):
            self._sem("e_" + e)

    def _sem(self, key):
        if key not in self.sems:
            self.sems[key] = self.nc.alloc_semaphore(key)
            self.cur[key] = Aff(0)
        return self.sems[key]

    def _treg(self, eng):
        if eng not in self.tregs:
            self.tregs[eng] = self.E[eng].alloc_register("kw_" + eng)
        return self.tregs[eng]

    def _val(self, a):
        v = a.c
        for lid, coef in a.k:
            var = [x for (l, x) in self.loops if l == lid][0]
            v = var * coef + v
        return v

    def _wait(self, eng, evs):
        for key, a in evs:
            if eng == "pe" and key == "e_pe":
                continue
            kn = self.known[eng].get(key)
            if kn is not None and a.le(kn):
                continue
            self.known[eng][key] = a
            if not self.dry:
                if a.k:
                    assert len(a.k) == 1
                    lid, coef = a.k[0]
                    var = [x for (l, x) in self.loops if l == lid][0]
                    T = self._treg(eng)
                    self.E[eng].reg_mul(T, var, coef)
                    self.E[eng].reg_add(T, T, a.c)
                    self.E[eng].wait_ge(self.sems[key], T)
                else:
                    self.E[eng].wait_ge(self.sems[key], a.c)

    def _deps(self, r, w):
        evs = []
        for t in r:
            if t.wr is not None:
                evs.append(t.wr)
        for t in w:
            if t.wr is not None:
                evs.append(t.wr)
            evs.extend(t.rd.items())
        return evs

    def _mark(self, ev, r, w):
        key, a = ev
        for t in r:
            t.rd[key] = a
        for t in w:
            t.wr = ev
            t.rd = {}

    def tile(self, es, name, shape, dt):
        self.uid = getattr(self, "uid", 0) + 1
        t = Tile(name, es.enter_context(self.nc.sbuf_tensor(f"{name}_{self.uid}", list(shape), dt)))
        self.tiles.append(t)
        return t

    def ptile(self, es, name, shape, dt=F32):
        self.uid = getattr(self, "uid", 0) + 1
        t = Tile(name, es.enter_context(self.nc.psum_tensor(f"{name}_{self.uid}", list(shape), dt)))
        self.tiles.append(t)
        return t

    def vtile(self, name):
        t = Tile(name, None)
        self.tiles.append(t)
        return t

    def op(self, eng, fn, r=(), w=(), inc=True):
        self._wait(eng, self._deps(r, w))
        if not inc:
            if not self.dry:
                fn()
            return
        key = "e_" + eng
        self.cur[key] = self.cur[key].add(1)
        self.dirty.add(key)
        if not self.dry:
            fn().then_inc(self.sems[key], 1)
        self._mark((key, self.cur[key]), r, w)

    def dma(self, q, out, in_, r=(), w=(), key=None):
        if key is None:
            key = "d_" + (w[0].name if w else r[0].name)
        self._sem(key)
        self._wait(q, self._deps(r, w))
        self.cur[key] = self.cur[key].add(16)
        self.dirty.add(key)
        if not self.dry:
            o = out() if callable(out) else out
            i = in_() if callable(in_) else in_
            self.E[q].dma_start(out=o, in_=i).then_inc(self.sems[key], 16)
        self._mark((key, self.cur[key]), r, w)

    def gather(self, out, src, idx, r=(), w=(), key=None):
        if key is None:
            key = "d_" + w[0].name
        self._sem(key)
        self._wait("pool", self._deps(r, w))
        self.cur[key] = self.cur[key].add(16)
        self.dirty.add(key)
        if not self.dry:
            self.nc.gpsimd.indirect_dma_start(
                out=out, out_offset=None, in_=src,
                in_offset=bass.IndirectOffsetOnAxis(ap=idx, axis=0)).then_inc(self.sems[key], 16)
        self._mark((key, self.cur[key]), r, w)

    def scatter(self, dst, in_, idx, r=(), w=(), key=None):
        if key is None:
            key = "d_" + r[0].name
        self._sem(key)
        self._wait("pool", self._deps(r, w))
        self.cur[key] = self.cur[key].add(16)
        self.dirty.add(key)
        if not self.dry:
            self.nc.gpsimd.indirect_dma_start(
                out=dst, out_offset=bass.IndirectOffsetOnAxis(ap=idx, axis=0), in_=in_,
                in_offset=None).then_inc(self.sems[key], 16)
        self._mark((key, self.cur[key]), r, w)

    def record(self, fn):
        rec = []
        names = ("op", "dma", "gather", "scatter")
        for nm in names:
            setattr(self, nm, (lambda nm: (lambda *a, **kw: rec.append((nm, a, kw))))(nm))
        try:
            fn()
        finally:
            for nm in names:
                delattr(self, nm)
        return rec

    def replay(self, *recs):
        pos = [0] * len(recs)
        total = sum(len(r) for r in recs)
        for _ in range(total):
            best = min((pos[i] / len(r), i) for i, r in enumerate(recs) if pos[i] < len(r))[1]
            nm, a, kw = recs[best][pos[best]]
            pos[best] += 1
            getattr(K, nm)(self, *a, **kw)

    def _clean(self):
        for t in self.tiles:
            t.wr = None
            t.rd = {}

    def barrier(self, keys=None):
        keys = sorted(self.dirty) if keys is None else sorted(keys)
        for e in self.ENG:
            for key in keys:
                self._wait(e, [(key, self.cur[key])])
        self.dirty -= set(keys)
        self._clean()

    def _release(self):
        for e in ("pe", "act", "dve", "pool"):
            h = self._sem("r_" + e)
            self.nc.sync.sem_inc(h, 1)
            self.E[e].wait_ge(h, 1)
            self.E[e].sem_clear(h)

    def _reset(self):
        for key in sorted(self.cur):
            if key.startswith("r_"):
                continue
            if self.cur[key].c:
                self.nc.sync.wait_ge(self.sems[key], self.cur[key].c)
                self.nc.sync.sem_clear(self.sems[key])
                self.cur[key] = Aff(0)
        self.dirty = set()
        self.known = {e: {} for e in self.ENG}
        self._clean()
        self._release()

    def loop_reset(self, n, body):
        assert n >= 1
        self.barrier()
        self._reset()
        with self.nc.Fori(0, n) as i:
            body(i)
            self._reset()


    def loop(self, n, body):
        assert n >= 1
        self.barrier()
        save = dict(self.cur)
        sk = {e: dict(v) for e, v in self.known.items()}
        self.dry += 1
        body(0)
        self.dry -= 1
        per = {}
        for key, a in self.cur.items():
            d = a.c - save[key].c if key in save else a.c
            if d:
                per[key] = d
        for key in list(self.cur):
            if key not in save:
                save[key] = Aff(0)
        self.cur = dict(save)
        self.known = sk
        self._clean()
        lid = self.nloop
        self.nloop += 1
        for key, d in sorted(per.items()):
            self.cur[key] = self.cur[key].add(d)
            if not self.dry:
                self.nc.sync.sem_inc(self.sems[key], d)
        for e in self.ENG:
            for key in sorted(per):
                self._wait(e, [(key, self.cur[key])])
        base = dict(self.cur)
        kn_entry = {e: dict(v) for e, v in self.known.items()}

        def set_iter(off):
            for key, d in per.items():
                b = base[key]
                self.cur[key] = Aff(b.c + off * d, b.k + ((lid, d),))

        if self.dry:
            for key, d in per.items():
                self.cur[key] = base[key].add(n * d)
            self.dirty |= set(per)
            self.barrier()
            return
        self.dry += 1
        set_iter(-1)
        self.known = {e: {} for e in self.ENG}
        body(0)
        self.dry -= 1
        with self.nc.Fori(0, n) as i:
            self.loops.append((lid, i))
            set_iter(0)
            self.known = {e: {} for e in self.ENG}
            body(i)
            self.loops.pop()
        for key, d in per.items():
            self.cur[key] = base[key].add(n * d)
        self.known = kn_entry
        self.dirty |= set(per)
        self.barrier()


GAMMA = [1.0 - 2.0 ** (-5.0 - h) for h in range(8)]


def make_consts():
    c = {}
    c["ident_bf"] = np.eye(128, dtype=np.float32).astype(ml_dtypes.bfloat16)
    c["ident_f"] = np.eye(128, dtype=np.float32)
    sp_, s_ = np.meshgrid(np.arange(128), np.arange(128), indexing="ij")
    c["tri"] = (sp_ > s_).astype(np.float32).astype(ml_dtypes.bfloat16)
    c["compl"] = (sp_ <= s_).astype(np.float32).astype(ml_dtypes.bfloat16)
    c["ntinc"] = (-(sp_ >= s_).astype(np.float32)).astype(ml_dtypes.bfloat16)
    c["nones"] = (-np.ones((128, 128), np.float32)).astype(ml_dtypes.bfloat16)
    m = np.zeros((128, 4, 512), np.float32)
    for r in range(4):
        s, t = np.meshgrid(np.arange(128), np.arange(512), indexing="ij")
        m[:, r, :] = (s + r * 128 < t)
    c["sbmask"] = m.astype(ml_dtypes.bfloat16)
    n = np.arange(128, dtype=np.float64)
    dm = np.zeros((128, 8, 128), np.float64)
    xi = np.zeros((128, 8), np.float64)
    zs = np.zeros((128, 8), np.float64)
    for h in range(8):
        g = GAMMA[h]
        dm[:, h, :] = np.where(n[None, :] >= n[:, None], g ** (-(n[:, None] + 1.0)), 0.0)
        xi[:, h] = g ** (n + 1.0)
        zs[:, h] = g ** (127.0 - n) * (128.0 ** -0.5)
    c["dmaskT"] = dm.astype(np.float32)
    c["xi"] = xi.astype(np.float32)
    c["zetas"] = zs.astype(np.float32)
    inv_freq = (1.0 / (10000.0 ** (np.arange(0, 128, 2, dtype=np.float32) / 128))).astype(np.float32)
    c["invfreq"] = np.tile(inv_freq[None, :], (128, 1)).astype(np.float32)
    c["iota"] = np.arange(128, dtype=np.int32).reshape(128, 1)
    c["ones16"] = np.ones((128, 16), np.float32)
    return c


CONST_DT = {"ident_bf": BF16, "ident_f": F32, "tri": BF16, "compl": BF16, "ntinc": BF16, "nones": BF16, "sbmask": BF16,
            "dmaskT": F32, "xi": F32, "zetas": F32, "invfreq": F32, "iota": I32, "ones16": F32}

WSHAPES = {
    "ada_w": [4, 2, 1024, 3072], "ada_b": [4, 2, 3072], "ln_g": [4, 2, 1024], "ln_b": [4, 2, 1024],
    "sb_w_in": [2, 1024, 3072], "sb_w_out": [2, 1024, 1024], "ret_w_in": [2, 1024, 6144],
    "ret_gn_g": [2, 2048], "ret_w_out": [2, 2048, 1024], "router_w": [1024, 16], "router_b": [16],
    "moe_w_gate": [4, 16, 1024, 512], "moe_w_up": [4, 16, 1024, 512], "moe_w_down": [4, 16, 512, 1024],
}


class Prog:
    def __init__(self, S, dbg=False):
        self.S = S
        self.NT = S // 128
        self.nc = nc = bass.Bass("TRN2", target_bir_lowering=False)
        self.k = K(nc)
        self.dbg = dbg
        inp = lambda name, shape, dt=F32: nc.dram_tensor(name, list(shape), dt, kind="ExternalInput").ap()
        self.x_in = inp("x", [S, D])
        self.cT = inp("cT", [128, 8])
        self.posT = inp("posT", [128, self.NT], I32)
        self.W = {n: inp(n, s) for n, s in WSHAPES.items()}
        cs = make_consts()
        self.C = {n: inp("c_" + n, cs[n].shape, CONST_DT[n]) for n in cs}
        self.out = nc.dram_tensor("out", [S, D], F32, kind="ExternalOutput").ap()
        self.scr = {}
        self.moe_ne = 16
        self.moe_lvl = 3
        self.warm_n = 0
        self.moe_ns = 8 if S % 1024 == 0 else 4

    def scratch(self, name, shape, dt):
        if name not in self.scr:
            kind = "ExternalOutput" if self.dbg else "Internal"
            self.scr[name] = self.nc.dram_tensor("s_" + name, list(shape), dt, kind=kind).ap()
        return self.scr[name]


def bc_load(P, es, name, src_row):
    t = P.k.tile(es, name, [128, src_row.shape[-1]], F32)
    P.k.dma("sp", t[:], src_row.partition_broadcast(128), w=[t])
    return t


def phase_adaln(P):
    nc, k = P.nc, P.k
    MOD = P.scratch("mod", [8, 3072], F32)
    with ExitStack() as es:
        ct = k.tile(es, "ct", [128, 8], F32)
        sc = k.tile(es, "sc", [128, 8], F32)
        Wt = k.tile(es, "adaW", [128, 8, 3072], F32)
        bias = k.tile(es, "adab", [1, 3072], F32)
        res = k.tile(es, "adar", [1, 3072], F32)
        ps = [k.ptile(es, f"adaps{i}", [1, 512]) for i in range(2)]
        k.dma("sp", ct[:], P.cT, w=[ct])
        k.op("act", lambda: nc.scalar.activation(out=sc[:], in_=ct[:], func=AF.Silu), r=[ct], w=[sc])
        for j in range(8):
            l, s = divmod(j, 2)
            k.dma("sp", Wt[:], P.W["ada_w"][l, s].rearrange("(k p) n -> p k n", p=128), w=[Wt])
            k.dma("sp", bias[:], P.W["ada_b"][l, s:s + 1, :], w=[bias])
            for n in range(6):
                p = ps[n % 2]
                for kc in range(8):
                    k.op("pe", lambda p=p, kc=kc, n=n: nc.tensor.matmul(
                        p[0:1, :], lhsT=sc[:, kc:kc + 1], rhs=Wt[:, kc, n * 512:(n + 1) * 512],
                        start=(kc == 0), stop=(kc == 7)), r=[sc, Wt], w=[p], inc=(kc == 7))
                k.op("dve", lambda p=p, n=n: nc.vector.tensor_tensor(
                    res[0:1, n * 512:(n + 1) * 512], p[0:1, :], bias[0:1, n * 512:(n + 1) * 512], ALU.add),
                    r=[p, bias], w=[res])
            k.op("dve", lambda: nc.vector.tensor_scalar_add(res[0:1, 1024:3072], res[0:1, 1024:3072], 1.0),
                 r=[res], w=[res])
            k.dma("sp", MOD[j:j + 1, :], res[0:1, :], r=[res])
    k.barrier()


def make_idx(P, es, name, bases):
    nc, k = P.nc, P.k
    n = len(bases)
    io = k.tile(es, name + "_io", [128, 1], I32)
    k.dma("sp", io[:], P.C["iota"], w=[io])
    t = k.tile(es, name, [128, n], I32)
    for j, b in enumerate(bases):
        k.op("dve", lambda j=j, b=b: nc.vector.tensor_scalar(t[:, j:j + 1], io[:], float(b), None, ALU.add),
             r=[io], w=[t])
    return t


def bump_idx(P, t, step):
    P.k.op("dve", lambda: P.nc.vector.tensor_scalar(t[:], t[:], float(step), None, ALU.add), r=[t], w=[t])


def modulate(P, xt, scale_bc, shift_bc, out_t):
    nc, k = P.nc, P.k
    k.op("dve", lambda: nc.vector.tensor_tensor(xt[:], xt[:], scale_bc[:], ALU.mult), r=[xt, scale_bc], w=[xt])
    k.op("dve", lambda: nc.vector.tensor_tensor(out_t[:], xt[:], shift_bc[:], ALU.add),
         r=[xt, shift_bc], w=[out_t])


def phase_sb_inproj(P, li, x_src):
    nc, k, S = P.nc, P.k, P.S
    NTB = S // 512
    MOD = P.scratch("mod", [8, 3072], F32)
    A = P.scratch("sbA", [NTB * 128, 24 * 512], BF16)
    B = P.scratch("sbB", [24 * 128, S], BF16)
    j = (2 * li) * 2 + 0
    with ExitStack() as es:
        Win = k.tile(es, "Win", [128, 8, 3072], BF16)
        k.dma("pool", Win[:], P.W["sb_w_in"][li].rearrange("(k p) n -> p k n", p=128), w=[Win])
        ident = k.tile(es, "ident", [128, 128], BF16)
        k.dma("sp", ident[:], P.C["ident_bf"], w=[ident])
        shift_bc = bc_load(P, es, "shift_bc", MOD[j:j + 1, 0:1024])
        scale_bc = bc_load(P, es, "scale_bc", MOD[j:j + 1, 1024:2048])
        idx = make_idx(P, es, "idx", [sub * 128 for sub in range(4)] + [0])
        xt = [k.tile(es, f"xt{u}", [128, 1024], F32) for u in range(2)]
        hb = [k.tile(es, f"hb{u}", [128, 1024], BF16) for u in range(2)]
        hT = k.tile(es, "hT", [128, 8, 512], BF16)
        tp = [k.ptile(es, f"tp{u}", [128, 8, 128], BF16) for u in range(2)]
        mm = [k.ptile(es, f"mm{u}", [128, 512]) for u in range(4)]
        obig = k.tile(es, "obig", [128, 24, 512], BF16)

        def body(tb):
            for sub in range(4):
                u = sub % 2
                k.gather(xt[u][:], x_src, idx[:, sub:sub + 1], r=[idx], w=[xt[u]])
                modulate(P, xt[u], scale_bc, shift_bc, hb[u])
                for kc in range(8):
                    k.op("pe", lambda u=u, kc=kc: nc.tensor.transpose(
                        tp[u][:, kc, :], hb[u][:, kc * 128:(kc + 1) * 128], ident[:]),
                        r=[hb[u], ident], w=[tp[u]])
                k.op("act", lambda u=u, sub=sub: nc.scalar.copy(
                    out=hT[:, :, sub * 128:(sub + 1) * 128], in_=tp[u][:]), r=[tp[u]], w=[hT])
            for oc in range(24):
                m = oc % 4
                for kc in range(8):
                    k.op("pe", lambda m=m, kc=kc, oc=oc: nc.tensor.matmul(
                        mm[m][:], lhsT=Win[:, kc, oc * 128:(oc + 1) * 128], rhs=hT[:, kc, :],
                        start=(kc == 0), stop=(kc == 7)), r=[Win, hT], w=[mm[m]], inc=(kc == 7))
                sc_ = 0.125 if oc < 8 else 1.0
                if oc % 2 == 0:
                    k.op("act", lambda m=m, oc=oc, sc_=sc_: nc.scalar.activation(
                        out=obig[:, oc, :], in_=mm[m][:], func=AF.Copy, scale=sc_), r=[mm[m]], w=[obig])
                else:
                    k.op("dve", lambda m=m, oc=oc, sc_=sc_: nc.vector.tensor_scalar(
                        obig[:, oc, :], mm[m][:], sc_, None, ALU.mult), r=[mm[m]], w=[obig])
            k.scatter(A, obig[:].rearrange("p a b -> p (a b)"), idx[:, 4:5], r=[obig, idx])
            k.op("dve", lambda: nc.vector.tensor_scalar(idx[:, 0:4], idx[:, 0:4], 512.0, None, ALU.add),
                 r=[idx], w=[idx])
            k.op("dve", lambda: nc.vector.tensor_scalar(idx[:, 4:5], idx[:, 4:5], 128.0, None, ALU.add),
                 r=[idx], w=[idx])
        k.loop(NTB, body)
        Av = A.rearrange("(tb p) (oc t) -> oc p tb t", p=128, t=512)
        Bv = B.rearrange("(oc p) (tb t) -> oc p tb t", p=128, t=512)
        for oc in range(24):
            k.dma("sp" if oc % 2 == 0 else "act", Bv[oc], Av[oc], key=f"d_rl{oc % 8}")
    k.barrier()


def phase_sb_attn(P):
    nc, k, S = P.nc, P.k, P.S
    B = P.scratch("sbB", [24 * 128, S], BF16)
    OTB = P.scratch("sbOT", [1024, S], BF16)
    Cc = P.scratch("sbC", [S, 1024], BF16)
    NT = S // 128
    NC = S // 512
    with ExitStack() as es:
        tri = k.tile(es, "tri", [128, 128], BF16)
        cpl = k.tile(es, "cpl", [128, 128], BF16)
        msk = k.tile(es, "msk", [128, 4, 512], BF16)
        ident = k.tile(es, "ident", [128, 128], BF16)
        k.dma("sp", tri[:], P.C["ntinc"], w=[tri])
        k.dma("sp", cpl[:], P.C["nones"], w=[cpl])
        k.dma("sp", msk[:], P.C["sbmask"], w=[msk])
        k.dma("sp", ident[:], P.C["ident_bf"], w=[ident])
        idx = make_idx(P, es, "idx", [0, 1024, 2048, 0])
        qT = k.tile(es, "qT", [128, S], BF16)
        kT = k.tile(es, "kT", [128, S], BF16)
        vT = k.tile(es, "vT", [128, S], BF16)
        vp = k.tile(es, "vp", [128, NT, 128], BF16)
        osb = k.tile(es, "osb", [128, S], BF16)
        Z = [[k.ptile(es, f"Z{a}{u}", [128, 512]) for u in range(3)] for a in range(2)]
        SPACC = [k.tile(es, f"SPACC{a}", [128, 512], BF16) for a in range(2)]
        OTp = [k.ptile(es, f"OTp{a}", [128, 512]) for a in range(2)]
        Et = [[k.tile(es, f"E{a}{u}", [128, 512], F32) for u in range(3)] for a in range(2)]
        SPt = [[k.tile(es, f"SP{a}{u}", [128, 512], BF16) for u in range(3)] for a in range(2)]
        At = [[k.tile(es, f"A{a}{u}", [128, 512], BF16) for u in range(3)] for a in range(2)]

        def stageA(g):
            c, j, r, u, first, last = g
            qs = slice(c * 512, (c + 1) * 512)
            for a in range(2):
                pa = slice(a * 64, (a + 1) * 64)
                k.op("pe", lambda a=a, pa=pa: nc.tensor.matmul(
                    Z[a][u][:], lhsT=kT[pa, j * 128:(j + 1) * 128], rhs=qT[pa, qs],
                    start=True, stop=False, skip_group_check=True), r=[kT, qT], w=[Z[a][u]])
            for a in range(2):
                k.op("act", lambda a=a: nc.scalar.activation(
                    out=Et[a][u][:], in_=Z[a][u][:], func=AF.Exp), r=[Z[a][u]], w=[Et[a][u]])
            for a in range(2):
                k.op("act", lambda a=a: nc.scalar.activation(
                    out=SPt[a][u][:], in_=Et[a][u][:], func=AF.Ln, bias=P.one_t[:, 0:1]),
                    r=[Et[a][u], P.one_t], w=[SPt[a][u]])
                if r is not None:
                    k.op("dve", lambda a=a: nc.vector.tensor_tensor(
                        SPt[a][u][:], SPt[a][u][:], msk[:, r, :], ALU.mult), r=[SPt[a][u], msk], w=[SPt[a][u]])

        def stageB(g):
            c, j, r, u, first, last = g
            for a in range(2):
                k.op("pe", lambda a=a: nc.tensor.matmul(
                    Z[a][u][:], lhsT=tri[:], rhs=SPt[a][u][:], start=False, stop=first, skip_group_check=True),
                    r=[tri, SPt[a][u]], w=[Z[a][u]])
                if not first:
                    k.op("pe", lambda a=a: nc.tensor.matmul(
                        Z[a][u][:], lhsT=cpl[:], rhs=SPACC[a][:], start=False, stop=True, skip_group_check=True),
                        r=[cpl, SPACC[a]], w=[Z[a][u]])
            if not last:
                for a in range(2):
                    if first:
                        k.op("pool", lambda a=a: nc.gpsimd.tensor_copy(SPACC[a][:], SPt[a][u][:]),
                             r=[SPt[a][u]], w=[SPACC[a]])
                    else:
                        k.op("pool", lambda a=a: nc.gpsimd.tensor_tensor(
                            SPACC[a][:], SPACC[a][:], SPt[a][u][:], ALU.add), r=[SPACC[a], SPt[a][u]], w=[SPACC[a]])
            for a in range(2):
                k.op("act", lambda a=a: nc.scalar.activation(
                    out=At[a][u][:], in_=Z[a][u][:], func=AF.Exp), r=[Z[a][u]], w=[At[a][u]])
                if r is not None:
                    k.op("dve", lambda a=a: nc.vector.tensor_tensor(
                        At[a][u][:], At[a][u][:], msk[:, r, :], ALU.mult), r=[At[a][u], msk], w=[At[a][u]])
            for a in range(2):
                k.op("pe", lambda a=a: nc.tensor.matmul(
                    OTp[a][:], lhsT=vp[:, j, :], rhs=At[a][u][:], start=first, stop=True, skip_group_check=True),
                    r=[vp, At[a][u]], w=[OTp[a]])

        def evac(c):
            k.op("act", lambda: nc.scalar.copy(out=osb[0:64, c * 512:(c + 1) * 512], in_=OTp[0][0:64, :]),
                 r=[OTp[0]], w=[osb])
            k.op("dve", lambda: nc.vector.tensor_copy(osb[64:128, c * 512:(c + 1) * 512], OTp[1][64:128, :]),
                 r=[OTp[1]], w=[osb])

        def pair_body(hp):
            k.gather(qT[:], B, idx[:, 0:1], r=[idx], w=[qT])
            k.gather(kT[:], B, idx[:, 1:2], r=[idx], w=[kT])
            k.gather(vT[:], B, idx[:, 2:3], r=[idx], w=[vT])
            for g in range(NT // 8):
                a = g % 2
                tpv = Z[a][0][:].bitcast(BF16).rearrange("p (j d) -> p j d", d=128)
                for jj in range(8):
                    j = g * 8 + jj
                    k.op("pe", lambda tpv=tpv, jj=jj, j=j: nc.tensor.transpose(
                        tpv[:, jj, :], vT[:, j * 128:(j + 1) * 128], ident[:]), r=[vT, ident], w=[Z[a][0]])
                k.op("act", lambda tpv=tpv, g=g: nc.scalar.copy(out=vp[:, g * 8:(g + 1) * 8, :], in_=tpv),
                     r=[Z[a][0]], w=[vp])
            groups = []
            for c in range(NC):
                js = [(4 * c + 3, 3), (4 * c + 2, 2), (4 * c + 1, 1), (4 * c, 0)] + \
                     [(j, None) for j in range(4 * c - 1, -1, -1)]
                for t, (j, r) in enumerate(js):
                    groups.append((c, j, r, len(groups) % 3, t == 0, t == len(js) - 1))
            for w_ in range(P.warm_n):
                k.op("pe", lambda: nc.tensor.matmul(Z[0][2][:], lhsT=tri[:], rhs=msk[:, 0, :], start=True, stop=True,
                                                    skip_group_check=True), r=[tri, msk], w=[Z[0][2]],
                     inc=(w_ == P.warm_n - 1))
            stageA(groups[0])
            stageA(groups[1])
            for n, g in enumerate(groups):
                if n + 2 < len(groups):
                    stageA(groups[n + 2])
                stageB(g)
                if g[5]:
                    evac(g[0])
            k.scatter(OTB, osb[:], idx[:, 3:4], r=[osb, idx])
            bump_idx(P, idx, 128)
        k.loop(8, pair_body)
        Ov = OTB.rearrange("(kc p) (i t) -> kc p i t", p=128, t=128)
        Cv = Cc.rearrange("(i p) (kc t) -> kc p i t", p=128, t=128)
        for kc in range(8):
            k.dma("sp" if kc % 2 == 0 else "act", Cv[kc], Ov[kc], key=f"d_rl{kc}")
    k.barrier()


def layer_norm_tile(P, r, st, mv, rstd, g_bc, b_bc, out_t):
    nc, k = P.nc, P.k
    for hh in range(2):
        k.op("dve", lambda hh=hh: nc.vector.bn_stats(st[:, hh, :], r[:, hh * 512:(hh + 1) * 512]),
             r=[r], w=[st])
    k.op("dve", lambda: nc.vector.bn_aggr(mv[:], st[:].rearrange("p a b -> p (a b)")), r=[st], w=[mv])
    k.op("act", lambda: nc.scalar.activation(out=rstd[:], in_=mv[:, 1:2], func=AF.Sqrt, bias=P.eps_t[:, 0:1]),
         r=[mv, P.eps_t], w=[rstd])
    k.op("dve", lambda: nc.vector.reciprocal(rstd[:], rstd[:]), r=[rstd], w=[rstd])
    k.op("dve", lambda: nc.vector.tensor_scalar(mv[:, 0:1], mv[:, 0:1], rstd[:, 0:1], -1.0, ALU.mult, ALU.mult),
         r=[mv, rstd], w=[mv])
    k.op("act", lambda: nc.scalar.activation(out=r[:], in_=r[:], func=AF.Identity, bias=mv[:, 0:1],
                                             scale=rstd[:, 0:1]), r=[r, mv, rstd], w=[r])
    k.op("pool", lambda: nc.gpsimd.tensor_tensor(r[:], r[:], g_bc[:], ALU.mult), r=[r, g_bc], w=[r])
    k.op("dve", lambda: nc.vector.tensor_tensor(out_t[:], r[:], b_bc[:], ALU.add), r=[r, b_bc], w=[out_t])


def phase_post(P, A, KC, fm, w_out, modj, lng, lnb, x_src, x_dst):
    nc, k, S = P.nc, P.k, P.S
    MOD = P.scratch("mod", [8, 3072], F32)
    with ExitStack() as es:
        Wo = k.tile(es, "Wo", [128, KC, 1024], BF16)
        k.dma("pool", Wo[:], w_out.rearrange("(k p) n -> p k n", p=128), w=[Wo])
        gate_bc = bc_load(P, es, "gate_bc", MOD[modj:modj + 1, 2048:3072])
        g_bc = bc_load(P, es, "g_bc", lng)
        b_bc = bc_load(P, es, "b_bc", lnb)
        ident = k.tile(es, "ident", [128, 128], BF16)
        k.dma("sp", ident[:], P.C["ident_bf"], w=[ident])
        U = 2
        idx = make_idx(P, es, "idx", [u * 128 for u in range(U)])
        at = [k.tile(es, f"at{u}", [128, KC * 128], BF16) for u in range(U)]
        if not fm:
            gt = [k.tile(es, f"gt{u}", [128, KC * 128], BF16) for u in range(U)]
            tp = [k.ptile(es, f"tp{u}", [128, 8, 128], BF16) for u in range(U)]
        xt = [k.tile(es, f"xt{u}", [128, 1024], F32) for u in range(U)]
        rt = [k.tile(es, f"rt{u}", [128, 1024], F32) for u in range(U)]
        yo = [k.tile(es, f"yo{u}", [128, 1024], F32) for u in range(U)]
        st = [k.tile(es, f"st{u}", [128, 2, 6], F32) for u in range(U)]
        mv = [k.tile(es, f"mv{u}", [128, 2], F32) for u in range(U)]
        rstd = [k.tile(es, f"rstd{u}", [128, 1], F32) for u in range(U)]
        yp = [[k.ptile(es, f"yp{u}{h}", [128, 512]) for h in range(2)] for u in range(U)]

        def body(i):
            for u in range(U):
                k.gather(xt[u][:], x_src, idx[:, u:u + 1], r=[idx], w=[xt[u]])
                if fm:
                    k.gather(at[u][:], A, idx[:, u:u + 1], r=[idx], w=[at[u]])
                else:
                    k.gather(gt[u][:], A, idx[:, u:u + 1], r=[idx], w=[gt[u]])
                    for g8 in range(KC // 8):
                        for kc in range(8):
                            kk = g8 * 8 + kc
                            k.op("pe", lambda u=u, kc=kc, kk=kk: nc.tensor.transpose(
                                tp[u][:, kc, :], gt[u][:, kk * 128:(kk + 1) * 128], ident[:]),
                                r=[gt[u], ident], w=[tp[u]])
                        k.op("act", lambda u=u, g8=g8: nc.scalar.copy(
                            out=at[u][:, g8 * 1024:(g8 + 1) * 1024].rearrange("p (a b) -> p a b", b=128),
                            in_=tp[u][:]), r=[tp[u]], w=[at[u]])
                for h in range(2):
                    for kc in range(KC):
                        k.op("pe", lambda u=u, h=h, kc=kc: nc.tensor.matmul(
                            yp[u][h][:], lhsT=at[u][:, kc * 128:(kc + 1) * 128],
                            rhs=Wo[:, kc, h * 512:(h + 1) * 512],
                            start=(kc == 0), stop=(kc == KC - 1)), r=[at[u], Wo], w=[yp[u][h]], inc=(kc == KC - 1))
                for h in range(2):
                    hs = slice(h * 512, (h + 1) * 512)
                    k.op("dve", lambda u=u, h=h, hs=hs: nc.vector.tensor_tensor(
                        rt[u][:, hs], yp[u][h][:], gate_bc[:, hs], ALU.mult), r=[yp[u][h], gate_bc], w=[rt[u]])
                k.op("dve", lambda u=u: nc.vector.scalar_tensor_tensor(
                    rt[u][:], xt[u][:], ALPHA, rt[u][:], ALU.mult, ALU.add), r=[xt[u], rt[u]], w=[rt[u]])
                layer_norm_tile(P, rt[u], st[u], mv[u], rstd[u], g_bc, b_bc, yo[u])
                k.scatter(x_dst, yo[u][:], idx[:, u:u + 1], r=[yo[u], idx])
            bump_idx(P, idx, 128 * U)
        k.loop(S // (128 * U), body)
    k.barrier()


def phase_moe(P, li, x_src, x_dst):
    nc, k, S = P.nc, P.k, P.S
    NS = P.moe_ns
    NH = NS // 4
    NTB = S // (128 * NS)
    MOD = P.scratch("mod", [8, 3072], F32)
    j = (2 * li + 1)
    BIG = 1.0e30
    with ExitStack() as es:
        shift_bc = bc_load(P, es, "shift_bc", MOD[j:j + 1, 0:1024])
        scale_bc = bc_load(P, es, "scale_bc", MOD[j:j + 1, 1024:2048])
        gate_bc = bc_load(P, es, "gate_bc", MOD[j:j + 1, 2048:3072])
        g_bc = bc_load(P, es, "g_bc", P.W["ln_g"][li, 1:2, :])
        b_bc = bc_load(P, es, "b_bc", P.W["ln_b"][li, 1:2, :])
        rb_bc = bc_load(P, es, "rb_bc", P.W["router_b"].rearrange("(o e) -> o e", o=1))
        rw = k.tile(es, "rw", [128, 8, 16], F32)
        k.dma("sp", rw[:], P.W["router_w"].rearrange("(k p) e -> p k e", p=128), w=[rw])
        identf = k.tile(es, "identf", [128, 128], F32)
        k.dma("sp", identf[:], P.C["ident_f"], w=[identf])
        idx = make_idx(P, es, "idx", [sub * 128 for sub in range(NS)])
        xs = [k.tile(es, f"xs{s_}", [128, 1024], F32) for s_ in range(2)]
        hf = [k.tile(es, f"hf{u}", [128, 1024], F32) for u in range(2)]
        hT32 = [k.tile(es, f"hT32{u}", [128, 8, 128], F32) for u in range(2)]
        hT = k.tile(es, "hT", [128, 8, 128 * NS], BF16)
        comb = k.tile(es, "comb", [128, NS, 16], F32)
        yacc = [k.tile(es, f"yacc{s_}", [128, 1024], F32) for s_ in range(NS)]
        wg = [k.tile(es, f"wg{u}", [128, 8, 512], BF16) for u in range(2)]
        wu = [k.tile(es, f"wu{u}", [128, 8, 512], BF16) for u in range(2)]
        wd = [k.tile(es, f"wd{u}", [128, 4, 1024], BF16) for u in range(2)]
        sg = [k.tile(es, f"sg{u}", [128, 512], F32) for u in range(2)]
        hid = [k.tile(es, f"hid{u}", [128, 4, 512], BF16) for u in range(2)]
        sm = {n: k.tile(es, "r_" + n, [128, w_], F32) for n, w_ in
              [("lg", 16), ("mx", 1), ("nmx", 1), ("pe", 16), ("sum", 1), ("rs", 1), ("hi", 8), ("lo", 8),
               ("m1", 4), ("m2", 4), ("gs", 4), ("gm", 1), ("gmask", 4), ("ml", 16), ("pen", 16), ("v1", 1),
               ("eq1", 16), ("ml2", 16), ("v2", 1), ("eq2", 16), ("d", 1), ("w1", 1), ("w2", 1), ("t16", 16)]}
        st = [k.tile(es, f"st{u}", [128, 2, 6], F32) for u in range(2)]
        mv = [k.tile(es, f"mv{u}", [128, 2], F32) for u in range(2)]
        rstd = [k.tile(es, f"rstd{u}", [128, 1], F32) for u in range(2)]
        yo = [k.tile(es, f"yo{u}", [128, 1024], F32) for u in range(2)]
        tpf = [k.ptile(es, f"tpf{u}", [128, 4, 128], F32) for u in range(2)]
        Gp = [k.ptile(es, f"Gp{u}", [128, 512]) for u in range(2)]
        Up = [k.ptile(es, f"Up{u}", [128, 512]) for u in range(2)]
        Yp = [k.ptile(es, f"Yp{u}", [128, 512]) for u in range(2)]
        V = nc.vector

        def route(sub, lgp):
            T = sm
            def dv(fn, r, w):
                k.op("dve", fn, r=[T[x] if isinstance(x, str) else x for x in r],
                     w=[T[x] if isinstance(x, str) else x for x in w])
            dv(lambda: V.tensor_tensor(T["lg"][:], lgp[:, 0:16], rb_bc[:], ALU.add), [lgp, rb_bc], ["lg"])
            dv(lambda: V.reduce_max(T["mx"][:], T["lg"][:], axis=AX.X), ["lg"], ["mx"])
            dv(lambda: V.tensor_scalar(T["nmx"][:], T["mx"][:], -1.0, None, ALU.mult), ["mx"], ["nmx"])
            k.op("act", lambda: nc.scalar.activation(out=T["pe"][:], in_=T["lg"][:], func=AF.Exp,
                                                     bias=T["nmx"][:, 0:1]), r=[T["lg"], T["nmx"]], w=[T["pe"]])
            dv(lambda: V.reduce_sum(T["sum"][:], T["pe"][:], axis=AX.X), ["pe"], ["sum"])
            dv(lambda: V.reciprocal(T["rs"][:], T["sum"][:]), ["sum"], ["rs"])
            dv(lambda: V.tensor_scalar(T["pe"][:], T["pe"][:], T["rs"][:, 0:1], None, ALU.mult), ["pe", "rs"], ["pe"])
            pg = T["pe"][:].rearrange("p (g e) -> p g e", e=4)
            hi = T["hi"][:].rearrange("p (g e) -> p g e", e=2)
            lo = T["lo"][:].rearrange("p (g e) -> p g e", e=2)
            dv(lambda: V.tensor_tensor(hi, pg[:, :, 0:4:2], pg[:, :, 1:4:2], ALU.max), ["pe"], ["hi"])
            dv(lambda: V.tensor_tensor(lo, pg[:, :, 0:4:2], pg[:, :, 1:4:2], ALU.min), ["pe"], ["lo"])
            dv(lambda: V.tensor_tensor(T["m1"][:], hi[:, :, 0], hi[:, :, 1], ALU.max), ["hi"], ["m1"])
            dv(lambda: V.tensor_tensor(T["m2"][:], hi[:, :, 0], hi[:, :, 1], ALU.min), ["hi"], ["m2"])
            dv(lambda: V.tensor_tensor(T["gs"][:], lo[:, :, 0], lo[:, :, 1], ALU.max), ["lo"], ["gs"])
            dv(lambda: V.tensor_tensor(T["m2"][:], T["m2"][:], T["gs"][:], ALU.max), ["m2", "gs"], ["m2"])
            dv(lambda: V.tensor_tensor(T["gs"][:], T["m1"][:], T["m2"][:], ALU.add), ["m1", "m2"], ["gs"])
            dv(lambda: V.reduce_max(T["gm"][:], T["gs"][:], axis=AX.X), ["gs"], ["gm"])
            dv(lambda: V.tensor_scalar(T["gmask"][:], T["gs"][:], T["gm"][:, 0:1], None, ALU.is_ge),
               ["gs", "gm"], ["gmask"])
            mlv = T["ml"][:].rearrange("p (g e) -> p g e", e=4)
            penv = T["pen"][:].rearrange("p (g e) -> p g e", e=4)
            lgv = T["lg"][:].rearrange("p (g e) -> p g e", e=4)
            gmb = T["gmask"][:].unsqueeze(2).to_broadcast([128, 4, 4])
            dv(lambda: V.tensor_tensor(penv, P.ones16[:].rearrange("p (g e) -> p g e", e=4), gmb, ALU.mult),
               ["gmask", P.ones16], ["pen"])
            dv(lambda: V.tensor_scalar(T["pen"][:], T["pen"][:], -1.0, BIG, ALU.add, ALU.mult), ["pen"], ["pen"])
            dv(lambda: V.tensor_tensor(T["ml"][:], T["lg"][:], T["pen"][:], ALU.add), ["lg", "pen"], ["ml"])
            dv(lambda: V.reduce_max(T["v1"][:], T["ml"][:], axis=AX.X), ["ml"], ["v1"])
            dv(lambda: V.tensor_scalar(T["eq1"][:], T["ml"][:], T["v1"][:, 0:1], None, ALU.is_ge), ["ml", "v1"], ["eq1"])
            dv(lambda: V.scalar_tensor_tensor(T["ml2"][:], T["eq1"][:], -BIG, T["ml"][:], ALU.mult, ALU.add),
               ["eq1", "ml"], ["ml2"])
            dv(lambda: V.reduce_max(T["v2"][:], T["ml2"][:], axis=AX.X), ["ml2"], ["v2"])
            dv(lambda: V.tensor_scalar(T["eq2"][:], T["ml2"][:], T["v2"][:, 0:1], None, ALU.is_ge), ["ml2", "v2"], ["eq2"])
            dv(lambda: V.tensor_tensor(T["d"][:], T["v2"][:], T["v1"][:], ALU.subtract), ["v2", "v1"], ["d"])
            k.op("act", lambda: nc.scalar.activation(out=T["d"][:], in_=T["d"][:], func=AF.Exp), r=[T["d"]], w=[T["d"]])
            dv(lambda: V.tensor_scalar(T["w1"][:], T["d"][:], 1.0, None, ALU.add), ["d"], ["w1"])
            dv(lambda: V.reciprocal(T["w1"][:], T["w1"][:]), ["w1"], ["w1"])
            dv(lambda: V.tensor_tensor(T["w2"][:], T["d"][:], T["w1"][:], ALU.mult), ["d", "w1"], ["w2"])
            dv(lambda: V.tensor_scalar(T["t16"][:], T["eq1"][:], T["w1"][:, 0:1], None, ALU.mult), ["eq1", "w1"], ["t16"])
            dv(lambda: V.scalar_tensor_tensor(comb[:, sub, :], T["eq2"][:], T["w2"][:, 0:1], T["t16"][:],
                                              ALU.mult, ALU.add), ["eq2", "w2", "t16"], [comb])

        def load_w(e):
            u = e % 2
            k.dma("pool", wg[u][:], P.W["moe_w_gate"][li, e].rearrange("(k p) f -> p k f", p=128), w=[wg[u]])
            k.dma("pool", wu[u][:], P.W["moe_w_up"][li, e].rearrange("(k p) f -> p k f", p=128), w=[wu[u]])
            k.dma("pool", wd[u][:], P.W["moe_w_down"][li, e].rearrange("(k p) d -> p k d", p=128), w=[wd[u]])

        def body(tb):
            if P.moe_ne > 0:
                load_w(0)
            for sub in range(NS):
                u = sub % 2
                k.gather(xs[u][:], x_src, idx[:, sub:sub + 1], r=[idx], w=[xs[u]])
                if P.moe_lvl < 1:
                    continue
                k.op("dve", lambda sub=sub, u=u: V.tensor_tensor(hf[u][:], xs[u][:], scale_bc[:], ALU.mult),
                     r=[xs[u], scale_bc], w=[hf[u]])
                k.op("dve", lambda u=u: V.tensor_tensor(hf[u][:], hf[u][:], shift_bc[:], ALU.add),
                     r=[hf[u], shift_bc], w=[hf[u]])
                for g in range(2):
                    if P.moe_lvl < 0.5:
                        continue
                    for kk in range(4):
                        kc = g * 4 + kk
                        k.op("pe", lambda g=g, kk=kk, kc=kc, u=u: nc.tensor.matmul(
                            tpf[g][:, kk, :], lhsT=hf[u][:, kc * 128:(kc + 1) * 128], rhs=identf[:],
                            start=True, stop=True), r=[hf[u], identf], w=[tpf[g]])
                    if P.moe_lvl < 0.7:
                        continue
                    k.op("act", lambda g=g, u=u: nc.scalar.copy(out=hT32[u][:, g * 4:(g + 1) * 4, :], in_=tpf[g][:]),
                         r=[tpf[g]], w=[hT32[u]])
                    if P.moe_lvl < 0.9:
                        continue
                    k.op("act", lambda g=g, sub=sub: nc.scalar.copy(
                        out=hT[:, g * 4:(g + 1) * 4, sub * 128:(sub + 1) * 128], in_=tpf[g][:]), r=[tpf[g]], w=[hT])
                lgp = Yp[u]
                if P.moe_lvl < 2:
                    continue
                for kc in range(8):
                    k.op("pe", lambda kc=kc, u=u, lgp=lgp: nc.tensor.matmul(
                        lgp[:, 0:16], lhsT=hT32[u][:, kc, :], rhs=rw[:, kc, :], start=(kc == 0), stop=(kc == 7)),
                        r=[hT32[u], rw], w=[lgp], inc=(kc == 7))
                if P.moe_lvl < 3:
                    continue
                route(sub, lgp)
            if P.dbg and P.moe_lvl >= 3:
                for sub in range(NS):
                    k.scatter(P.scratch("comb", [S, 16], F32), comb[:, sub, :], idx[:, sub:sub + 1], r=[comb, idx],
                              key="d_combdbg")
            for e in range(P.moe_ne):
                u = e % 2
                if e + 1 < P.moe_ne:
                    load_w(e + 1)
                for hh in range(NH):
                    hu = (e * NH + hh) % 2
                    ts = slice(hh * 512, (hh + 1) * 512)
                    for fc in range(4):
                        pu = fc % 2
                        fs = slice(fc * 128, (fc + 1) * 128)
                        for kc in range(8):
                            k.op("pe", lambda pu=pu, kc=kc, fs=fs, u=u, ts=ts: nc.tensor.matmul(
                                Gp[pu][:], lhsT=wg[u][:, kc, fs], rhs=hT[:, kc, ts], start=(kc == 0), stop=(kc == 7)),
                                r=[wg[u], hT], w=[Gp[pu]], inc=(kc == 7))
                        for kc in range(8):
                            k.op("pe", lambda pu=pu, kc=kc, fs=fs, u=u, ts=ts: nc.tensor.matmul(
                                Up[pu][:], lhsT=wu[u][:, kc, fs], rhs=hT[:, kc, ts], start=(kc == 0), stop=(kc == 7)),
                                r=[wu[u], hT], w=[Up[pu]], inc=(kc == 7))
                        k.op("act", lambda pu=pu: nc.scalar.activation(out=sg[pu][:], in_=Gp[pu][:], func=AF.Silu),
                             r=[Gp[pu]], w=[sg[pu]])
                        k.op("dve", lambda pu=pu, fc=fc, hu=hu: V.tensor_tensor(hid[hu][:, fc, :], sg[pu][:], Up[pu][:],
                                                                               ALU.mult), r=[sg[pu], Up[pu]], w=[hid[hu]])
                    for s4 in range(4):
                        sub = hh * 4 + s4
                        for nh in range(2):
                            py = (s4 * 2 + nh) % 2
                            for fc in range(4):
                                k.op("pe", lambda py=py, fc=fc, s4=s4, nh=nh, u=u, hu=hu: nc.tensor.matmul(
                                    Yp[py][:], lhsT=hid[hu][:, fc, s4 * 128:(s4 + 1) * 128],
                                    rhs=wd[u][:, fc, nh * 512:(nh + 1) * 512], start=(fc == 0), stop=(fc == 3)),
                                    r=[hid[hu], wd[u]], w=[Yp[py]], inc=(fc == 3))
                            hs = slice(nh * 512, (nh + 1) * 512)
                            if e == 0:
                                k.op("dve", lambda py=py, sub=sub, hs=hs, e=e: V.tensor_scalar(
                                    yacc[sub][:, hs], Yp[py][:], comb[:, sub, e:e + 1], None, ALU.mult),
                                    r=[Yp[py], comb], w=[yacc[sub]])
                            else:
                                k.op("dve", lambda py=py, sub=sub, hs=hs, e=e: V.scalar_tensor_tensor(
                                    yacc[sub][:, hs], Yp[py][:], comb[:, sub, e:e + 1], yacc[sub][:, hs],
                                    ALU.mult, ALU.add), r=[Yp[py], comb, yacc[sub]], w=[yacc[sub]])
            for sub in range(NS):
                u = sub % 2
                k.gather(xs[u][:], x_src, idx[:, sub:sub + 1], r=[idx], w=[xs[u]])
                k.op("dve", lambda sub=sub: V.tensor_tensor(yacc[sub][:], yacc[sub][:], gate_bc[:], ALU.mult),
                     r=[yacc[sub], gate_bc], w=[yacc[sub]])
                k.op("dve", lambda sub=sub, u=u: V.scalar_tensor_tensor(
                    yacc[sub][:], xs[u][:], ALPHA, yacc[sub][:], ALU.mult, ALU.add),
                    r=[xs[u], yacc[sub]], w=[yacc[sub]])
                layer_norm_tile(P, yacc[sub], st[u], mv[u], rstd[u], g_bc, b_bc, yo[u])
                k.scatter(x_dst, yo[u][:], idx[:, sub:sub + 1], r=[yo[u], idx])
            bump_idx(P, idx, 128 * NS)
        k.loop(NTB, body)
    k.barrier()


def phase_rope_table(P):
    nc, k, S = P.nc, P.k, P.S
    NT = S // 128
    TAB = P.scratch("rope", [S + 128, 128], F32)
    V = nc.vector
    TWO_PI = 6.283185307179586
    C1, C2 = 6.28125, TWO_PI - 6.28125
    PI_LO = 3.1415925
    with ExitStack() as es:
        pi_ = k.tile(es, "pos_i", [128, NT], I32)
        pf = k.tile(es, "pos_f", [128, NT], F32)
        inv = k.tile(es, "invf", [128, 64], F32)
        k.dma("sp", pi_[:], P.posT, w=[pi_])
        k.dma("sp", inv[:], P.C["invfreq"], w=[inv])
        k.op("dve", lambda: V.tensor_copy(pf[:], pi_[:]), r=[pi_], w=[pf])
        ang = [k.tile(es, f"ang{u}", [128, 128], F32) for u in range(2)]
        kq = [k.tile(es, f"kq{u}", [128, 128], F32) for u in range(2)]
        ki = [k.tile(es, f"ki{u}", [128, 128], I32) for u in range(2)]
        mk = [k.tile(es, f"mk{u}", [128, 128], F32) for u in range(2)]
        tb = [k.tile(es, f"tbl{u}", [128, 128], F32) for u in range(2)]
        for i in range(NT):
            u = i % 2
            a, q, qi, m, t = ang[u], kq[u], ki[u], mk[u], tb[u]
            k.op("dve", lambda a=a, i=i: V.tensor_scalar(a[:, 64:128], inv[:], pf[:, i:i + 1], None, ALU.mult),
                 r=[inv, pf], w=[a])
            k.op("dve", lambda a=a: V.tensor_scalar(a[:, 0:64], a[:, 64:128], 1.5707963267948966, None, ALU.add),
                 r=[a], w=[a])
            k.op("dve", lambda a=a, q=q: V.tensor_scalar(q[:], a[:], 1.0 / TWO_PI, None, ALU.mult), r=[a], w=[q])
            k.op("dve", lambda q=q, qi=qi: V.tensor_copy(qi[:], q[:]), r=[q], w=[qi])
            k.op("dve", lambda q=q, qi=qi: V.tensor_copy(q[:], qi[:]), r=[qi], w=[q])
            k.op("dve", lambda a=a, q=q: V.scalar_tensor_tensor(a[:], q[:], -C1, a[:], ALU.mult, ALU.add),
                 r=[a, q], w=[a])
            k.op("dve", lambda a=a, q=q: V.scalar_tensor_tensor(a[:], q[:], -C2, a[:], ALU.mult, ALU.add),
                 r=[a, q], w=[a])
            for sgn, cmp_ in ((-1.0, ALU.is_gt), (1.0, ALU.is_lt)):
                k.op("dve", lambda a=a, m=m, sgn=sgn, cmp_=cmp_: V.tensor_scalar(
                    m[:], a[:], -sgn * 3.141592653589793, None, cmp_), r=[a], w=[m])
                k.op("dve", lambda a=a, m=m, sgn=sgn: V.scalar_tensor_tensor(
                    a[:], m[:], sgn * TWO_PI, a[:], ALU.mult, ALU.add), r=[a, m], w=[a])
            k.op("dve", lambda a=a: V.tensor_scalar(a[:], a[:], PI_LO, -PI_LO, ALU.min, ALU.max), r=[a], w=[a])
            k.op("act", lambda a=a, t=t: nc.scalar.activation(out=t[:], in_=a[:], func=AF.Sin), r=[a], w=[t])
            k.dma("sp", TAB[i * 128:(i + 1) * 128, :], t[:], r=[t])
    k.barrier()


def phase_ret(P, li, x_src):
    nc, k, S = P.nc, P.k, P.S
    NT = S // 128
    MOD = P.scratch("mod", [8, 3072], F32)
    TAB = P.scratch("rope", [S + 128, 128], F32)
    G = P.scratch("retG", [S, 2048], BF16)
    j = (2 * li + 1) * 2 + 0
    V = nc.vector
    with ExitStack() as es:
        Win = k.tile(es, "Win", [128, 8, 6144], BF16)
        for part in range(4):
            k.dma("pool", Win[:, 2 * part:2 * part + 2, :],
                  P.W["ret_w_in"][li, part * 256:(part + 1) * 256, :].rearrange("(k p) n -> p k n", p=128),
                  w=[Win], key=f"d_Win{part}")
        ident = k.tile(es, "ident", [128, 128], BF16)
        k.dma("sp", ident[:], P.C["ident_bf"], w=[ident])
        shift_bc = bc_load(P, es, "shift_bc", MOD[j:j + 1, 0:1024])
        scale_bc = bc_load(P, es, "scale_bc", MOD[j:j + 1, 1024:2048])
        gn_bc = bc_load(P, es, "gn_bc", P.W["ret_gn_g"][li:li + 1, :])
        dmk = k.tile(es, "dmk", [128, 8, 128], F32)
        k.dma("sp", dmk[:], P.C["dmaskT"], w=[dmk])
        xi = k.tile(es, "xi", [128, 8], F32)
        k.dma("sp", xi[:], P.C["xi"], w=[xi])
        zs = k.tile(es, "zs", [128, 8], F32)
        k.dma("sp", zs[:], P.C["zetas"], w=[zs])
        idx = make_idx(P, es, "idx", [0, 128])
        xt = k.tile(es, "xt", [128, 1024], F32)
        hbs = [k.tile(es, f"hb{u}", [128, 1024], BF16) for u in range(2)]
        hTs = [k.tile(es, f"hT{u}", [128, 8, 128], BF16) for u in range(2)]
        css = [k.tile(es, f"cs{u}", [128, 128], F32) for u in range(2)]
        qf = k.tile(es, "qf", [128, 1024], F32)
        qr = k.tile(es, "qr", [128, 1024], F32)
        kf, kr = qf, qr
        t1 = k.tile(es, "t1", [128, 512], F32)
        t2 = k.tile(es, "t2", [128, 512], F32)
        qb = k.tile(es, "qb", [128, 1024], BF16)
        kb = k.tile(es, "kb", [128, 1024], BF16)
        kzs = [k.tile(es, f"kz{u}", [128, 1024], BF16) for u in range(2)]
        qTs = [k.tile(es, f"qT{u}", [128, 8, 128], BF16) for u in range(2)]
        kTs = [k.tile(es, f"kT{u}", [128, 8, 128], BF16) for u in range(2)]
        vbs = [k.tile(es, f"vb{u}", [128, 2048], BF16) for u in range(2)]
        sgts = [k.tile(es, f"sgt{u}", [128, 2048], BF16) for u in range(2)]
        gouts = [k.tile(es, f"gout{u}", [128, 2048], BF16) for u in range(2)]
        state = [k.tile(es, f"state{h}", [128, 256], F32) for h in range(8)]
        sbf = [k.tile(es, f"sbf{h}", [128, 256], BF16) for h in range(8)]
        Pm = [k.tile(es, f"Pm{u}", [128, 128], BF16) for u in range(2)]
        on = [k.tile(es, f"on{u}", [128, 256], F32) for u in range(2)]
        st = [k.tile(es, f"st{u}", [128, 6], F32) for u in range(2)]
        mv = [k.tile(es, f"mv{u}", [128, 2], F32) for u in range(2)]
        rstd = [k.tile(es, f"rstd{u}", [128, 1], F32) for u in range(2)]
        tp = [k.ptile(es, f"tp{u}", [128, 8, 128], BF16) for u in range(2)]
        mm = [k.ptile(es, f"mm{u}", [128, 512]) for u in range(2)]
        recA = [k.ptile(es, f"recA{u}", [128, 512]) for u in range(2)]
        recB = [k.ptile(es, f"recB{u}", [128, 512]) for u in range(2)]
        for h in range(8):
            k.op("dve", lambda h=h: V.memset(state[h][:], 0.0), w=[state[h]])
            k.op("dve", lambda h=h: V.memset(sbf[h][:], 0.0), w=[sbf[h]])

        def rotary(src, dst, cs):
            sv = src[:].rearrange("p (h t d) -> p h t d", h=8, t=2)
            dv_ = dst[:].rearrange("p (h t d) -> p h t d", h=8, t=2)
            cosb = cs[:, 0:64].unsqueeze(1).to_broadcast([128, 8, 64])
            sinb = cs[:, 64:128].unsqueeze(1).to_broadcast([128, 8, 64])
            a1 = t1[:].rearrange("p (h d) -> p h d", h=8)
            a2 = t2[:].rearrange("p (h d) -> p h d", h=8)
            k.op("dve", lambda: V.tensor_tensor(a1, sv[:, :, 0, :], cosb, ALU.mult), r=[src, cs], w=[t1])
            k.op("dve", lambda: V.tensor_tensor(a2, sv[:, :, 1, :], sinb, ALU.mult), r=[src, cs], w=[t2])
            k.op("dve", lambda: V.tensor_tensor(dv_[:, :, 0, :], a1, a2, ALU.subtract), r=[t1, t2], w=[dst])
            k.op("dve", lambda: V.tensor_tensor(a1, sv[:, :, 1, :], cosb, ALU.mult), r=[src, cs], w=[t1])
            k.op("dve", lambda: V.tensor_tensor(a2, sv[:, :, 0, :], sinb, ALU.mult), r=[src, cs], w=[t2])
            k.op("dve", lambda: V.tensor_tensor(dv_[:, :, 1, :], a1, a2, ALU.add), r=[t1, t2], w=[dst])

        def inproj(c_):
            hb, hT, cs, kz, qT, kT, vb, sgt = hbs[c_], hTs[c_], css[c_], kzs[c_], qTs[c_], kTs[c_], vbs[c_], sgts[c_]
            k.gather(xt[:], x_src, idx[:, c_:c_ + 1], r=[idx], w=[xt])
            k.gather(cs[:], TAB, idx[:, c_:c_ + 1], r=[idx], w=[cs])
            modulate(P, xt, scale_bc, shift_bc, hb)
            for kc in range(8):
                k.op("pe", lambda kc=kc: nc.tensor.transpose(tp[0][:, kc, :], hb[:, kc * 128:(kc + 1) * 128],
                                                             ident[:]), r=[hb, ident], w=[tp[0]])
            k.op("act", lambda: nc.scalar.copy(out=hT[:], in_=tp[0][:]), r=[tp[0]], w=[hT])
            def proj_chunk(n_i, n):
                m = mm[n_i % 2]
                for kc in range(8):
                    k.op("pe", lambda m=m, kc=kc, n=n: nc.tensor.matmul(
                        m[:], lhsT=hT[:, kc, :], rhs=Win[:, kc, n * 512:(n + 1) * 512],
                        start=(kc == 0), stop=(kc == 7)), r=[hT, Win], w=[m], inc=(kc == 7))
                if n < 2:
                    k.op("act", lambda m=m, n=n: nc.scalar.copy(out=qf[:, n * 512:(n + 1) * 512], in_=m[:]),
                         r=[m], w=[qf])
                elif n < 4:
                    k.op("act", lambda m=m, n=n: nc.scalar.copy(out=kf[:, (n - 2) * 512:(n - 1) * 512], in_=m[:]),
                         r=[m], w=[kf])
                elif n < 8:
                    k.op("act", lambda m=m, n=n: nc.scalar.copy(out=vb[:, (n - 4) * 512:(n - 3) * 512], in_=m[:]),
                         r=[m], w=[vb])
                else:
                    k.op("act", lambda m=m, n=n: nc.scalar.activation(
                        out=sgt[:, (n - 8) * 512:(n - 7) * 512], in_=m[:], func=AF.Silu), r=[m], w=[sgt])
            for n_i, n in enumerate([4, 5, 6, 7, 8, 9, 10, 11, 0, 1]):
                proj_chunk(n_i, n)
            rotary(qf, qr, cs)
            k.op("dve", lambda: V.tensor_tensor(qb[:].rearrange("p (h d) -> p h d", h=8),
                                                qr[:].rearrange("p (h d) -> p h d", h=8),
                                                xi[:].unsqueeze(2).to_broadcast([128, 8, 128]), ALU.mult),
                 r=[qr, xi], w=[qb])
            for n_i, n in enumerate([2, 3]):
                proj_chunk(n_i, n)
            rotary(kf, kr, cs)
            k.op("dve", lambda: V.tensor_scalar(kb[:], kr[:], 128.0 ** -0.5, None, ALU.mult), r=[kr], w=[kb])
            k.op("dve", lambda: V.tensor_tensor(kz[:].rearrange("p (h d) -> p h d", h=8),
                                                kr[:].rearrange("p (h d) -> p h d", h=8),
                                                zs[:].unsqueeze(2).to_broadcast([128, 8, 128]), ALU.mult),
                 r=[kr, zs], w=[kz])
            for h in range(8):
                k.op("pe", lambda h=h: nc.tensor.transpose(tp[0][:, h, :], qb[:, h * 128:(h + 1) * 128], ident[:]),
                     r=[qb, ident], w=[tp[0]])
            k.op("act", lambda: nc.scalar.copy(out=qT[:], in_=tp[0][:]), r=[tp[0]], w=[qT])
            for h in range(8):
                k.op("pe", lambda h=h: nc.tensor.transpose(tp[1][:, h, :], kb[:, h * 128:(h + 1) * 128], ident[:]),
                     r=[kb, ident], w=[tp[1]])
            k.op("act", lambda: nc.scalar.copy(out=kT[:], in_=tp[1][:]), r=[tp[1]], w=[kT])

        def recur(c_):
            kz, qT, kT, vb, sgt, gout = kzs[c_], qTs[c_], kTs[c_], vbs[c_], sgts[c_], gouts[c_]
            for h in range(8):
                u = h % 2
                vs = slice(h * 256, (h + 1) * 256)
                k.op("pe", lambda h=h, u=u: nc.tensor.matmul(recA[u][:, 0:128], lhsT=kT[:, h, :], rhs=qT[:, h, :],
                                                            start=True, stop=True), r=[kT, qT], w=[recA[u]])
                k.op("dve", lambda h=h, u=u: V.tensor_tensor(Pm[u][:], recA[u][:, 0:128], dmk[:, h, :], ALU.mult),
                     r=[recA[u], dmk], w=[Pm[u]])
                k.op("pe", lambda h=h, u=u, vs=vs: nc.tensor.matmul(recB[u][:, 0:256], lhsT=Pm[u][:], rhs=vb[:, vs],
                                                                   start=True, stop=False), r=[Pm[u], vb], w=[recB[u]])
                k.op("pe", lambda h=h, u=u: nc.tensor.matmul(recB[u][:, 0:256], lhsT=qT[:, h, :], rhs=sbf[h][:],
                                                            start=False, stop=True), r=[qT, sbf[h]], w=[recB[u]])
                k.op("pe", lambda h=h, u=u, vs=vs: nc.tensor.matmul(recA[u][:, 128:384],
                                                                   lhsT=kz[:, h * 128:(h + 1) * 128], rhs=vb[:, vs],
                                                                   start=True, stop=True), r=[kz, vb], w=[recA[u]])
                k.op("dve", lambda h=h, u=u: V.scalar_tensor_tensor(
                    state[h][:], state[h][:], GAMMA[h] ** 128, recA[u][:, 128:384], ALU.mult, ALU.add),
                    r=[state[h], recA[u]], w=[state[h]])
                k.op("act", lambda h=h: nc.scalar.copy(out=sbf[h][:], in_=state[h][:]), r=[state[h]], w=[sbf[h]])
                k.op("dve", lambda u=u: V.bn_stats(st[u][:], recB[u][:, 0:256]), r=[recB[u]], w=[st[u]])
                k.op("dve", lambda u=u: V.bn_aggr(mv[u][:], st[u][:]), r=[st[u]], w=[mv[u]])
                k.op("act", lambda u=u: nc.scalar.activation(out=rstd[u][:], in_=mv[u][:, 1:2], func=AF.Sqrt,
                                                             bias=P.eps_t[:, 0:1]), r=[mv[u], P.eps_t], w=[rstd[u]])
                k.op("dve", lambda u=u: V.reciprocal(rstd[u][:], rstd[u][:]), r=[rstd[u]], w=[rstd[u]])
                k.op("dve", lambda u=u: V.tensor_scalar(on[u][:], recB[u][:, 0:256], mv[u][:, 0:1], rstd[u][:, 0:1],
                                                        ALU.subtract, ALU.mult), r=[recB[u], mv[u], rstd[u]], w=[on[u]])
                k.op("dve", lambda u=u, vs=vs: V.tensor_tensor(on[u][:], on[u][:], gn_bc[:, vs], ALU.mult),
                     r=[on[u], gn_bc], w=[on[u]])
                k.op("dve", lambda u=u, vs=vs: V.tensor_tensor(gout[:, vs], on[u][:], sgt[:, vs], ALU.mult),
                     r=[on[u], sgt], w=[gout])
            k.scatter(G, gout[:], idx[:, c_:c_ + 1], r=[gout, idx])

        def bump_col(c_):
            k.op("dve", lambda: V.tensor_scalar(idx[:, c_:c_ + 1], idx[:, c_:c_ + 1], 256.0, None, ALU.add),
                 r=[idx], w=[idx])

        inproj(0)

        def body(i):
            k.replay(k.record(lambda: inproj(1)), k.record(lambda: recur(0)))
            bump_col(0)
            k.replay(k.record(lambda: inproj(0)), k.record(lambda: recur(1)))
            bump_col(1)
        k.loop(NT // 2, body)
    k.barrier()


def setup_globals(P):
    nc, k = P.nc, P.k
    def gt(name, shape, dt):
        t = Tile(name, nc.alloc_sbuf_tensor(name, list(shape), dt))
        k.tiles.append(t)
        return t
    P.one_t = gt("one_t", [128, 1], F32)
    P.eps_t = gt("eps_t", [128, 1], F32)
    P.ones16 = gt("ones16", [128, 16], F32)
    k.op("dve", lambda: nc.vector.memset(P.one_t[:], 1.0), w=[P.one_t])
    k.op("dve", lambda: nc.vector.memset(P.eps_t[:], LN_EPS), w=[P.eps_t])
    k.dma("sp", P.ones16[:], P.C["ones16"], w=[P.ones16])
    k.barrier()


def build_program(S, n_layers=DEPTH, dbg=False):
    P = Prog(S, dbg)
    setup_globals(P)
    XR = P.scratch("xr", [S + 128, D], F32)
    phase_adaln(P)
    if n_layers > 1:
        phase_rope_table(P)
    src = P.x_in
    for l in range(n_layers):
        last = (l == n_layers - 1)
        li = l // 2
        if l % 2 == 0:
            phase_sb_inproj(P, li, src)
            phase_sb_attn(P)
            phase_post(P, P.scratch("sbC", [S, 1024], BF16), 8, True, P.W["sb_w_out"][li], 2 * l,
                       P.W["ln_g"][l, 0:1, :], P.W["ln_b"][l, 0:1, :], src, XR)
        else:
            phase_ret(P, li, src)
            phase_post(P, P.scratch("retG", [S, 2048], BF16), 16, False, P.W["ret_w_out"][li], 2 * l,
                       P.W["ln_g"][l, 0:1, :], P.W["ln_b"][l, 0:1, :], src, XR)
        src = XR
        phase_moe(P, l, XR, P.out if last else XR)
    P.k.barrier()
    return P


_CACHE = {}


def kernel(**inputs):
    S = inputs["x"].shape[1]
    B = inputs["x"].shape[0]
    if S not in _CACHE:
        _CACHE[S] = build_program(S)
    P = _CACHE[S]
    consts = make_consts()
    in_maps = []
    for b in range(B):
        m = {"x": np.ascontiguousarray(inputs["x"][b], dtype=np.float32),
             "cT": np.ascontiguousarray(np.asarray(inputs["c"][b], dtype=np.float32).reshape(8, 128).T),
             "posT": np.ascontiguousarray(np.asarray(inputs["positions"][b], dtype=np.int32).reshape(S // 128, 128).T)}
        for n in WSHAPES:
            m[n] = np.ascontiguousarray(inputs[n], dtype=np.float32)
        for n, v in consts.items():
            m["c_" + n] = v
        in_maps.append(m)
    res = run_bass_kernel_spmd(P.nc, in_maps, core_ids=list(range(B)))
    return np.stack([np.asarray(r["out"], dtype=np.float32) for r in res.results], axis=0)
```

```python
import numpy as np
import ml_dtypes
from contextlib import ExitStack
import concourse.bass as bass
import concourse.mybir as mybir
from concourse.bass_utils import run_bass_kernel_spmd

F32 = mybir.dt.float32
BF16 = mybir.dt.bfloat16
I32 = mybir.dt.int32
AF = mybir.ActivationFunctionType
ALU = mybir.AluOpType
AX = mybir.AxisListType

D = 1024
DEPTH = 4
ALPHA = (2 * DEPTH) ** 0.25
LN_EPS = 1e-5


class Aff:
    __slots__ = ("c", "k")

    def __init__(self, c=0, k=()):
        self.c = c
        self.k = tuple(k)

    def add(self, n):
        return Aff(self.c + n, self.k)

    def le(self, o):
        return self.k == o.k and self.c <= o.c


class Tile:
    def __init__(self, name, t):
        self.name = name
        self.t = t
        self.wr = None
        self.rd = {}

    def __getitem__(self, idx):
        return self.t[idx]


class K:
    ENG = ("pe", "act", "dve", "pool", "sp")

    def __init__(self, nc):
        self.nc = nc
        self.E = {"pe": nc.tensor, "act": nc.scalar, "dve": nc.vector,
                  "pool": nc.gpsimd, "sp": nc.sync}
        self.sems = {}
        self.cur = {}
        self.known = {e: {} for e in self.ENG}
        self.dirty = set()
        self.tiles = []
        self.dry = 0
        self.loops = []
        self.nloop = 0
        self.tregs = {}
        for e in ("pe", "act", "dve", "pool"):
            self._sem("e_" + e)

    def _sem(self, key):
        if key not in self.sems:
            self.sems[key] = self.nc.alloc_semaphore(key)
            self.cur[key] = Aff(0)
        return self.sems[key]

    def _treg(self, eng):
        if eng not in self.tregs:
            self.tregs[eng] = self.E[eng].alloc_register("kw_" + eng)
        return self.tregs[eng]

    def _val(self, a):
        v = a.c
        for lid, coef in a.k:
            var = [x for (l, x) in self.loops if l == lid][0]
            v = var * coef + v
        return v

    def _wait(self, eng, evs):
        for key, a in evs:
            if eng == "pe" and key == "e_pe":
                continue
            kn = self.known[eng].get(key)
            if kn is not None and a.le(kn):
                continue
            self.known[eng][key] = a
            if not self.dry:
                if a.k:
                    assert len(a.k) == 1
                    lid, coef = a.k[0]
                    var = [x for (l, x) in self.loops if l == lid][0]
                    T = self._treg(eng)
                    self.E[eng].reg_mul(T, var, coef)
                    self.E[eng].reg_add(T, T, a.c)
                    self.E[eng].wait_ge(self.sems[key], T)
                else:
                    self.E[eng].wait_ge(self.sems[key], a.c)

    def _deps(self, r, w):
        evs = []
        for t in r:
            if t.wr is not None:
                evs.append(t.wr)
        for t in w:
            if t.wr is not None:
                evs.append(t.wr)
            evs.extend(t.rd.items())
        return evs

    def _mark(self, ev, r, w):
        key, a = ev
        for t in r:
            t.rd[key] = a
        for t in w:
            t.wr = ev
            t.rd = {}

    def tile(self, es, name, shape, dt):
        self.uid = getattr(self, "uid", 0) + 1
        t = Tile(name, es.enter_context(self.nc.sbuf_tensor(f"{name}_{self.uid}", list(shape), dt)))
        self.tiles.append(t)
        return t

    def ptile(self, es, name, shape, dt=F32):
        self.uid = getattr(self, "uid", 0) + 1
        t = Tile(name, es.enter_context(self.nc.psum_tensor(f"{name}_{self.uid}", list(shape), dt)))
        self.tiles.append(t)
        return t

    def vtile(self, name):
        t = Tile(name, None)
        self.tiles.append(t)
        return t

    def op(self, eng, fn, r=(), w=(), inc=True):
        self._wait(eng, self._deps(r, w))
        if not inc:
            if not self.dry:
                fn()
            return
        key = "e_" + eng
        self.cur[key] = self.cur[key].add(1)
        self.dirty.add(key)
        if not self.dry:
            fn().then_inc(self.sems[key], 1)
        self._mark((key, self.cur[key]), r, w)

    def dma(self, q, out, in_, r=(), w=(), key=None):
        if key is None:
            key = "d_" + (w[0].name if w else r[0].name)
        self._sem(key)
        self._wait(q, self._deps(r, w))
        self.cur[key] = self.cur[key].add(16)
        self.dirty.add(key)
        if not self.dry:
            o = out() if callable(out) else out
            i = in_() if callable(in_) else in_
            self.E[q].dma_start(out=o, in_=i).then_inc(self.sems[key], 16)
        self._mark((key, self.cur[key]), r, w)

    def gather(self, out, src, idx, r=(), w=(), key=None):
        if key is None:
            key = "d_" + w[0].name
        self._sem(key)
        self._wait("pool", self._deps(r, w))
        self.cur[key] = self.cur[key].add(16)
        self.dirty.add(key)
        if not self.dry:
            self.nc.gpsimd.indirect_dma_start(
                out=out, out_offset=None, in_=src,
                in_offset=bass.IndirectOffsetOnAxis(ap=idx, axis=0)).then_inc(self.sems[key], 16)
        self._mark((key, self.cur[key]), r, w)

    def scatter(self, dst, in_, idx, r=(), w=(), key=None):
        if key is None:
            key = "d_" + r[0].name
        self._sem(key)
        self._wait("pool", self._deps(r, w))
        self.cur[key] = self.cur[key].add(16)
        self.dirty.add(key)
        if not self.dry:
            self.nc.gpsimd.indirect_dma_start(
                out=dst, out_offset=bass.IndirectOffsetOnAxis(ap=idx, axis=0), in_=in_,
                in_offset=None).then_inc(self.sems[key], 16)
        self._mark((key, self.cur[key]), r, w)

    def record(self, fn):
        rec = []
        names = ("op", "dma", "gather", "scatter")
        for nm in names:
            setattr(self, nm, (lambda nm: (lambda *a, **kw: rec.append((nm, a, kw))))(nm))
        try:
            fn()
        finally:
            for nm in names:
                delattr(self, nm)
        return rec

    def replay(self, *recs):
        pos = [0] * len(recs)
        total = sum(len(r) for r in recs)
        for _ in range(total):
            best = min((pos[i] / len(r), i) for i, r in enumerate(recs) if pos[i] < len(r))[1]
            nm, a, kw = recs[best][pos[best]]
            pos[best] += 1
            getattr(K, nm)(self, *a, **kw)

    def _clean(self):
        for t in self.tiles:
            t.wr = None
            t.rd = {}

    def barrier(self, keys=None):
        keys = sorted(self.dirty) if keys is None else sorted(keys)
        for e in self.ENG:
            for key in keys:
                self._wait(e, [(key, self.cur[key])])
        self.dirty -= set(keys)
        self._clean()

    def _release(self):
        for e in ("pe", "act", "dve", "pool"):
            h = self._sem("r_" + e)
            self.nc.sync.sem_inc(h, 1)
            self.E[e].wait_ge(h, 1)
            self.E[e].sem_clear(h)

    def _reset(self):
        for key in sorted(self.cur):
            if key.startswith("r_"):
                continue
            if self.cur[key].c:
                self.nc.sync.wait_ge(self.sems[key], self.cur[key].c)
                self.nc.sync.sem_clear(self.sems[key])
                self.cur[key] = Aff(0)
        self.dirty = set()
        self.known = {e: {} for e in self.ENG}
        self._clean()
        self._release()

    def loop_reset(self, n, body):
        assert n >= 1
        self.barrier()
        self._reset()
        with self.nc.Fori(0, n) as i:
            body(i)
            self._reset()


    def loop(self, n, body):
        assert n >= 1
        self.barrier()
        save = dict(self.cur)
        sk = {e: dict(v) for e, v in self.known.items()}
        self.dry += 1
        body(0)
        self.dry -= 1
        per = {}
        for key, a in self.cur.items():
            d = a.c - save[key].c if key in save else a.c
            if d:
                per[key] = d
        for key in list(self.cur):
            if key not in save:
                save[key] = Aff(0)
        self.cur = dict(save)
        self.known = sk
        self._clean()
        lid = self.nloop
        self.nloop += 1
        for key, d in sorted(per.items()):
            self.cur[key] = self.cur[key].add(d)
            if not self.dry:
                self.nc.sync.sem_inc(self.sems[key], d)
        for e in self.ENG:
            for key in sorted(per):
                self._wait(e, [(key, self.cur[key])])
        base = dict(self.cur)
        kn_entry = {e: dict(v) for e, v in self.known.items()}

        def set_iter(off):
            for key, d in per.items():
                b = base[key]
                self.cur[key] = Aff(b.c + off * d, b.k + ((lid, d),))

        if self.dry:
            for key, d in per.items():
                self.cur[key] = base[key].add(n * d)
            self.dirty |= set(per)
            self.barrier()
            return
        self.dry += 1
        set_iter(-1)
        self.known = {e: {} for e in self.ENG}
        body(0)
        self.dry -= 1
        with self.nc.Fori(0, n) as i:
            self.loops.append((lid, i))
            set_iter(0)
            self.known = {e: {} for e in self.ENG}
            body(i)
            self.loops.pop()
        for key, d in per.items():
            self.cur[key] = base[key].add(n * d)
        self.known = kn_entry
        self.dirty |= set(per)
        self.barrier()


GAMMA = [1.0 - 2.0 ** (-5.0 - h) for h in range(8)]


def make_consts():
    c = {}
    c["ident_bf"] = np.eye(128, dtype=np.float32).astype(ml_dtypes.bfloat16)
    c["ident_f"] = np.eye(128, dtype=np.float32)
    sp_, s_ = np.meshgrid(np.arange(128), np.arange(128), indexing="ij")
    c["tri"] = (sp_ > s_).astype(np.float32).astype(ml_dtypes.bfloat16)
    c["compl"] = (sp_ <= s_).astype(np.float32).astype(ml_dtypes.bfloat16)
    c["ntinc"] = (-(sp_ >= s_).astype(np.float32)).astype(ml_dtypes.bfloat16)
    c["nones"] = (-np.ones((128, 128), np.float32)).astype(ml_dtypes.bfloat16)
    m = np.zeros((128, 4, 512), np.float32)
    for r in range(4):
        s, t = np.meshgrid(np.arange(128), np.arange(512), indexing="ij")
        m[:, r, :] = (s + r * 128 < t)
    c["sbmask"] = m.astype(ml_dtypes.bfloat16)
    n = np.arange(128, dtype=np.float64)
    dm = np.zeros((128, 8, 128), np.float64)
    xi = np.zeros((128, 8), np.float64)
    zs = np.zeros((128, 8), np.float64)
    for h in range(8):
        g = GAMMA[h]
        dm[:, h, :] = np.where(n[None, :] >= n[:, None], g ** (-(n[:, None] + 1.0)), 0.0)
        xi[:, h] = g ** (n + 1.0)
        zs[:, h] = g ** (127.0 - n) * (128.0 ** -0.5)
    c["dmaskT"] = dm.astype(np.float32)
    c["xi"] = xi.astype(np.float32)
    c["zetas"] = zs.astype(np.float32)
    inv_freq = (1.0 / (10000.0 ** (np.arange(0, 128, 2, dtype=np.float32) / 128))).astype(np.float32)
    c["invfreq"] = np.tile(inv_freq[None, :], (128, 1)).astype(np.float32)
    c["iota"] = np.arange(128, dtype=np.int32).reshape(128, 1)
    c["ones16"] = np.ones((128, 16), np.float32)
    return c


CONST_DT = {"ident_bf": BF16, "ident_f": F32, "tri": BF16, "compl": BF16, "ntinc": BF16, "nones": BF16, "sbmask": BF16,
            "dmaskT": F32, "xi": F32, "zetas": F32, "invfreq": F32, "iota": I32, "ones16": F32}

WSHAPES = {
    "ada_w": [4, 2, 1024, 3072], "ada_b": [4, 2, 3072], "ln_g": [4, 2, 1024], "ln_b": [4, 2, 1024],
    "sb_w_in": [2, 1024, 3072], "sb_w_out": [2, 1024, 1024], "ret_w_in": [2, 1024, 6144],
    "ret_gn_g": [2, 2048], "ret_w_out": [2, 2048, 1024], "router_w": [1024, 16], "router_b": [16],
    "moe_w_gate": [4, 16, 1024, 512], "moe_w_up": [4, 16, 1024, 512], "moe_w_down": [4, 16, 512, 1024],
}


class Prog:
    def __init__(self, S, dbg=False):
        self.S = S
        self.NT = S // 128
        self.nc = nc = bass.Bass("TRN2", target_bir_lowering=False)
        self.k = K(nc)
        self.dbg = dbg
        inp = lambda name, shape, dt=F32: nc.dram_tensor(name, list(shape), dt, kind="ExternalInput").ap()
        self.x_in = inp("x", [S, D])
        self.cT = inp("cT", [128, 8])
        self.posT = inp("posT", [128, self.NT], I32)
        self.W = {n: inp(n, s) for n, s in WSHAPES.items()}
        cs = make_consts()
        self.C = {n: inp("c_" + n, cs[n].shape, CONST_DT[n]) for n in cs}
        self.out = nc.dram_tensor("out", [S, D], F32, kind="ExternalOutput").ap()
        self.scr = {}
        self.moe_ne = 16
        self.moe_lvl = 3
        self.warm_n = 0
        self.moe_ns = 8 if S >= 2048 else 4

    def scratch(self, name, shape, dt):
        if name not in self.scr:
            kind = "ExternalOutput" if self.dbg else "Internal"
            self.scr[name] = self.nc.dram_tensor("s_" + name, list(shape), dt, kind=kind).ap()
        return self.scr[name]


def bc_load(P, es, name, src_row):
    t = P.k.tile(es, name, [128, src_row.shape[-1]], F32)
    P.k.dma("sp", t[:], src_row.partition_broadcast(128), w=[t])
    return t


def phase_adaln(P):
    nc, k = P.nc, P.k
    MOD = P.scratch("mod", [8, 3072], F32)
    with ExitStack() as es:
        ct = k.tile(es, "ct", [128, 8], F32)
        sc = k.tile(es, "sc", [128, 8], F32)
        Wt = k.tile(es, "adaW", [128, 8, 3072], F32)
        bias = k.tile(es, "adab", [1, 3072], F32)
        res = k.tile(es, "adar", [1, 3072], F32)
        ps = [k.ptile(es, f"adaps{i}", [1, 512]) for i in range(2)]
        k.dma("sp", ct[:], P.cT, w=[ct])
        k.op("act", lambda: nc.scalar.activation(out=sc[:], in_=ct[:], func=AF.Silu), r=[ct], w=[sc])
        for j in range(8):
            l, s = divmod(j, 2)
            k.dma("sp", Wt[:], P.W["ada_w"][l, s].rearrange("(k p) n -> p k n", p=128), w=[Wt])
            k.dma("sp", bias[:], P.W["ada_b"][l, s:s + 1, :], w=[bias])
            for n in range(6):
                p = ps[n % 2]
                for kc in range(8):
                    k.op("pe", lambda p=p, kc=kc, n=n: nc.tensor.matmul(
                        p[0:1, :], lhsT=sc[:, kc:kc + 1], rhs=Wt[:, kc, n * 512:(n + 1) * 512],
                        start=(kc == 0), stop=(kc == 7)), r=[sc, Wt], w=[p], inc=(kc == 7))
                k.op("dve", lambda p=p, n=n: nc.vector.tensor_tensor(
                    res[0:1, n * 512:(n + 1) * 512], p[0:1, :], bias[0:1, n * 512:(n + 1) * 512], ALU.add),
                    r=[p, bias], w=[res])
            k.op("dve", lambda: nc.vector.tensor_scalar_add(res[0:1, 1024:3072], res[0:1, 1024:3072], 1.0),
                 r=[res], w=[res])
            k.dma("sp", MOD[j:j + 1, :], res[0:1, :], r=[res])
    k.barrier()


def make_idx(P, es, name, bases):
    nc, k = P.nc, P.k
    n = len(bases)
    io = k.tile(es, name + "_io", [128, 1], I32)
    k.dma("sp", io[:], P.C["iota"], w=[io])
    t = k.tile(es, name, [128, n], I32)
    for j, b in enumerate(bases):
        k.op("dve", lambda j=j, b=b: nc.vector.tensor_scalar(t[:, j:j + 1], io[:], float(b), None, ALU.add),
             r=[io], w=[t])
    return t


def bump_idx(P, t, step):
    P.k.op("dve", lambda: P.nc.vector.tensor_scalar(t[:], t[:], float(step), None, ALU.add), r=[t], w=[t])


def modulate(P, xt, scale_bc, shift_bc, out_t):
    nc, k = P.nc, P.k
    k.op("dve", lambda: nc.vector.tensor_tensor(xt[:], xt[:], scale_bc[:], ALU.mult), r=[xt, scale_bc], w=[xt])
    k.op("dve", lambda: nc.vector.tensor_tensor(out_t[:], xt[:], shift_bc[:], ALU.add),
         r=[xt, shift_bc], w=[out_t])


def phase_sb_inproj(P, li, x_src):
    nc, k, S = P.nc, P.k, P.S
    NTB = S // 512
    MOD = P.scratch("mod", [8, 3072], F32)
    A = P.scratch("sbA", [NTB * 128, 24 * 512], BF16)
    B = P.scratch("sbB", [24 * 128, S], BF16)
    j = (2 * li) * 2 + 0
    with ExitStack() as es:
        Win = k.tile(es, "Win", [128, 8, 3072], BF16)
        k.dma("pool", Win[:], P.W["sb_w_in"][li].rearrange("(k p) n -> p k n", p=128), w=[Win])
        ident = k.tile(es, "ident", [128, 128], BF16)
        k.dma("sp", ident[:], P.C["ident_bf"], w=[ident])
        shift_bc = bc_load(P, es, "shift_bc", MOD[j:j + 1, 0:1024])
        scale_bc = bc_load(P, es, "scale_bc", MOD[j:j + 1, 1024:2048])
        idx = make_idx(P, es, "idx", [sub * 128 for sub in range(4)] + [0])
        xt = [k.tile(es, f"xt{u}", [128, 1024], F32) for u in range(2)]
        hb = [k.tile(es, f"hb{u}", [128, 1024], BF16) for u in range(2)]
        hT = k.tile(es, "hT", [128, 8, 512], BF16)
        tp = [k.ptile(es, f"tp{u}", [128, 8, 128], BF16) for u in range(2)]
        mm = [k.ptile(es, f"mm{u}", [128, 512]) for u in range(4)]
        obig = k.tile(es, "obig", [128, 24, 512], BF16)

        def body(tb):
            for sub in range(4):
                u = sub % 2
                k.gather(xt[u][:], x_src, idx[:, sub:sub + 1], r=[idx], w=[xt[u]])
                modulate(P, xt[u], scale_bc, shift_bc, hb[u])
                for kc in range(8):
                    k.op("pe", lambda u=u, kc=kc: nc.tensor.transpose(
                        tp[u][:, kc, :], hb[u][:, kc * 128:(kc + 1) * 128], ident[:]),
                        r=[hb[u], ident], w=[tp[u]])
                k.op("act", lambda u=u, sub=sub: nc.scalar.copy(
                    out=hT[:, :, sub * 128:(sub + 1) * 128], in_=tp[u][:]), r=[tp[u]], w=[hT])
            for oc in range(24):
                m = oc % 4
                for kc in range(8):
                    k.op("pe", lambda m=m, kc=kc, oc=oc: nc.tensor.matmul(
                        mm[m][:], lhsT=Win[:, kc, oc * 128:(oc + 1) * 128], rhs=hT[:, kc, :],
                        start=(kc == 0), stop=(kc == 7)), r=[Win, hT], w=[mm[m]], inc=(kc == 7))
                sc_ = 0.125 if oc < 8 else 1.0
                if oc % 2 == 0:
                    k.op("act", lambda m=m, oc=oc, sc_=sc_: nc.scalar.activation(
                        out=obig[:, oc, :], in_=mm[m][:], func=AF.Copy, scale=sc_), r=[mm[m]], w=[obig])
                else:
                    k.op("dve", lambda m=m, oc=oc, sc_=sc_: nc.vector.tensor_scalar(
                        obig[:, oc, :], mm[m][:], sc_, None, ALU.mult), r=[mm[m]], w=[obig])
            k.scatter(A, obig[:].rearrange("p a b -> p (a b)"), idx[:, 4:5], r=[obig, idx])
            k.op("dve", lambda: nc.vector.tensor_scalar(idx[:, 0:4], idx[:, 0:4], 512.0, None, ALU.add),
                 r=[idx], w=[idx])
            k.op("dve", lambda: nc.vector.tensor_scalar(idx[:, 4:5], idx[:, 4:5], 128.0, None, ALU.add),
                 r=[idx], w=[idx])
        k.loop(NTB, body)
        Av = A.rearrange("(tb p) (oc t) -> oc p tb t", p=128, t=512)
        Bv = B.rearrange("(oc p) (tb t) -> oc p tb t", p=128, t=512)
        for oc in range(24):
            k.dma("sp" if oc % 2 == 0 else "act", Bv[oc], Av[oc], key=f"d_rl{oc % 8}")
    k.barrier()


def phase_sb_attn(P):
    nc, k, S = P.nc, P.k, P.S
    B = P.scratch("sbB", [24 * 128, S], BF16)
    OTB = P.scratch("sbOT", [1024, S], BF16)
    Cc = P.scratch("sbC", [S, 1024], BF16)
    NT = S // 128
    NC = S // 512
    with ExitStack() as es:
        tri = k.tile(es, "tri", [128, 128], BF16)
        cpl = k.tile(es, "cpl", [128, 128], BF16)
        msk = k.tile(es, "msk", [128, 4, 512], BF16)
        ident = k.tile(es, "ident", [128, 128], BF16)
        k.dma("sp", tri[:], P.C["ntinc"], w=[tri])
        k.dma("sp", cpl[:], P.C["nones"], w=[cpl])
        k.dma("sp", msk[:], P.C["sbmask"], w=[msk])
        k.dma("sp", ident[:], P.C["ident_bf"], w=[ident])
        idx = make_idx(P, es, "idx", [0, 1024, 2048, 0])
        qT = k.tile(es, "qT", [128, S], BF16)
        kT = k.tile(es, "kT", [128, S], BF16)
        vT = k.tile(es, "vT", [128, S], BF16)
        vp = k.tile(es, "vp", [128, NT, 128], BF16)
        osb = k.tile(es, "osb", [128, S], BF16)
        Z = [[k.ptile(es, f"Z{a}{u}", [128, 512]) for u in range(3)] for a in range(2)]
        SPACC = [k.tile(es, f"SPACC{a}", [128, 512], BF16) for a in range(2)]
        OTp = [k.ptile(es, f"OTp{a}", [128, 512]) for a in range(2)]
        Et = [[k.tile(es, f"E{a}{u}", [128, 512], F32) for u in range(3)] for a in range(2)]
        SPt = [[k.tile(es, f"SP{a}{u}", [128, 512], BF16) for u in range(3)] for a in range(2)]
        At = [[k.tile(es, f"A{a}{u}", [128, 512], BF16) for u in range(3)] for a in range(2)]

        def stageA(g):
            c, j, r, u, first, last = g
            qs = slice(c * 512, (c + 1) * 512)
            for a in range(2):
                pa = slice(a * 64, (a + 1) * 64)
                k.op("pe", lambda a=a, pa=pa: nc.tensor.matmul(
                    Z[a][u][:], lhsT=kT[pa, j * 128:(j + 1) * 128], rhs=qT[pa, qs],
                    start=True, stop=False, skip_group_check=True), r=[kT, qT], w=[Z[a][u]])
            for a in range(2):
                k.op("act", lambda a=a: nc.scalar.activation(
                    out=Et[a][u][:], in_=Z[a][u][:], func=AF.Exp), r=[Z[a][u]], w=[Et[a][u]])
            for a in range(2):
                k.op("act", lambda a=a: nc.scalar.activation(
                    out=SPt[a][u][:], in_=Et[a][u][:], func=AF.Ln, bias=P.one_t[:, 0:1]),
                    r=[Et[a][u], P.one_t], w=[SPt[a][u]])
                if r is not None:
                    k.op("dve", lambda a=a: nc.vector.tensor_tensor(
                        SPt[a][u][:], SPt[a][u][:], msk[:, r, :], ALU.mult), r=[SPt[a][u], msk], w=[SPt[a][u]])

        def stageB(g):
            c, j, r, u, first, last = g
            for a in range(2):
                k.op("pe", lambda a=a: nc.tensor.matmul(
                    Z[a][u][:], lhsT=tri[:], rhs=SPt[a][u][:], start=False, stop=first, skip_group_check=True),
                    r=[tri, SPt[a][u]], w=[Z[a][u]])
                if not first:
                    k.op("pe", lambda a=a: nc.tensor.matmul(
                        Z[a][u][:], lhsT=cpl[:], rhs=SPACC[a][:], start=False, stop=True, skip_group_check=True),
                        r=[cpl, SPACC[a]], w=[Z[a][u]])
            if not last:
                for a in range(2):
                    if first:
                        k.op("pool", lambda a=a: nc.gpsimd.tensor_copy(SPACC[a][:], SPt[a][u][:]),
                             r=[SPt[a][u]], w=[SPACC[a]])
                    else:
                        k.op("pool", lambda a=a: nc.gpsimd.tensor_tensor(
                            SPACC[a][:], SPACC[a][:], SPt[a][u][:], ALU.add), r=[SPACC[a], SPt[a][u]], w=[SPACC[a]])
            for a in range(2):
                k.op("act", lambda a=a: nc.scalar.activation(
                    out=At[a][u][:], in_=Z[a][u][:], func=AF.Exp), r=[Z[a][u]], w=[At[a][u]])
                if r is not None:
                    k.op("dve", lambda a=a: nc.vector.tensor_tensor(
                        At[a][u][:], At[a][u][:], msk[:, r, :], ALU.mult), r=[At[a][u], msk], w=[At[a][u]])
            for a in range(2):
                k.op("pe", lambda a=a: nc.tensor.matmul(
                    OTp[a][:], lhsT=vp[:, j, :], rhs=At[a][u][:], start=first, stop=True, skip_group_check=True),
                    r=[vp, At[a][u]], w=[OTp[a]])

        def evac(c):
            k.op("act", lambda: nc.scalar.copy(out=osb[0:64, c * 512:(c + 1) * 512], in_=OTp[0][0:64, :]),
                 r=[OTp[0]], w=[osb])
            k.op("dve", lambda: nc.vector.tensor_copy(osb[64:128, c * 512:(c + 1) * 512], OTp[1][64:128, :]),
                 r=[OTp[1]], w=[osb])

        def pair_body(hp):
            k.gather(qT[:], B, idx[:, 0:1], r=[idx], w=[qT])
            k.gather(kT[:], B, idx[:, 1:2], r=[idx], w=[kT])
            k.gather(vT[:], B, idx[:, 2:3], r=[idx], w=[vT])
            for g in range(NT // 8):
                a = g % 2
                tpv = Z[a][0][:].bitcast(BF16).rearrange("p (j d) -> p j d", d=128)
                for jj in range(8):
                    j = g * 8 + jj
                    k.op("pe", lambda tpv=tpv, jj=jj, j=j: nc.tensor.transpose(
                        tpv[:, jj, :], vT[:, j * 128:(j + 1) * 128], ident[:]), r=[vT, ident], w=[Z[a][0]])
                k.op("act", lambda tpv=tpv, g=g: nc.scalar.copy(out=vp[:, g * 8:(g + 1) * 8, :], in_=tpv),
                     r=[Z[a][0]], w=[vp])
            groups = []
            for c in range(NC):
                js = [(4 * c + 3, 3), (4 * c + 2, 2), (4 * c + 1, 1), (4 * c, 0)] + \
                     [(j, None) for j in range(4 * c - 1, -1, -1)]
                for t, (j, r) in enumerate(js):
                    groups.append((c, j, r, len(groups) % 3, t == 0, t == len(js) - 1))
            for w_ in range(P.warm_n):
                k.op("pe", lambda: nc.tensor.matmul(Z[0][2][:], lhsT=tri[:], rhs=msk[:, 0, :], start=True, stop=True,
                                                    skip_group_check=True), r=[tri, msk], w=[Z[0][2]],
                     inc=(w_ == P.warm_n - 1))
            stageA(groups[0])
            stageA(groups[1])
            for n, g in enumerate(groups):
                if n + 2 < len(groups):
                    stageA(groups[n + 2])
                stageB(g)
                if g[5]:
                    evac(g[0])
            k.scatter(OTB, osb[:], idx[:, 3:4], r=[osb, idx])
            bump_idx(P, idx, 128)
        k.loop(8, pair_body)
        Ov = OTB.rearrange("(kc p) (i t) -> kc p i t", p=128, t=128)
        Cv = Cc.rearrange("(i p) (kc t) -> kc p i t", p=128, t=128)
        for kc in range(8):
            k.dma("sp" if kc % 2 == 0 else "act", Cv[kc], Ov[kc], key=f"d_rl{kc}")
    k.barrier()


def layer_norm_tile(P, r, st, mv, rstd, g_bc, b_bc, out_t):
    nc, k = P.nc, P.k
    for hh in range(2):
        k.op("dve", lambda hh=hh: nc.vector.bn_stats(st[:, hh, :], r[:, hh * 512:(hh + 1) * 512]),
             r=[r], w=[st])
    k.op("dve", lambda: nc.vector.bn_aggr(mv[:], st[:].rearrange("p a b -> p (a b)")), r=[st], w=[mv])
    k.op("act", lambda: nc.scalar.activation(out=rstd[:], in_=mv[:, 1:2], func=AF.Sqrt, bias=P.eps_t[:, 0:1]),
         r=[mv, P.eps_t], w=[rstd])
    k.op("dve", lambda: nc.vector.reciprocal(rstd[:], rstd[:]), r=[rstd], w=[rstd])
    k.op("dve", lambda: nc.vector.tensor_scalar(r[:], r[:], mv[:, 0:1], rstd[:, 0:1], ALU.subtract, ALU.mult),
         r=[r, mv, rstd], w=[r])
    k.op("dve", lambda: nc.vector.tensor_tensor(r[:], r[:], g_bc[:], ALU.mult), r=[r, g_bc], w=[r])
    k.op("dve", lambda: nc.vector.tensor_tensor(out_t[:], r[:], b_bc[:], ALU.add), r=[r, b_bc], w=[out_t])


def phase_post(P, A, KC, fm, w_out, modj, lng, lnb, x_src, x_dst):
    nc, k, S = P.nc, P.k, P.S
    MOD = P.scratch("mod", [8, 3072], F32)
    with ExitStack() as es:
        Wo = k.tile(es, "Wo", [128, KC, 1024], BF16)
        k.dma("pool", Wo[:], w_out.rearrange("(k p) n -> p k n", p=128), w=[Wo])
        gate_bc = bc_load(P, es, "gate_bc", MOD[modj:modj + 1, 2048:3072])
        g_bc = bc_load(P, es, "g_bc", lng)
        b_bc = bc_load(P, es, "b_bc", lnb)
        ident = k.tile(es, "ident", [128, 128], BF16)
        k.dma("sp", ident[:], P.C["ident_bf"], w=[ident])
        U = 2
        idx = make_idx(P, es, "idx", [u * 128 for u in range(U)])
        at = [k.tile(es, f"at{u}", [128, KC * 128], BF16) for u in range(U)]
        if not fm:
            gt = [k.tile(es, f"gt{u}", [128, KC * 128], BF16) for u in range(U)]
            tp = [k.ptile(es, f"tp{u}", [128, 8, 128], BF16) for u in range(U)]
        xt = [k.tile(es, f"xt{u}", [128, 1024], F32) for u in range(U)]
        rt = [k.tile(es, f"rt{u}", [128, 1024], F32) for u in range(U)]
        yo = [k.tile(es, f"yo{u}", [128, 1024], F32) for u in range(U)]
        st = [k.tile(es, f"st{u}", [128, 2, 6], F32) for u in range(U)]
        mv = [k.tile(es, f"mv{u}", [128, 2], F32) for u in range(U)]
        rstd = [k.tile(es, f"rstd{u}", [128, 1], F32) for u in range(U)]
        yp = [[k.ptile(es, f"yp{u}{h}", [128, 512]) for h in range(2)] for u in range(U)]

        def body(i):
            for u in range(U):
                k.gather(xt[u][:], x_src, idx[:, u:u + 1], r=[idx], w=[xt[u]])
                if fm:
                    k.gather(at[u][:], A, idx[:, u:u + 1], r=[idx], w=[at[u]])
                else:
                    k.gather(gt[u][:], A, idx[:, u:u + 1], r=[idx], w=[gt[u]])
                    for g8 in range(KC // 8):
                        for kc in range(8):
                            kk = g8 * 8 + kc
                            k.op("pe", lambda u=u, kc=kc, kk=kk: nc.tensor.transpose(
                                tp[u][:, kc, :], gt[u][:, kk * 128:(kk + 1) * 128], ident[:]),
                                r=[gt[u], ident], w=[tp[u]])
                        k.op("act", lambda u=u, g8=g8: nc.scalar.copy(
                            out=at[u][:, g8 * 1024:(g8 + 1) * 1024].rearrange("p (a b) -> p a b", b=128),
                            in_=tp[u][:]), r=[tp[u]], w=[at[u]])
                for h in range(2):
                    for kc in range(KC):
                        k.op("pe", lambda u=u, h=h, kc=kc: nc.tensor.matmul(
                            yp[u][h][:], lhsT=at[u][:, kc * 128:(kc + 1) * 128],
                            rhs=Wo[:, kc, h * 512:(h + 1) * 512],
                            start=(kc == 0), stop=(kc == KC - 1)), r=[at[u], Wo], w=[yp[u][h]], inc=(kc == KC - 1))
                for h in range(2):
                    hs = slice(h * 512, (h + 1) * 512)
                    k.op("dve", lambda u=u, h=h, hs=hs: nc.vector.tensor_tensor(
                        rt[u][:, hs], yp[u][h][:], gate_bc[:, hs], ALU.mult), r=[yp[u][h], gate_bc], w=[rt[u]])
                k.op("dve", lambda u=u: nc.vector.scalar_tensor_tensor(
                    rt[u][:], xt[u][:], ALPHA, rt[u][:], ALU.mult, ALU.add), r=[xt[u], rt[u]], w=[rt[u]])
                layer_norm_tile(P, rt[u], st[u], mv[u], rstd[u], g_bc, b_bc, yo[u])
                k.scatter(x_dst, yo[u][:], idx[:, u:u + 1], r=[yo[u], idx])
            bump_idx(P, idx, 128 * U)
        k.loop(S // (128 * U), body)
    k.barrier()


def phase_moe(P, li, x_src, x_dst):
    nc, k, S = P.nc, P.k, P.S
    NS = P.moe_ns
    NH = NS // 4
    NTB = S // (128 * NS)
    MOD = P.scratch("mod", [8, 3072], F32)
    j = (2 * li + 1)
    BIG = 1.0e30
    with ExitStack() as es:
        shift_bc = bc_load(P, es, "shift_bc", MOD[j:j + 1, 0:1024])
        scale_bc = bc_load(P, es, "scale_bc", MOD[j:j + 1, 1024:2048])
        gate_bc = bc_load(P, es, "gate_bc", MOD[j:j + 1, 2048:3072])
        g_bc = bc_load(P, es, "g_bc", P.W["ln_g"][li, 1:2, :])
        b_bc = bc_load(P, es, "b_bc", P.W["ln_b"][li, 1:2, :])
        rb_bc = bc_load(P, es, "rb_bc", P.W["router_b"].rearrange("(o e) -> o e", o=1))
        rw = k.tile(es, "rw", [128, 8, 16], F32)
        k.dma("sp", rw[:], P.W["router_w"].rearrange("(k p) e -> p k e", p=128), w=[rw])
        identf = k.tile(es, "identf", [128, 128], F32)
        k.dma("sp", identf[:], P.C["ident_f"], w=[identf])
        idx = make_idx(P, es, "idx", [sub * 128 for sub in range(2 * NS)])
        xs = [k.tile(es, f"xs{s_}", [128, 1024], F32) for s_ in range(2)]
        hf = [k.tile(es, f"hf{u}", [128, 1024], F32) for u in range(2)]
        hT32 = [k.tile(es, f"hT32{u}", [128, 8, 128], F32) for u in range(2)]
        hTs = [k.tile(es, f"hT{u}", [128, 8, 128 * NS], BF16) for u in range(2)]
        combs = [k.tile(es, f"comb{u}", [128, NS, 16], F32) for u in range(2)]
        yacc = [k.tile(es, f"yacc{s_}", [128, 1024], F32) for s_ in range(NS)]
        wg = [k.tile(es, f"wg{u}", [128, 8, 512], BF16) for u in range(2)]
        wu = [k.tile(es, f"wu{u}", [128, 8, 512], BF16) for u in range(2)]
        wd = [k.tile(es, f"wd{u}", [128, 4, 1024], BF16) for u in range(2)]
        sg = [k.tile(es, f"sg{u}", [128, 512], F32) for u in range(2)]
        hid = [k.tile(es, f"hid{u}", [128, 4, 512], BF16) for u in range(2)]
        sm = {n: k.tile(es, "r_" + n, [128, w_], F32) for n, w_ in
              [("lg", 16), ("mx", 1), ("nmx", 1), ("pe", 16), ("sum", 1), ("rs", 1), ("hi", 8), ("lo", 8),
               ("m1", 4), ("m2", 4), ("gs", 4), ("gm", 1), ("gmask", 4), ("ml", 16), ("pen", 16), ("v1", 1),
               ("eq1", 16), ("ml2", 16), ("v2", 1), ("eq2", 16), ("d", 1), ("w1", 1), ("w2", 1), ("t16", 16)]}
        st = [k.tile(es, f"st{u}", [128, 2, 6], F32) for u in range(2)]
        mv = [k.tile(es, f"mv{u}", [128, 2], F32) for u in range(2)]
        rstd = [k.tile(es, f"rstd{u}", [128, 1], F32) for u in range(2)]
        yo = [k.tile(es, f"yo{u}", [128, 1024], F32) for u in range(2)]
        tpf = [k.ptile(es, f"tpf{u}", [128, 4, 128], F32) for u in range(2)]
        Gp = [k.ptile(es, f"Gp{u}", [128, 512]) for u in range(2)]
        Up = [k.ptile(es, f"Up{u}", [128, 512]) for u in range(2)]
        Yp = [k.ptile(es, f"Yp{u}", [128, 512]) for u in range(2)]
        V = nc.vector

        def route(sub, lgp, comb):
            T = sm
            def dv(fn, r, w):
                k.op("dve", fn, r=[T[x] if isinstance(x, str) else x for x in r],
                     w=[T[x] if isinstance(x, str) else x for x in w])
            dv(lambda: V.tensor_tensor(T["lg"][:], lgp[:, 0, 0:16], rb_bc[:], ALU.add), [lgp, rb_bc], ["lg"])
            dv(lambda: V.reduce_max(T["mx"][:], T["lg"][:], axis=AX.X), ["lg"], ["mx"])
            dv(lambda: V.tensor_scalar(T["nmx"][:], T["mx"][:], -1.0, None, ALU.mult), ["mx"], ["nmx"])
            k.op("act", lambda: nc.scalar.activation(out=T["pe"][:], in_=T["lg"][:], func=AF.Exp,
                                                     bias=T["nmx"][:, 0:1]), r=[T["lg"], T["nmx"]], w=[T["pe"]])
            dv(lambda: V.reduce_sum(T["sum"][:], T["pe"][:], axis=AX.X), ["pe"], ["sum"])
            dv(lambda: V.reciprocal(T["rs"][:], T["sum"][:]), ["sum"], ["rs"])
            dv(lambda: V.tensor_scalar(T["pe"][:], T["pe"][:], T["rs"][:, 0:1], None, ALU.mult), ["pe", "rs"], ["pe"])
            pg = T["pe"][:].rearrange("p (g e) -> p g e", e=4)
            hi = T["hi"][:].rearrange("p (g e) -> p g e", e=2)
            lo = T["lo"][:].rearrange("p (g e) -> p g e", e=2)
            dv(lambda: V.tensor_tensor(hi, pg[:, :, 0:4:2], pg[:, :, 1:4:2], ALU.max), ["pe"], ["hi"])
            dv(lambda: V.tensor_tensor(lo, pg[:, :, 0:4:2], pg[:, :, 1:4:2], ALU.min), ["pe"], ["lo"])
            dv(lambda: V.tensor_tensor(T["m1"][:], hi[:, :, 0], hi[:, :, 1], ALU.max), ["hi"], ["m1"])
            dv(lambda: V.tensor_tensor(T["m2"][:], hi[:, :, 0], hi[:, :, 1], ALU.min), ["hi"], ["m2"])
            dv(lambda: V.tensor_tensor(T["gs"][:], lo[:, :, 0], lo[:, :, 1], ALU.max), ["lo"], ["gs"])
            dv(lambda: V.tensor_tensor(T["m2"][:], T["m2"][:], T["gs"][:], ALU.max), ["m2", "gs"], ["m2"])
            dv(lambda: V.tensor_tensor(T["gs"][:], T["m1"][:], T["m2"][:], ALU.add), ["m1", "m2"], ["gs"])
            dv(lambda: V.reduce_max(T["gm"][:], T["gs"][:], axis=AX.X), ["gs"], ["gm"])
            dv(lambda: V.tensor_scalar(T["gmask"][:], T["gs"][:], T["gm"][:, 0:1], None, ALU.is_ge),
               ["gs", "gm"], ["gmask"])
            mlv = T["ml"][:].rearrange("p (g e) -> p g e", e=4)
            penv = T["pen"][:].rearrange("p (g e) -> p g e", e=4)
            lgv = T["lg"][:].rearrange("p (g e) -> p g e", e=4)
            gmb = T["gmask"][:].unsqueeze(2).to_broadcast([128, 4, 4])
            dv(lambda: V.tensor_tensor(penv, P.ones16[:].rearrange("p (g e) -> p g e", e=4), gmb, ALU.mult),
               ["gmask", P.ones16], ["pen"])
            dv(lambda: V.tensor_scalar(T["pen"][:], T["pen"][:], -1.0, BIG, ALU.add, ALU.mult), ["pen"], ["pen"])
            dv(lambda: V.tensor_tensor(T["ml"][:], T["lg"][:], T["pen"][:], ALU.add), ["lg", "pen"], ["ml"])
            dv(lambda: V.reduce_max(T["v1"][:], T["ml"][:], axis=AX.X), ["ml"], ["v1"])
            dv(lambda: V.tensor_scalar(T["eq1"][:], T["ml"][:], T["v1"][:, 0:1], None, ALU.is_ge), ["ml", "v1"], ["eq1"])
            dv(lambda: V.scalar_tensor_tensor(T["ml2"][:], T["eq1"][:], -BIG, T["ml"][:], ALU.mult, ALU.add),
               ["eq1", "ml"], ["ml2"])
            dv(lambda: V.reduce_max(T["v2"][:], T["ml2"][:], axis=AX.X), ["ml2"], ["v2"])
            dv(lambda: V.tensor_scalar(T["eq2"][:], T["ml2"][:], T["v2"][:, 0:1], None, ALU.is_ge), ["ml2", "v2"], ["eq2"])
            dv(lambda: V.tensor_tensor(T["d"][:], T["v2"][:], T["v1"][:], ALU.subtract), ["v2", "v1"], ["d"])
            k.op("act", lambda: nc.scalar.activation(out=T["d"][:], in_=T["d"][:], func=AF.Exp), r=[T["d"]], w=[T["d"]])
            dv(lambda: V.tensor_scalar(T["w1"][:], T["d"][:], 1.0, None, ALU.add), ["d"], ["w1"])
            dv(lambda: V.reciprocal(T["w1"][:], T["w1"][:]), ["w1"], ["w1"])
            dv(lambda: V.tensor_tensor(T["w2"][:], T["d"][:], T["w1"][:], ALU.mult), ["d", "w1"], ["w2"])
            dv(lambda: V.tensor_scalar(T["t16"][:], T["eq1"][:], T["w1"][:, 0:1], None, ALU.mult), ["eq1", "w1"], ["t16"])
            dv(lambda: V.scalar_tensor_tensor(comb[:, sub, :], T["eq2"][:], T["w2"][:, 0:1], T["t16"][:],
                                              ALU.mult, ALU.add), ["eq2", "w2", "t16"], [comb])

        def load_w(e):
            u = e % 2
            k.dma("pool", wg[u][:], P.W["moe_w_gate"][li, e].rearrange("(k p) f -> p k f", p=128), w=[wg[u]])
            k.dma("pool", wu[u][:], P.W["moe_w_up"][li, e].rearrange("(k p) f -> p k f", p=128), w=[wu[u]])
            k.dma("pool", wd[u][:], P.W["moe_w_down"][li, e].rearrange("(k p) d -> p k d", p=128), w=[wd[u]])

        def prologue(bp):
            hT, comb = hTs[bp], combs[bp]
            for sub in range(NS):
                u = sub % 2
                k.gather(xs[u][:], x_src, idx[:, bp * NS + sub:bp * NS + sub + 1], r=[idx], w=[xs[u]])
                if P.moe_lvl < 1:
                    continue
                k.op("dve", lambda sub=sub, u=u: V.tensor_tensor(hf[u][:], xs[u][:], scale_bc[:], ALU.mult),
                     r=[xs[u], scale_bc], w=[hf[u]])
                k.op("dve", lambda u=u: V.tensor_tensor(hf[u][:], hf[u][:], shift_bc[:], ALU.add),
                     r=[hf[u], shift_bc], w=[hf[u]])
                for g in range(2):
                    if P.moe_lvl < 0.5:
                        continue
                    for kk in range(4):
                        kc = g * 4 + kk
                        k.op("pe", lambda g=g, kk=kk, kc=kc, u=u: nc.tensor.matmul(
                            tpf[g][:, kk, :], lhsT=hf[u][:, kc * 128:(kc + 1) * 128], rhs=identf[:],
                            start=True, stop=True), r=[hf[u], identf], w=[tpf[g]])
                    if P.moe_lvl < 0.7:
                        continue
                    k.op("act", lambda g=g, u=u: nc.scalar.copy(out=hT32[u][:, g * 4:(g + 1) * 4, :], in_=tpf[g][:]),
                         r=[tpf[g]], w=[hT32[u]])
                    if P.moe_lvl < 0.9:
                        continue
                    k.op("act", lambda g=g, sub=sub: nc.scalar.copy(
                        out=hT[:, g * 4:(g + 1) * 4, sub * 128:(sub + 1) * 128], in_=tpf[g][:]), r=[tpf[g]], w=[hT])
                lgp = tpf[0]
                if P.moe_lvl < 2:
                    continue
                for kc in range(8):
                    k.op("pe", lambda kc=kc, u=u, lgp=lgp: nc.tensor.matmul(
                        lgp[:, 0, 0:16], lhsT=hT32[u][:, kc, :], rhs=rw[:, kc, :], start=(kc == 0), stop=(kc == 7)),
                        r=[hT32[u], rw], w=[lgp], inc=(kc == 7))
                if P.moe_lvl < 3:
                    continue
                route(sub, lgp, comb)

        def experts(bp):
            hT, comb = hTs[bp], combs[bp]
            load_w(0)
            for e in range(P.moe_ne):
                u = e % 2
                if e + 1 < P.moe_ne:
                    load_w(e + 1)
                for hh in range(NH):
                    hu = (e * NH + hh) % 2
                    ts = slice(hh * 512, (hh + 1) * 512)
                    for fc in range(4):
                        pu = fc % 2
                        fs = slice(fc * 128, (fc + 1) * 128)
                        for kc in range(8):
                            k.op("pe", lambda pu=pu, kc=kc, fs=fs, u=u, ts=ts: nc.tensor.matmul(
                                Gp[pu][:], lhsT=wg[u][:, kc, fs], rhs=hT[:, kc, ts], start=(kc == 0), stop=(kc == 7)),
                                r=[wg[u], hT], w=[Gp[pu]], inc=(kc == 7))
                        for kc in range(8):
                            k.op("pe", lambda pu=pu, kc=kc, fs=fs, u=u, ts=ts: nc.tensor.matmul(
                                Up[pu][:], lhsT=wu[u][:, kc, fs], rhs=hT[:, kc, ts], start=(kc == 0), stop=(kc == 7)),
                                r=[wu[u], hT], w=[Up[pu]], inc=(kc == 7))
                        k.op("act", lambda pu=pu: nc.scalar.activation(out=sg[pu][:], in_=Gp[pu][:], func=AF.Silu),
                             r=[Gp[pu]], w=[sg[pu]])
                        k.op("dve", lambda pu=pu, fc=fc, hu=hu: V.tensor_tensor(hid[hu][:, fc, :], sg[pu][:], Up[pu][:],
                                                                               ALU.mult), r=[sg[pu], Up[pu]], w=[hid[hu]])
                    for s4 in range(4):
                        sub = hh * 4 + s4
                        for nh in range(2):
                            py = (s4 * 2 + nh) % 2
                            for fc in range(4):
                                k.op("pe", lambda py=py, fc=fc, s4=s4, nh=nh, u=u, hu=hu: nc.tensor.matmul(
                                    Yp[py][:], lhsT=hid[hu][:, fc, s4 * 128:(s4 + 1) * 128],
                                    rhs=wd[u][:, fc, nh * 512:(nh + 1) * 512], start=(fc == 0), stop=(fc == 3)),
                                    r=[hid[hu], wd[u]], w=[Yp[py]], inc=(fc == 3))
                            hs = slice(nh * 512, (nh + 1) * 512)
                            if e == 0:
                                k.op("dve", lambda py=py, sub=sub, hs=hs, e=e: V.tensor_scalar(
                                    yacc[sub][:, hs], Yp[py][:], comb[:, sub, e:e + 1], None, ALU.mult),
                                    r=[Yp[py], comb], w=[yacc[sub]])
                            else:
                                k.op("dve", lambda py=py, sub=sub, hs=hs, e=e: V.scalar_tensor_tensor(
                                    yacc[sub][:, hs], Yp[py][:], comb[:, sub, e:e + 1], yacc[sub][:, hs],
                                    ALU.mult, ALU.add), r=[Yp[py], comb, yacc[sub]], w=[yacc[sub]])

        def epilogue(bp):
            for sub in range(NS):
                u = sub % 2
                k.gather(xs[u][:], x_src, idx[:, bp * NS + sub:bp * NS + sub + 1], r=[idx], w=[xs[u]])
                k.op("dve", lambda sub=sub: V.tensor_tensor(yacc[sub][:], yacc[sub][:], gate_bc[:], ALU.mult),
                     r=[yacc[sub], gate_bc], w=[yacc[sub]])
                k.op("dve", lambda sub=sub, u=u: V.scalar_tensor_tensor(
                    yacc[sub][:], xs[u][:], ALPHA, yacc[sub][:], ALU.mult, ALU.add),
                    r=[xs[u], yacc[sub]], w=[yacc[sub]])
                layer_norm_tile(P, yacc[sub], st[u], mv[u], rstd[u], g_bc, b_bc, yo[u])
                k.scatter(x_dst, yo[u][:], idx[:, bp * NS + sub:bp * NS + sub + 1], r=[yo[u], idx])
            k.op("dve", lambda: V.tensor_scalar(idx[:, bp * NS:(bp + 1) * NS], idx[:, bp * NS:(bp + 1) * NS],
                                                float(256 * NS), None, ALU.add), r=[idx], w=[idx])

        prologue(0)

        def body(tb):
            k.replay(k.record(lambda: experts(0)), k.record(lambda: prologue(1)))
            epilogue(0)
            k.replay(k.record(lambda: experts(1)), k.record(lambda: prologue(0)))
            epilogue(1)
        k.loop(NTB // 2, body)
    k.barrier()


def phase_rope_table(P):
    nc, k, S = P.nc, P.k, P.S
    NT = S // 128
    TAB = P.scratch("rope", [S + 128, 128], F32)
    V = nc.vector
    TWO_PI = 6.283185307179586
    C1, C2 = 6.28125, TWO_PI - 6.28125
    PI_LO = 3.1415925
    with ExitStack() as es:
        pi_ = k.tile(es, "pos_i", [128, NT], I32)
        pf = k.tile(es, "pos_f", [128, NT], F32)
        inv = k.tile(es, "invf", [128, 64], F32)
        k.dma("sp", pi_[:], P.posT, w=[pi_])
        k.dma("sp", inv[:], P.C["invfreq"], w=[inv])
        k.op("dve", lambda: V.tensor_copy(pf[:], pi_[:]), r=[pi_], w=[pf])
        ang = [k.tile(es, f"ang{u}", [128, 128], F32) for u in range(2)]
        kq = [k.tile(es, f"kq{u}", [128, 128], F32) for u in range(2)]
        ki = [k.tile(es, f"ki{u}", [128, 128], I32) for u in range(2)]
        mk = [k.tile(es, f"mk{u}", [128, 128], F32) for u in range(2)]
        tb = [k.tile(es, f"tbl{u}", [128, 128], F32) for u in range(2)]
        for i in range(NT):
            u = i % 2
            a, q, qi, m, t = ang[u], kq[u], ki[u], mk[u], tb[u]
            k.op("dve", lambda a=a, i=i: V.tensor_scalar(a[:, 64:128], inv[:], pf[:, i:i + 1], None, ALU.mult),
                 r=[inv, pf], w=[a])
            k.op("dve", lambda a=a: V.tensor_scalar(a[:, 0:64], a[:, 64:128], 1.5707963267948966, None, ALU.add),
                 r=[a], w=[a])
            k.op("dve", lambda a=a, q=q: V.tensor_scalar(q[:], a[:], 1.0 / TWO_PI, None, ALU.mult), r=[a], w=[q])
            k.op("dve", lambda q=q, qi=qi: V.tensor_copy(qi[:], q[:]), r=[q], w=[qi])
            k.op("dve", lambda q=q, qi=qi: V.tensor_copy(q[:], qi[:]), r=[qi], w=[q])
            k.op("dve", lambda a=a, q=q: V.scalar_tensor_tensor(a[:], q[:], -C1, a[:], ALU.mult, ALU.add),
                 r=[a, q], w=[a])
            k.op("dve", lambda a=a, q=q: V.scalar_tensor_tensor(a[:], q[:], -C2, a[:], ALU.mult, ALU.add),
                 r=[a, q], w=[a])
            for sgn, cmp_ in ((-1.0, ALU.is_gt), (1.0, ALU.is_lt)):
                k.op("dve", lambda a=a, m=m, sgn=sgn, cmp_=cmp_: V.tensor_scalar(
                    m[:], a[:], -sgn * 3.141592653589793, None, cmp_), r=[a], w=[m])
                k.op("dve", lambda a=a, m=m, sgn=sgn: V.scalar_tensor_tensor(
                    a[:], m[:], sgn * TWO_PI, a[:], ALU.mult, ALU.add), r=[a, m], w=[a])
            k.op("dve", lambda a=a: V.tensor_scalar(a[:], a[:], PI_LO, -PI_LO, ALU.min, ALU.max), r=[a], w=[a])
            k.op("act", lambda a=a, t=t: nc.scalar.activation(out=t[:], in_=a[:], func=AF.Sin), r=[a], w=[t])
            k.dma("sp", TAB[i * 128:(i + 1) * 128, :], t[:], r=[t])
    k.barrier()


def phase_ret(P, li, x_src):
    nc, k, S = P.nc, P.k, P.S
    NT = S // 128
    MOD = P.scratch("mod", [8, 3072], F32)
    TAB = P.scratch("rope", [S + 128, 128], F32)
    G = P.scratch("retG", [S, 2048], BF16)
    j = (2 * li + 1) * 2 + 0
    V = nc.vector
    with ExitStack() as es:
        Win = k.tile(es, "Win", [128, 8, 6144], BF16)
        for part in range(4):
            k.dma("pool", Win[:, 2 * part:2 * part + 2, :],
                  P.W["ret_w_in"][li, part * 256:(part + 1) * 256, :].rearrange("(k p) n -> p k n", p=128),
                  w=[Win], key=f"d_Win{part}")
        ident = k.tile(es, "ident", [128, 128], BF16)
        k.dma("sp", ident[:], P.C["ident_bf"], w=[ident])
        shift_bc = bc_load(P, es, "shift_bc", MOD[j:j + 1, 0:1024])
        scale_bc = bc_load(P, es, "scale_bc", MOD[j:j + 1, 1024:2048])
        gn_bc = bc_load(P, es, "gn_bc", P.W["ret_gn_g"][li:li + 1, :])
        dmk = k.tile(es, "dmk", [128, 8, 128], F32)
        k.dma("sp", dmk[:], P.C["dmaskT"], w=[dmk])
        xi = k.tile(es, "xi", [128, 8], F32)
        k.dma("sp", xi[:], P.C["xi"], w=[xi])
        zs = k.tile(es, "zs", [128, 8], F32)
        k.dma("sp", zs[:], P.C["zetas"], w=[zs])
        idx = make_idx(P, es, "idx", [0, 128])
        xt = k.tile(es, "xt", [128, 1024], F32)
        hbs = [k.tile(es, f"hb{u}", [128, 1024], BF16) for u in range(2)]
        hTs = [k.tile(es, f"hT{u}", [128, 8, 128], BF16) for u in range(2)]
        css = [k.tile(es, f"cs{u}", [128, 128], F32) for u in range(2)]
        qf = k.tile(es, "qf", [128, 1024], F32)
        qr = k.tile(es, "qr", [128, 1024], F32)
        kf, kr = qf, qr
        t1 = k.tile(es, "t1", [128, 512], F32)
        t2 = k.tile(es, "t2", [128, 512], F32)
        qb = k.tile(es, "qb", [128, 1024], BF16)
        kb = k.tile(es, "kb", [128, 1024], BF16)
        kzs = [k.tile(es, f"kz{u}", [128, 1024], BF16) for u in range(2)]
        qTs = [k.tile(es, f"qT{u}", [128, 8, 128], BF16) for u in range(2)]
        kTs = [k.tile(es, f"kT{u}", [128, 8, 128], BF16) for u in range(2)]
        vbs = [k.tile(es, f"vb{u}", [128, 2048], BF16) for u in range(2)]
        sgts = [k.tile(es, f"sgt{u}", [128, 2048], BF16) for u in range(2)]
        gouts = [k.tile(es, f"gout{u}", [128, 2048], BF16) for u in range(2)]
        state = [k.tile(es, f"state{h}", [128, 256], F32) for h in range(8)]
        sbf = [k.tile(es, f"sbf{h}", [128, 256], BF16) for h in range(8)]
        Pm = [k.tile(es, f"Pm{u}", [128, 128], BF16) for u in range(2)]
        on = [k.tile(es, f"on{u}", [128, 256], F32) for u in range(2)]
        st = [k.tile(es, f"st{u}", [128, 6], F32) for u in range(2)]
        mv = [k.tile(es, f"mv{u}", [128, 2], F32) for u in range(2)]
        rstd = [k.tile(es, f"rstd{u}", [128, 1], F32) for u in range(2)]
        tp = [k.ptile(es, f"tp{u}", [128, 8, 128], BF16) for u in range(2)]
        mm = [k.ptile(es, f"mm{u}", [128, 512]) for u in range(2)]
        recA = [k.ptile(es, f"recA{u}", [128, 512]) for u in range(2)]
        recB = [k.ptile(es, f"recB{u}", [128, 512]) for u in range(2)]
        for h in range(8):
            k.op("dve", lambda h=h: V.memset(state[h][:], 0.0), w=[state[h]])
            k.op("dve", lambda h=h: V.memset(sbf[h][:], 0.0), w=[sbf[h]])

        def rotary(src, dst, cs):
            sv = src[:].rearrange("p (h t d) -> p h t d", h=8, t=2)
            dv_ = dst[:].rearrange("p (h t d) -> p h t d", h=8, t=2)
            cosb = cs[:, 0:64].unsqueeze(1).to_broadcast([128, 8, 64])
            sinb = cs[:, 64:128].unsqueeze(1).to_broadcast([128, 8, 64])
            a1 = t1[:].rearrange("p (h d) -> p h d", h=8)
            a2 = t2[:].rearrange("p (h d) -> p h d", h=8)
            k.op("dve", lambda: V.tensor_tensor(a1, sv[:, :, 0, :], cosb, ALU.mult), r=[src, cs], w=[t1])
            k.op("dve", lambda: V.tensor_tensor(a2, sv[:, :, 1, :], sinb, ALU.mult), r=[src, cs], w=[t2])
            k.op("dve", lambda: V.tensor_tensor(dv_[:, :, 0, :], a1, a2, ALU.subtract), r=[t1, t2], w=[dst])
            k.op("dve", lambda: V.tensor_tensor(a1, sv[:, :, 1, :], cosb, ALU.mult), r=[src, cs], w=[t1])
            k.op("dve", lambda: V.tensor_tensor(a2, sv[:, :, 0, :], sinb, ALU.mult), r=[src, cs], w=[t2])
            k.op("dve", lambda: V.tensor_tensor(dv_[:, :, 1, :], a1, a2, ALU.add), r=[t1, t2], w=[dst])

        def inproj(c_):
            hb, hT, cs, kz, qT, kT, vb, sgt = hbs[c_], hTs[c_], css[c_], kzs[c_], qTs[c_], kTs[c_], vbs[c_], sgts[c_]
            k.gather(xt[:], x_src, idx[:, c_:c_ + 1], r=[idx], w=[xt])
            k.gather(cs[:], TAB, idx[:, c_:c_ + 1], r=[idx], w=[cs])
            modulate(P, xt, scale_bc, shift_bc, hb)
            for kc in range(8):
                k.op("pe", lambda kc=kc: nc.tensor.transpose(tp[0][:, kc, :], hb[:, kc * 128:(kc + 1) * 128],
                                                             ident[:]), r=[hb, ident], w=[tp[0]])
            k.op("act", lambda: nc.scalar.copy(out=hT[:], in_=tp[0][:]), r=[tp[0]], w=[hT])
            def proj_chunk(n_i, n):
                m = mm[n_i % 2]
                for kc in range(8):
                    k.op("pe", lambda m=m, kc=kc, n=n: nc.tensor.matmul(
                        m[:], lhsT=hT[:, kc, :], rhs=Win[:, kc, n * 512:(n + 1) * 512],
                        start=(kc == 0), stop=(kc == 7)), r=[hT, Win], w=[m], inc=(kc == 7))
                if n < 2:
                    k.op("act", lambda m=m, n=n: nc.scalar.copy(out=qf[:, n * 512:(n + 1) * 512], in_=m[:]),
                         r=[m], w=[qf])
                elif n < 4:
                    k.op("act", lambda m=m, n=n: nc.scalar.copy(out=kf[:, (n - 2) * 512:(n - 1) * 512], in_=m[:]),
                         r=[m], w=[kf])
                elif n < 8:
                    k.op("act", lambda m=m, n=n: nc.scalar.copy(out=vb[:, (n - 4) * 512:(n - 3) * 512], in_=m[:]),
                         r=[m], w=[vb])
                else:
                    k.op("act", lambda m=m, n=n: nc.scalar.activation(
                        out=sgt[:, (n - 8) * 512:(n - 7) * 512], in_=m[:], func=AF.Silu), r=[m], w=[sgt])
            for n_i, n in enumerate([4, 5, 6, 7, 8, 9, 10, 11, 0, 1]):
                proj_chunk(n_i, n)
            rotary(qf, qr, cs)
            k.op("dve", lambda: V.tensor_tensor(qb[:].rearrange("p (h d) -> p h d", h=8),
                                                qr[:].rearrange("p (h d) -> p h d", h=8),
                                                xi[:].unsqueeze(2).to_broadcast([128, 8, 128]), ALU.mult),
                 r=[qr, xi], w=[qb])
            for n_i, n in enumerate([2, 3]):
                proj_chunk(n_i, n)
            rotary(kf, kr, cs)
            k.op("dve", lambda: V.tensor_scalar(kb[:], kr[:], 128.0 ** -0.5, None, ALU.mult), r=[kr], w=[kb])
            k.op("dve", lambda: V.tensor_tensor(kz[:].rearrange("p (h d) -> p h d", h=8),
                                                kr[:].rearrange("p (h d) -> p h d", h=8),
                                                zs[:].unsqueeze(2).to_broadcast([128, 8, 128]), ALU.mult),
                 r=[kr, zs], w=[kz])
            for h in range(8):
                k.op("pe", lambda h=h: nc.tensor.transpose(tp[0][:, h, :], qb[:, h * 128:(h + 1) * 128], ident[:]),
                     r=[qb, ident], w=[tp[0]])
            k.op("act", lambda: nc.scalar.copy(out=qT[:], in_=tp[0][:]), r=[tp[0]], w=[qT])
            for h in range(8):
                k.op("pe", lambda h=h: nc.tensor.transpose(tp[1][:, h, :], kb[:, h * 128:(h + 1) * 128], ident[:]),
                     r=[kb, ident], w=[tp[1]])
            k.op("act", lambda: nc.scalar.copy(out=kT[:], in_=tp[1][:]), r=[tp[1]], w=[kT])

        def recur(c_):
            kz, qT, kT, vb, sgt, gout = kzs[c_], qTs[c_], kTs[c_], vbs[c_], sgts[c_], gouts[c_]
            for h in range(8):
                u = h % 2
                vs = slice(h * 256, (h + 1) * 256)
                k.op("pe", lambda h=h, u=u: nc.tensor.matmul(recA[u][:, 0:128], lhsT=kT[:, h, :], rhs=qT[:, h, :],
                                                            start=True, stop=True), r=[kT, qT], w=[recA[u]])
                k.op("dve", lambda h=h, u=u: V.tensor_tensor(Pm[u][:], recA[u][:, 0:128], dmk[:, h, :], ALU.mult),
                     r=[recA[u], dmk], w=[Pm[u]])
                k.op("pe", lambda h=h, u=u, vs=vs: nc.tensor.matmul(recB[u][:, 0:256], lhsT=Pm[u][:], rhs=vb[:, vs],
                                                                   start=True, stop=False), r=[Pm[u], vb], w=[recB[u]])
                k.op("pe", lambda h=h, u=u: nc.tensor.matmul(recB[u][:, 0:256], lhsT=qT[:, h, :], rhs=sbf[h][:],
                                                            start=False, stop=True), r=[qT, sbf[h]], w=[recB[u]])
                k.op("pe", lambda h=h, u=u, vs=vs: nc.tensor.matmul(recA[u][:, 128:384],
                                                                   lhsT=kz[:, h * 128:(h + 1) * 128], rhs=vb[:, vs],
                                                                   start=True, stop=True), r=[kz, vb], w=[recA[u]])
                k.op("dve", lambda h=h, u=u: V.scalar_tensor_tensor(
                    state[h][:], state[h][:], GAMMA[h] ** 128, recA[u][:, 128:384], ALU.mult, ALU.add),
                    r=[state[h], recA[u]], w=[state[h]])
                k.op("act", lambda h=h: nc.scalar.copy(out=sbf[h][:], in_=state[h][:]), r=[state[h]], w=[sbf[h]])
                k.op("dve", lambda u=u: V.bn_stats(st[u][:], recB[u][:, 0:256]), r=[recB[u]], w=[st[u]])
                k.op("dve", lambda u=u: V.bn_aggr(mv[u][:], st[u][:]), r=[st[u]], w=[mv[u]])
                k.op("act", lambda u=u: nc.scalar.activation(out=rstd[u][:], in_=mv[u][:, 1:2], func=AF.Sqrt,
                                                             bias=P.eps_t[:, 0:1]), r=[mv[u], P.eps_t], w=[rstd[u]])
                k.op("dve", lambda u=u: V.reciprocal(rstd[u][:], rstd[u][:]), r=[rstd[u]], w=[rstd[u]])
                k.op("dve", lambda u=u: V.tensor_scalar(on[u][:], recB[u][:, 0:256], mv[u][:, 0:1], rstd[u][:, 0:1],
                                                        ALU.subtract, ALU.mult), r=[recB[u], mv[u], rstd[u]], w=[on[u]])
                k.op("dve", lambda u=u, vs=vs: V.tensor_tensor(on[u][:], on[u][:], gn_bc[:, vs], ALU.mult),
                     r=[on[u], gn_bc], w=[on[u]])
                k.op("dve", lambda u=u, vs=vs: V.tensor_tensor(gout[:, vs], on[u][:], sgt[:, vs], ALU.mult),
                     r=[on[u], sgt], w=[gout])
            k.scatter(G, gout[:], idx[:, c_:c_ + 1], r=[gout, idx])

        def bump_col(c_):
            k.op("dve", lambda: V.tensor_scalar(idx[:, c_:c_ + 1], idx[:, c_:c_ + 1], 256.0, None, ALU.add),
                 r=[idx], w=[idx])

        inproj(0)

        def body(i):
            k.replay(k.record(lambda: inproj(1)), k.record(lambda: recur(0)))
            bump_col(0)
            k.replay(k.record(lambda: inproj(0)), k.record(lambda: recur(1)))
            bump_col(1)
        k.loop(NT // 2, body)
    k.barrier()


def setup_globals(P):
    nc, k = P.nc, P.k
    def gt(name, shape, dt):
        t = Tile(name, nc.alloc_sbuf_tensor(name, list(shape), dt))
        k.tiles.append(t)
        return t
    P.one_t = gt("one_t", [128, 1], F32)
    P.eps_t = gt("eps_t", [128, 1], F32)
    P.ones16 = gt("ones16", [128, 16], F32)
    k.op("dve", lambda: nc.vector.memset(P.one_t[:], 1.0), w=[P.one_t])
    k.op("dve", lambda: nc.vector.memset(P.eps_t[:], LN_EPS), w=[P.eps_t])
    k.dma("sp", P.ones16[:], P.C["ones16"], w=[P.ones16])
    k.barrier()


def build_program(S, n_layers=DEPTH, dbg=False):
    P = Prog(S, dbg)
    setup_globals(P)
    XR = P.scratch("xr", [S + 1024, D], F32)
    phase_adaln(P)
    if n_layers > 1:
        phase_rope_table(P)
    src = P.x_in
    for l in range(n_layers):
        last = (l == n_layers - 1)
        li = l // 2
        if l % 2 == 0:
            phase_sb_inproj(P, li, src)
            phase_sb_attn(P)
            phase_post(P, P.scratch("sbC", [S, 1024], BF16), 8, True, P.W["sb_w_out"][li], 2 * l,
                       P.W["ln_g"][l, 0:1, :], P.W["ln_b"][l, 0:1, :], src, XR)
        else:
            phase_ret(P, li, src)
            phase_post(P, P.scratch("retG", [S, 2048], BF16), 16, False, P.W["ret_w_out"][li], 2 * l,
                       P.W["ln_g"][l, 0:1, :], P.W["ln_b"][l, 0:1, :], src, XR)
        src = XR
        phase_moe(P, l, XR, P.out if last else XR)
    P.k.barrier()
    return P


_CACHE = {}


def kernel(**inputs):
    S = inputs["x"].shape[1]
    B = inputs["x"].shape[0]
    if S not in _CACHE:
        _CACHE[S] = build_program(S)
    P = _CACHE[S]
    consts = make_consts()
    in_maps = []
    for b in range(B):
        m = {"x": np.ascontiguousarray(inputs["x"][b], dtype=np.float32),
             "cT": np.ascontiguousarray(np.asarray(inputs["c"][b], dtype=np.float32).reshape(8, 128).T),
             "posT": np.ascontiguousarray(np.asarray(inputs["positions"][b], dtype=np.int32).reshape(S // 128, 128).T)}
        for n in WSHAPES:
            m[n] = np.ascontiguousarray(inputs[n], dtype=np.float32)
        for n, v in consts.items():
            m["c_" + n] = v
        in_maps.append(m)
    res = run_bass_kernel_spmd(P.nc, in_maps, core_ids=list(range(B)))
    return np.stack([np.asarray(r["out"], dtype=np.float32) for r in res.results], axis=0)
```

```python
import numpy as np
import ml_dtypes
from contextlib import ExitStack
import concourse.bass as bass
import concourse.mybir as mybir
from concourse.bass_utils import run_bass_kernel_spmd

F32 = mybir.dt.float32
BF16 = mybir.dt.bfloat16
I32 = mybir.dt.int32
AF = mybir.ActivationFunctionType
ALU = mybir.AluOpType
AX = mybir.AxisListType

D = 1024
DEPTH = 4
ALPHA = (2 * DEPTH) ** 0.25
LN_EPS = 1e-5


class Aff:
    __slots__ = ("c", "k")

    def __init__(self, c=0, k=()):
        self.c = c
        self.k = tuple(k)

    def add(self, n):
        return Aff(self.c + n, self.k)

    def le(self, o):
        return self.k == o.k and self.c <= o.c


class Tile:
    def __init__(self, name, t):
        self.name = name
        self.t = t
        self.wr = None
        self.rd = {}

    def __getitem__(self, idx):
        return self.t[idx]


class K:
    ENG = ("pe", "act", "dve", "pool", "sp")

    def __init__(self, nc):
        self.nc = nc
        self.E = {"pe": nc.tensor, "act": nc.scalar, "dve": nc.vector,
                  "pool": nc.gpsimd, "sp": nc.sync}
        self.sems = {}
        self.cur = {}
        self.known = {e: {} for e in self.ENG}
        self.dirty = set()
        self.tiles = []
        self.dry = 0
        self.loops = []
        self.nloop = 0
        self.tregs = {}
        for e in ("pe", "act", "dve", "pool"):
            self._sem("e_" + e)

    def _sem(self, key):
        if key not in self.sems:
            self.sems[key] = self.nc.alloc_semaphore(key)
            self.cur[key] = Aff(0)
        return self.sems[key]

    def _treg(self, eng):
        if eng not in self.tregs:
            self.tregs[eng] = self.E[eng].alloc_register("kw_" + eng)
        return self.tregs[eng]

    def _val(self, a):
        v = a.c
        for lid, coef in a.k:
            var = [x for (l, x) in self.loops if l == lid][0]
            v = var * coef + v
        return v

    def _wait(self, eng, evs):
        for key, a in evs:
            if eng == "pe" and key == "e_pe":
                continue
            kn = self.known[eng].get(key)
            if kn is not None and a.le(kn):
                continue
            self.known[eng][key] = a
            if not self.dry:
                if a.k:
                    assert len(a.k) == 1
                    lid, coef = a.k[0]
                    var = [x for (l, x) in self.loops if l == lid][0]
                    T = self._treg(eng)
                    self.E[eng].reg_mul(T, var, coef)
                    self.E[eng].reg_add(T, T, a.c)
                    self.E[eng].wait_ge(self.sems[key], T)
                else:
                    self.E[eng].wait_ge(self.sems[key], a.c)

    def _deps(self, r, w):
        evs = []
        for t in r:
            if t.wr is not None:
                evs.append(t.wr)
        for t in w:
            if t.wr is not None:
                evs.append(t.wr)
            evs.extend(t.rd.items())
        return evs

    def _mark(self, ev, r, w):
        key, a = ev
        for t in r:
            t.rd[key] = a
        for t in w:
            t.wr = ev
            t.rd = {}

    def tile(self, es, name, shape, dt):
        self.uid = getattr(self, "uid", 0) + 1
        t = Tile(name, es.enter_context(self.nc.sbuf_tensor(f"{name}_{self.uid}", list(shape), dt)))
        self.tiles.append(t)
        return t

    def ptile(self, es, name, shape, dt=F32):
        self.uid = getattr(self, "uid", 0) + 1
        t = Tile(name, es.enter_context(self.nc.psum_tensor(f"{name}_{self.uid}", list(shape), dt)))
        self.tiles.append(t)
        return t

    def vtile(self, name):
        t = Tile(name, None)
        self.tiles.append(t)
        return t

    def op(self, eng, fn, r=(), w=(), inc=True):
        self._wait(eng, self._deps(r, w))
        if not inc:
            if not self.dry:
                fn()
            return
        key = "e_" + eng
        self.cur[key] = self.cur[key].add(1)
        self.dirty.add(key)
        if not self.dry:
            fn().then_inc(self.sems[key], 1)
        self._mark((key, self.cur[key]), r, w)

    def dma(self, q, out, in_, r=(), w=(), key=None):
        if key is None:
            key = "d_" + (w[0].name if w else r[0].name)
        self._sem(key)
        self._wait(q, self._deps(r, w))
        self.cur[key] = self.cur[key].add(16)
        self.dirty.add(key)
        if not self.dry:
            o = out() if callable(out) else out
            i = in_() if callable(in_) else in_
            self.E[q].dma_start(out=o, in_=i).then_inc(self.sems[key], 16)
        self._mark((key, self.cur[key]), r, w)

    def gather(self, out, src, idx, r=(), w=(), key=None):
        if key is None:
            key = "d_" + w[0].name
        self._sem(key)
        self._wait("pool", self._deps(r, w))
        self.cur[key] = self.cur[key].add(16)
        self.dirty.add(key)
        if not self.dry:
            self.nc.gpsimd.indirect_dma_start(
                out=out, out_offset=None, in_=src,
                in_offset=bass.IndirectOffsetOnAxis(ap=idx, axis=0)).then_inc(self.sems[key], 16)
        self._mark((key, self.cur[key]), r, w)

    def scatter(self, dst, in_, idx, r=(), w=(), key=None):
        if key is None:
            key = "d_" + r[0].name
        self._sem(key)
        self._wait("pool", self._deps(r, w))
        self.cur[key] = self.cur[key].add(16)
        self.dirty.add(key)
        if not self.dry:
            self.nc.gpsimd.indirect_dma_start(
                out=dst, out_offset=bass.IndirectOffsetOnAxis(ap=idx, axis=0), in_=in_,
                in_offset=None).then_inc(self.sems[key], 16)
        self._mark((key, self.cur[key]), r, w)

    def record(self, fn):
        rec = []
        names = ("op", "dma", "gather", "scatter")
        for nm in names:
            setattr(self, nm, (lambda nm: (lambda *a, **kw: rec.append((nm, a, kw))))(nm))
        try:
            fn()
        finally:
            for nm in names:
                delattr(self, nm)
        return rec

    def replay(self, *recs):
        pos = [0] * len(recs)
        total = sum(len(r) for r in recs)
        for _ in range(total):
            best = min((pos[i] / len(r), i) for i, r in enumerate(recs) if pos[i] < len(r))[1]
            nm, a, kw = recs[best][pos[best]]
            pos[best] += 1
            getattr(K, nm)(self, *a, **kw)

    def _clean(self):
        for t in self.tiles:
            t.wr = None
            t.rd = {}

    def barrier(self, keys=None):
        keys = sorted(self.dirty) if keys is None else sorted(keys)
        for e in self.ENG:
            for key in keys:
                self._wait(e, [(key, self.cur[key])])
        self.dirty -= set(keys)
        self._clean()

    def _release(self):
        for e in ("pe", "act", "dve", "pool"):
            h = self._sem("r_" + e)
            self.nc.sync.sem_inc(h, 1)
            self.E[e].wait_ge(h, 1)
            self.E[e].sem_clear(h)

    def _reset(self):
        for key in sorted(self.cur):
            if key.startswith("r_"):
                continue
            if self.cur[key].c:
                self.nc.sync.wait_ge(self.sems[key], self.cur[key].c)
                self.nc.sync.sem_clear(self.sems[key])
                self.cur[key] = Aff(0)
        self.dirty = set()
        self.known = {e: {} for e in self.ENG}
        self._clean()
        self._release()

    def loop_reset(self, n, body):
        assert n >= 1
        self.barrier()
        self._reset()
        with self.nc.Fori(0, n) as i:
            body(i)
            self._reset()


    def loop(self, n, body):
        assert n >= 1
        self.barrier()
        save = dict(self.cur)
        sk = {e: dict(v) for e, v in self.known.items()}
        self.dry += 1
        body(0)
        self.dry -= 1
        per = {}
        for key, a in self.cur.items():
            d = a.c - save[key].c if key in save else a.c
            if d:
                per[key] = d
        for key in list(self.cur):
            if key not in save:
                save[key] = Aff(0)
        self.cur = dict(save)
        self.known = sk
        self._clean()
        lid = self.nloop
        self.nloop += 1
        for key, d in sorted(per.items()):
            self.cur[key] = self.cur[key].add(d)
            if not self.dry:
                self.nc.sync.sem_inc(self.sems[key], d)
        for e in self.ENG:
            for key in sorted(per):
                self._wait(e, [(key, self.cur[key])])
        base = dict(self.cur)
        kn_entry = {e: dict(v) for e, v in self.known.items()}

        def set_iter(off):
            for key, d in per.items():
                b = base[key]
                self.cur[key] = Aff(b.c + off * d, b.k + ((lid, d),))

        if self.dry:
            for key, d in per.items():
                self.cur[key] = base[key].add(n * d)
            self.dirty |= set(per)
            self.barrier()
            return
        self.dry += 1
        set_iter(-1)
        self.known = {e: {} for e in self.ENG}
        body(0)
        self.dry -= 1
        with self.nc.Fori(0, n) as i:
            self.loops.append((lid, i))
            set_iter(0)
            self.known = {e: {} for e in self.ENG}
            body(i)
            self.loops.pop()
        for key, d in per.items():
            self.cur[key] = base[key].add(n * d)
        self.known = kn_entry
        self.dirty |= set(per)
        self.barrier()


GAMMA = [1.0 - 2.0 ** (-5.0 - h) for h in range(8)]


def make_consts():
    c = {}
    c["ident_bf"] = np.eye(128, dtype=np.float32).astype(ml_dtypes.bfloat16)
    c["ident_f"] = np.eye(128, dtype=np.float32)
    sp_, s_ = np.meshgrid(np.arange(128), np.arange(128), indexing="ij")
    c["tri"] = (sp_ > s_).astype(np.float32).astype(ml_dtypes.bfloat16)
    c["compl"] = (sp_ <= s_).astype(np.float32).astype(ml_dtypes.bfloat16)
    c["ntinc"] = (-(sp_ >= s_).astype(np.float32)).astype(ml_dtypes.bfloat16)
    c["nones"] = (-np.ones((128, 128), np.float32)).astype(ml_dtypes.bfloat16)
    m = np.zeros((128, 4, 512), np.float32)
    for r in range(4):
        s, t = np.meshgrid(np.arange(128), np.arange(512), indexing="ij")
        m[:, r, :] = (s + r * 128 < t)
    c["sbmask"] = m.astype(ml_dtypes.bfloat16)
    n = np.arange(128, dtype=np.float64)
    dm = np.zeros((128, 8, 128), np.float64)
    xi = np.zeros((128, 8), np.float64)
    zs = np.zeros((128, 8), np.float64)
    for h in range(8):
        g = GAMMA[h]
        dm[:, h, :] = np.where(n[None, :] >= n[:, None], g ** (-(n[:, None] + 1.0)), 0.0)
        xi[:, h] = g ** (n + 1.0)
        zs[:, h] = g ** (127.0 - n) * (128.0 ** -0.5)
    c["dmaskT"] = dm.astype(np.float32)
    c["xi"] = xi.astype(np.float32)
    c["zetas"] = zs.astype(np.float32)
    inv_freq = (1.0 / (10000.0 ** (np.arange(0, 128, 2, dtype=np.float32) / 128))).astype(np.float32)
    c["invfreq"] = np.tile(inv_freq[None, :], (128, 1)).astype(np.float32)
    c["iota"] = np.arange(128, dtype=np.int32).reshape(128, 1)
    c["ones16"] = np.ones((128, 16), np.float32)
    return c


CONST_DT = {"ident_bf": BF16, "ident_f": F32, "tri": BF16, "compl": BF16, "ntinc": BF16, "nones": BF16, "sbmask": BF16,
            "dmaskT": F32, "xi": F32, "zetas": F32, "invfreq": F32, "iota": I32, "ones16": F32}

WSHAPES = {
    "ada_w": [4, 2, 1024, 3072], "ada_b": [4, 2, 3072], "ln_g": [4, 2, 1024], "ln_b": [4, 2, 1024],
    "sb_w_in": [2, 1024, 3072], "sb_w_out": [2, 1024, 1024], "ret_w_in": [2, 1024, 6144],
    "ret_gn_g": [2, 2048], "ret_w_out": [2, 2048, 1024], "router_w": [1024, 16], "router_b": [16],
    "moe_w_gate": [4, 16, 1024, 512], "moe_w_up": [4, 16, 1024, 512], "moe_w_down": [4, 16, 512, 1024],
}


class Prog:
    def __init__(self, S, dbg=False):
        self.S = S
        self.NT = S // 128
        self.nc = nc = bass.Bass("TRN2", target_bir_lowering=False)
        self.k = K(nc)
        self.dbg = dbg
        inp = lambda name, shape, dt=F32: nc.dram_tensor(name, list(shape), dt, kind="ExternalInput").ap()
        self.x_in = inp("x", [S, D])
        self.cT = inp("cT", [128, 8])
        self.posT = inp("posT", [128, self.NT], I32)
        self.W = {n: inp(n, s) for n, s in WSHAPES.items()}
        cs = make_consts()
        self.C = {n: inp("c_" + n, cs[n].shape, CONST_DT[n]) for n in cs}
        self.out = nc.dram_tensor("out", [S, D], F32, kind="ExternalOutput").ap()
        self.scr = {}
        self.moe_ne = 16
        self.moe_lvl = 3
        self.warm_n = 0
        self.moe_ns = 8 if S >= 2048 else 4

    def scratch(self, name, shape, dt):
        if name not in self.scr:
            kind = "ExternalOutput" if self.dbg else "Internal"
            self.scr[name] = self.nc.dram_tensor("s_" + name, list(shape), dt, kind=kind).ap()
        return self.scr[name]


def bc_load(P, es, name, src_row):
    t = P.k.tile(es, name, [128, src_row.shape[-1]], F32)
    P.k.dma("sp", t[:], src_row.partition_broadcast(128), w=[t])
    return t


def phase_adaln(P):
    nc, k = P.nc, P.k
    MOD = P.scratch("mod", [8, 3072], F32)
    with ExitStack() as es:
        ct = k.tile(es, "ct", [128, 8], F32)
        sc = k.tile(es, "sc", [128, 8], F32)
        Wt = k.tile(es, "adaW", [128, 8, 3072], F32)
        bias = k.tile(es, "adab", [1, 3072], F32)
        res = k.tile(es, "adar", [1, 3072], F32)
        ps = [k.ptile(es, f"adaps{i}", [1, 512]) for i in range(2)]
        k.dma("sp", ct[:], P.cT, w=[ct])
        k.op("act", lambda: nc.scalar.activation(out=sc[:], in_=ct[:], func=AF.Silu), r=[ct], w=[sc])
        for j in range(8):
            l, s = divmod(j, 2)
            k.dma("sp", Wt[:], P.W["ada_w"][l, s].rearrange("(k p) n -> p k n", p=128), w=[Wt])
            k.dma("sp", bias[:], P.W["ada_b"][l, s:s + 1, :], w=[bias])
            for n in range(6):
                p = ps[n % 2]
                for kc in range(8):
                    k.op("pe", lambda p=p, kc=kc, n=n: nc.tensor.matmul(
                        p[0:1, :], lhsT=sc[:, kc:kc + 1], rhs=Wt[:, kc, n * 512:(n + 1) * 512],
                        start=(kc == 0), stop=(kc == 7)), r=[sc, Wt], w=[p], inc=(kc == 7))
                k.op("dve", lambda p=p, n=n: nc.vector.tensor_tensor(
                    res[0:1, n * 512:(n + 1) * 512], p[0:1, :], bias[0:1, n * 512:(n + 1) * 512], ALU.add),
                    r=[p, bias], w=[res])
            k.op("dve", lambda: nc.vector.tensor_scalar_add(res[0:1, 1024:3072], res[0:1, 1024:3072], 1.0),
                 r=[res], w=[res])
            k.dma("sp", MOD[j:j + 1, :], res[0:1, :], r=[res])
    k.barrier()


def make_idx(P, es, name, bases):
    nc, k = P.nc, P.k
    n = len(bases)
    io = k.tile(es, name + "_io", [128, 1], I32)
    k.dma("sp", io[:], P.C["iota"], w=[io])
    t = k.tile(es, name, [128, n], I32)
    for j, b in enumerate(bases):
        k.op("dve", lambda j=j, b=b: nc.vector.tensor_scalar(t[:, j:j + 1], io[:], float(b), None, ALU.add),
             r=[io], w=[t])
    return t


def bump_idx(P, t, step):
    P.k.op("dve", lambda: P.nc.vector.tensor_scalar(t[:], t[:], float(step), None, ALU.add), r=[t], w=[t])


def modulate(P, xt, scale_bc, shift_bc, out_t):
    nc, k = P.nc, P.k
    k.op("dve", lambda: nc.vector.tensor_tensor(xt[:], xt[:], scale_bc[:], ALU.mult), r=[xt, scale_bc], w=[xt])
    k.op("dve", lambda: nc.vector.tensor_tensor(out_t[:], xt[:], shift_bc[:], ALU.add),
         r=[xt, shift_bc], w=[out_t])


def phase_sb_inproj(P, li, x_src):
    nc, k, S = P.nc, P.k, P.S
    NTB = S // 512
    MOD = P.scratch("mod", [8, 3072], F32)
    A = P.scratch("sbA", [NTB * 128, 24 * 512], BF16)
    B = P.scratch("sbB", [24 * 128, S], BF16)
    j = (2 * li) * 2 + 0
    with ExitStack() as es:
        Win = k.tile(es, "Win", [128, 8, 3072], BF16)
        k.dma("pool", Win[:], P.W["sb_w_in"][li].rearrange("(k p) n -> p k n", p=128), w=[Win])
        ident = k.tile(es, "ident", [128, 128], BF16)
        k.dma("sp", ident[:], P.C["ident_bf"], w=[ident])
        shift_bc = bc_load(P, es, "shift_bc", MOD[j:j + 1, 0:1024])
        scale_bc = bc_load(P, es, "scale_bc", MOD[j:j + 1, 1024:2048])
        idx = make_idx(P, es, "idx", [sub * 128 for sub in range(4)] + [0])
        xt = [k.tile(es, f"xt{u}", [128, 1024], F32) for u in range(2)]
        hb = [k.tile(es, f"hb{u}", [128, 1024], BF16) for u in range(2)]
        hT = k.tile(es, "hT", [128, 8, 512], BF16)
        tp = [k.ptile(es, f"tp{u}", [128, 8, 128], BF16) for u in range(2)]
        mm = [k.ptile(es, f"mm{u}", [128, 512]) for u in range(4)]
        obig = k.tile(es, "obig", [128, 24, 512], BF16)

        def body(tb):
            for sub in range(4):
                u = sub % 2
                k.gather(xt[u][:], x_src, idx[:, sub:sub + 1], r=[idx], w=[xt[u]])
                modulate(P, xt[u], scale_bc, shift_bc, hb[u])
                for kc in range(8):
                    k.op("pe", lambda u=u, kc=kc: nc.tensor.transpose(
                        tp[u][:, kc, :], hb[u][:, kc * 128:(kc + 1) * 128], ident[:]),
                        r=[hb[u], ident], w=[tp[u]])
                k.op("act", lambda u=u, sub=sub: nc.scalar.copy(
                    out=hT[:, :, sub * 128:(sub + 1) * 128], in_=tp[u][:]), r=[tp[u]], w=[hT])
            for oc in range(24):
                m = oc % 4
                for kc in range(8):
                    k.op("pe", lambda m=m, kc=kc, oc=oc: nc.tensor.matmul(
                        mm[m][:], lhsT=Win[:, kc, oc * 128:(oc + 1) * 128], rhs=hT[:, kc, :],
                        start=(kc == 0), stop=(kc == 7)), r=[Win, hT], w=[mm[m]], inc=(kc == 7))
                sc_ = 0.125 if oc < 8 else 1.0
                if oc % 2 == 0:
                    k.op("act", lambda m=m, oc=oc, sc_=sc_: nc.scalar.activation(
                        out=obig[:, oc, :], in_=mm[m][:], func=AF.Copy, scale=sc_), r=[mm[m]], w=[obig])
                else:
                    k.op("dve", lambda m=m, oc=oc, sc_=sc_: nc.vector.tensor_scalar(
                        obig[:, oc, :], mm[m][:], sc_, None, ALU.mult), r=[mm[m]], w=[obig])
            k.scatter(A, obig[:].rearrange("p a b -> p (a b)"), idx[:, 4:5], r=[obig, idx])
            k.op("dve", lambda: nc.vector.tensor_scalar(idx[:, 0:4], idx[:, 0:4], 512.0, None, ALU.add),
                 r=[idx], w=[idx])
            k.op("dve", lambda: nc.vector.tensor_scalar(idx[:, 4:5], idx[:, 4:5], 128.0, None, ALU.add),
                 r=[idx], w=[idx])
        k.loop(NTB, body)
        Av = A.rearrange("(tb p) (oc t) -> oc p tb t", p=128, t=512)
        Bv = B.rearrange("(oc p) (tb t) -> oc p tb t", p=128, t=512)
        for oc in range(24):
            k.dma("sp" if oc % 2 == 0 else "act", Bv[oc], Av[oc], key=f"d_rl{oc % 8}")
    k.barrier()


def phase_sb_attn(P):
    nc, k, S = P.nc, P.k, P.S
    B = P.scratch("sbB", [24 * 128, S], BF16)
    OTB = P.scratch("sbOT", [1024, S], BF16)
    Cc = P.scratch("sbC", [S, 1024], BF16)
    NT = S // 128
    NC = S // 512
    with ExitStack() as es:
        tri = k.tile(es, "tri", [128, 128], BF16)
        cpl = k.tile(es, "cpl", [128, 128], BF16)
        msk = k.tile(es, "msk", [128, 4, 512], BF16)
        ident = k.tile(es, "ident", [128, 128], BF16)
        k.dma("sp", tri[:], P.C["ntinc"], w=[tri])
        k.dma("sp", cpl[:], P.C["nones"], w=[cpl])
        k.dma("sp", msk[:], P.C["sbmask"], w=[msk])
        k.dma("sp", ident[:], P.C["ident_bf"], w=[ident])
        idx = make_idx(P, es, "idx", [0, 1024, 2048, 0])
        qT = k.tile(es, "qT", [128, S], BF16)
        kT = k.tile(es, "kT", [128, S], BF16)
        vT = k.tile(es, "vT", [128, S], BF16)
        vp = k.tile(es, "vp", [128, NT, 128], BF16)
        osb = k.tile(es, "osb", [128, S], BF16)
        Z = [[k.ptile(es, f"Z{a}{u}", [128, 512]) for u in range(3)] for a in range(2)]
        SPACC = [k.tile(es, f"SPACC{a}", [128, 512], BF16) for a in range(2)]
        OTp = [k.ptile(es, f"OTp{a}", [128, 512]) for a in range(2)]
        Et = [[k.tile(es, f"E{a}{u}", [128, 512], F32) for u in range(3)] for a in range(2)]
        SPt = [[k.tile(es, f"SP{a}{u}", [128, 512], BF16) for u in range(3)] for a in range(2)]
        At = [[k.tile(es, f"A{a}{u}", [128, 512], BF16) for u in range(3)] for a in range(2)]

        def stageA(g):
            c, j, r, u, first, last = g
            qs = slice(c * 512, (c + 1) * 512)
            for a in range(2):
                pa = slice(a * 64, (a + 1) * 64)
                k.op("pe", lambda a=a, pa=pa: nc.tensor.matmul(
                    Z[a][u][:], lhsT=kT[pa, j * 128:(j + 1) * 128], rhs=qT[pa, qs],
                    start=True, stop=False, skip_group_check=True), r=[kT, qT], w=[Z[a][u]])
            for a in range(2):
                k.op("act", lambda a=a: nc.scalar.activation(
                    out=Et[a][u][:], in_=Z[a][u][:], func=AF.Exp), r=[Z[a][u]], w=[Et[a][u]])
            for a in range(2):
                k.op("act", lambda a=a: nc.scalar.activation(
                    out=SPt[a][u][:], in_=Et[a][u][:], func=AF.Ln, bias=P.one_t[:, 0:1]),
                    r=[Et[a][u], P.one_t], w=[SPt[a][u]])
                if r is not None:
                    k.op("dve", lambda a=a: nc.vector.tensor_tensor(
                        SPt[a][u][:], SPt[a][u][:], msk[:, r, :], ALU.mult), r=[SPt[a][u], msk], w=[SPt[a][u]])

        def stageB(g):
            c, j, r, u, first, last = g
            for a in range(2):
                k.op("pe", lambda a=a: nc.tensor.matmul(
                    Z[a][u][:], lhsT=tri[:], rhs=SPt[a][u][:], start=False, stop=first, skip_group_check=True),
                    r=[tri, SPt[a][u]], w=[Z[a][u]])
                if not first:
                    k.op("pe", lambda a=a: nc.tensor.matmul(
                        Z[a][u][:], lhsT=cpl[:], rhs=SPACC[a][:], start=False, stop=True, skip_group_check=True),
                        r=[cpl, SPACC[a]], w=[Z[a][u]])
            if not last:
                for a in range(2):
                    if first:
                        k.op("pool", lambda a=a: nc.gpsimd.tensor_copy(SPACC[a][:], SPt[a][u][:]),
                             r=[SPt[a][u]], w=[SPACC[a]])
                    else:
                        k.op("pool", lambda a=a: nc.gpsimd.tensor_tensor(
                            SPACC[a][:], SPACC[a][:], SPt[a][u][:], ALU.add), r=[SPACC[a], SPt[a][u]], w=[SPACC[a]])
            for a in range(2):
                k.op("act", lambda a=a: nc.scalar.activation(
                    out=At[a][u][:], in_=Z[a][u][:], func=AF.Exp), r=[Z[a][u]], w=[At[a][u]])
                if r is not None:
                    k.op("dve", lambda a=a: nc.vector.tensor_tensor(
                        At[a][u][:], At[a][u][:], msk[:, r, :], ALU.mult), r=[At[a][u], msk], w=[At[a][u]])
            for a in range(2):
                k.op("pe", lambda a=a: nc.tensor.matmul(
                    OTp[a][:], lhsT=vp[:, j, :], rhs=At[a][u][:], start=first, stop=True, skip_group_check=True),
                    r=[vp, At[a][u]], w=[OTp[a]])

        def evac(c):
            k.op("act", lambda: nc.scalar.copy(out=osb[0:64, c * 512:(c + 1) * 512], in_=OTp[0][0:64, :]),
                 r=[OTp[0]], w=[osb])
            k.op("dve", lambda: nc.vector.tensor_copy(osb[64:128, c * 512:(c + 1) * 512], OTp[1][64:128, :]),
                 r=[OTp[1]], w=[osb])

        def pair_body(hp):
            k.gather(qT[:], B, idx[:, 0:1], r=[idx], w=[qT])
            k.gather(kT[:], B, idx[:, 1:2], r=[idx], w=[kT])
            k.gather(vT[:], B, idx[:, 2:3], r=[idx], w=[vT])
            for g in range(NT // 8):
                a = g % 2
                tpv = Z[a][0][:].bitcast(BF16).rearrange("p (j d) -> p j d", d=128)
                for jj in range(8):
                    j = g * 8 + jj
                    k.op("pe", lambda tpv=tpv, jj=jj, j=j: nc.tensor.transpose(
                        tpv[:, jj, :], vT[:, j * 128:(j + 1) * 128], ident[:]), r=[vT, ident], w=[Z[a][0]])
                k.op("act", lambda tpv=tpv, g=g: nc.scalar.copy(out=vp[:, g * 8:(g + 1) * 8, :], in_=tpv),
                     r=[Z[a][0]], w=[vp])
            groups = []
            for c in range(NC):
                js = [(4 * c + 3, 3), (4 * c + 2, 2), (4 * c + 1, 1), (4 * c, 0)] + \
                     [(j, None) for j in range(4 * c - 1, -1, -1)]
                for t, (j, r) in enumerate(js):
                    groups.append((c, j, r, len(groups) % 3, t == 0, t == len(js) - 1))
            for w_ in range(P.warm_n):
                k.op("pe", lambda: nc.tensor.matmul(Z[0][2][:], lhsT=tri[:], rhs=msk[:, 0, :], start=True, stop=True,
                                                    skip_group_check=True), r=[tri, msk], w=[Z[0][2]],
                     inc=(w_ == P.warm_n - 1))
            stageA(groups[0])
            stageA(groups[1])
            for n, g in enumerate(groups):
                if n + 2 < len(groups):
                    stageA(groups[n + 2])
                stageB(g)
                if g[5]:
                    evac(g[0])
            k.scatter(OTB, osb[:], idx[:, 3:4], r=[osb, idx])
            bump_idx(P, idx, 128)
        k.loop(8, pair_body)
        Ov = OTB.rearrange("(kc p) (i t) -> kc p i t", p=128, t=128)
        Cv = Cc.rearrange("(i p) (kc t) -> kc p i t", p=128, t=128)
        for kc in range(8):
            k.dma("sp" if kc % 2 == 0 else "act", Cv[kc], Ov[kc], key=f"d_rl{kc}")
    k.barrier()


def layer_norm_tile(P, r, st, mv, rstd, g_bc, b_bc, out_t):
    nc, k = P.nc, P.k
    for hh in range(2):
        k.op("dve", lambda hh=hh: nc.vector.bn_stats(st[:, hh, :], r[:, hh * 512:(hh + 1) * 512]),
             r=[r], w=[st])
    k.op("dve", lambda: nc.vector.bn_aggr(mv[:], st[:].rearrange("p a b -> p (a b)")), r=[st], w=[mv])
    k.op("act", lambda: nc.scalar.activation(out=rstd[:], in_=mv[:, 1:2], func=AF.Sqrt, bias=P.eps_t[:, 0:1]),
         r=[mv, P.eps_t], w=[rstd])
    k.op("dve", lambda: nc.vector.reciprocal(rstd[:], rstd[:]), r=[rstd], w=[rstd])
    k.op("dve", lambda: nc.vector.tensor_scalar(r[:], r[:], mv[:, 0:1], rstd[:, 0:1], ALU.subtract, ALU.mult),
         r=[r, mv, rstd], w=[r])
    k.op("dve", lambda: nc.vector.tensor_tensor(r[:], r[:], g_bc[:], ALU.mult), r=[r, g_bc], w=[r])
    k.op("dve", lambda: nc.vector.tensor_tensor(out_t[:], r[:], b_bc[:], ALU.add), r=[r, b_bc], w=[out_t])


def phase_post(P, A, KC, fm, w_out, modj, lng, lnb, x_src, x_dst):
    nc, k, S = P.nc, P.k, P.S
    MOD = P.scratch("mod", [8, 3072], F32)
    with ExitStack() as es:
        Wo = k.tile(es, "Wo", [128, KC, 1024], BF16)
        k.dma("pool", Wo[:], w_out.rearrange("(k p) n -> p k n", p=128), w=[Wo])
        gate_bc = bc_load(P, es, "gate_bc", MOD[modj:modj + 1, 2048:3072])
        g_bc = bc_load(P, es, "g_bc", lng)
        b_bc = bc_load(P, es, "b_bc", lnb)
        ident = k.tile(es, "ident", [128, 128], BF16)
        k.dma("sp", ident[:], P.C["ident_bf"], w=[ident])
        U = 2
        idx = make_idx(P, es, "idx", [u * 128 for u in range(U)])
        at = [k.tile(es, f"at{u}", [128, KC * 128], BF16) for u in range(U)]
        if not fm:
            gt = [k.tile(es, f"gt{u}", [128, KC * 128], BF16) for u in range(U)]
            tp = [k.ptile(es, f"tp{u}", [128, 8, 128], BF16) for u in range(U)]
        xt = [k.tile(es, f"xt{u}", [128, 1024], F32) for u in range(U)]
        rt = [k.tile(es, f"rt{u}", [128, 1024], F32) for u in range(U)]
        yo = [k.tile(es, f"yo{u}", [128, 1024], F32) for u in range(U)]
        st = [k.tile(es, f"st{u}", [128, 2, 6], F32) for u in range(U)]
        mv = [k.tile(es, f"mv{u}", [128, 2], F32) for u in range(U)]
        rstd = [k.tile(es, f"rstd{u}", [128, 1], F32) for u in range(U)]
        yp = [[k.ptile(es, f"yp{u}{h}", [128, 512]) for h in range(2)] for u in range(U)]

        def tile_ops(u):
            k.gather(xt[u][:], x_src, idx[:, u:u + 1], r=[idx], w=[xt[u]])
            if fm:
                k.gather(at[u][:], A, idx[:, u:u + 1], r=[idx], w=[at[u]])
            else:
                k.gather(gt[u][:], A, idx[:, u:u + 1], r=[idx], w=[gt[u]])
                for g8 in range(KC // 8):
                    for kc in range(8):
                        kk = g8 * 8 + kc
                        k.op("pe", lambda u=u, kc=kc, kk=kk: nc.tensor.transpose(
                            tp[u][:, kc, :], gt[u][:, kk * 128:(kk + 1) * 128], ident[:]),
                            r=[gt[u], ident], w=[tp[u]])
                    k.op("act", lambda u=u, g8=g8: nc.scalar.copy(
                        out=at[u][:, g8 * 1024:(g8 + 1) * 1024].rearrange("p (a b) -> p a b", b=128),
                        in_=tp[u][:]), r=[tp[u]], w=[at[u]])
            for h in range(2):
                for kc in range(KC):
                    k.op("pe", lambda u=u, h=h, kc=kc: nc.tensor.matmul(
                        yp[u][h][:], lhsT=at[u][:, kc * 128:(kc + 1) * 128],
                        rhs=Wo[:, kc, h * 512:(h + 1) * 512],
                        start=(kc == 0), stop=(kc == KC - 1)), r=[at[u], Wo], w=[yp[u][h]], inc=(kc == KC - 1))
            for h in range(2):
                hs = slice(h * 512, (h + 1) * 512)
                k.op("dve", lambda u=u, h=h, hs=hs: nc.vector.tensor_tensor(
                    rt[u][:, hs], yp[u][h][:], gate_bc[:, hs], ALU.mult), r=[yp[u][h], gate_bc], w=[rt[u]])
            k.op("dve", lambda u=u: nc.vector.scalar_tensor_tensor(
                rt[u][:], xt[u][:], ALPHA, rt[u][:], ALU.mult, ALU.add), r=[xt[u], rt[u]], w=[rt[u]])
            layer_norm_tile(P, rt[u], st[u], mv[u], rstd[u], g_bc, b_bc, yo[u])
            k.scatter(x_dst, yo[u][:], idx[:, u:u + 1], r=[yo[u], idx])

        def body(i):
            k.replay(*[k.record(lambda u=u: tile_ops(u)) for u in range(U)])
            bump_idx(P, idx, 128 * U)
        k.loop(S // (128 * U), body)
    k.barrier()


def phase_moe(P, li, x_src, x_dst):
    nc, k, S = P.nc, P.k, P.S
    NS = P.moe_ns
    NH = NS // 4
    NTB = S // (128 * NS)
    MOD = P.scratch("mod", [8, 3072], F32)
    j = (2 * li + 1)
    BIG = 1.0e30
    with ExitStack() as es:
        shift_bc = bc_load(P, es, "shift_bc", MOD[j:j + 1, 0:1024])
        scale_bc = bc_load(P, es, "scale_bc", MOD[j:j + 1, 1024:2048])
        gate_bc = bc_load(P, es, "gate_bc", MOD[j:j + 1, 2048:3072])
        g_bc = bc_load(P, es, "g_bc", P.W["ln_g"][li, 1:2, :])
        b_bc = bc_load(P, es, "b_bc", P.W["ln_b"][li, 1:2, :])
        rb_bc = bc_load(P, es, "rb_bc", P.W["router_b"].rearrange("(o e) -> o e", o=1))
        rw = k.tile(es, "rw", [128, 8, 16], F32)
        k.dma("sp", rw[:], P.W["router_w"].rearrange("(k p) e -> p k e", p=128), w=[rw])
        identf = k.tile(es, "identf", [128, 128], F32)
        k.dma("sp", identf[:], P.C["ident_f"], w=[identf])
        idx = make_idx(P, es, "idx", [sub * 128 for sub in range(2 * NS)])
        xs = [k.tile(es, f"xs{s_}", [128, 1024], F32) for s_ in range(2)]
        hf = [k.tile(es, f"hf{u}", [128, 1024], F32) for u in range(2)]
        hT32 = [k.tile(es, f"hT32{u}", [128, 8, 128], F32) for u in range(2)]
        hTs = [k.tile(es, f"hT{u}", [128, 8, 128 * NS], BF16) for u in range(2)]
        combs = [k.tile(es, f"comb{u}", [128, NS, 16], F32) for u in range(2)]
        yacc = [k.tile(es, f"yacc{s_}", [128, 1024], F32) for s_ in range(NS)]
        wg = [k.tile(es, f"wg{u}", [128, 8, 512], BF16) for u in range(2)]
        wu = [k.tile(es, f"wu{u}", [128, 8, 512], BF16) for u in range(2)]
        wd = [k.tile(es, f"wd{u}", [128, 4, 1024], BF16) for u in range(2)]
        sg = [k.tile(es, f"sg{u}", [128, 512], F32) for u in range(2)]
        hid = [k.tile(es, f"hid{u}", [128, 4, 512], BF16) for u in range(2)]
        sm = {n: k.tile(es, "r_" + n, [128, w_], F32) for n, w_ in
              [("lg", 16), ("mx", 1), ("nmx", 1), ("pe", 16), ("sum", 1), ("rs", 1), ("hi", 8), ("lo", 8),
               ("m1", 4), ("m2", 4), ("gs", 4), ("gm", 1), ("gmask", 4), ("ml", 16), ("pen", 16), ("v1", 1),
               ("eq1", 16), ("ml2", 16), ("v2", 1), ("eq2", 16), ("d", 1), ("w1", 1), ("w2", 1), ("t16", 16)]}
        st = [k.tile(es, f"st{u}", [128, 2, 6], F32) for u in range(2)]
        mv = [k.tile(es, f"mv{u}", [128, 2], F32) for u in range(2)]
        rstd = [k.tile(es, f"rstd{u}", [128, 1], F32) for u in range(2)]
        yo = [k.tile(es, f"yo{u}", [128, 1024], F32) for u in range(2)]
        tpf = [k.ptile(es, f"tpf{u}", [128, 4, 128], F32) for u in range(2)]
        Gp = [k.ptile(es, f"Gp{u}", [128, 512]) for u in range(2)]
        Up = [k.ptile(es, f"Up{u}", [128, 512]) for u in range(2)]
        Yp = [k.ptile(es, f"Yp{u}", [128, 512]) for u in range(2)]
        V = nc.vector

        def route(sub, lgp, comb):
            T = sm
            def dv(fn, r, w):
                k.op("dve", fn, r=[T[x] if isinstance(x, str) else x for x in r],
                     w=[T[x] if isinstance(x, str) else x for x in w])
            dv(lambda: V.tensor_tensor(T["lg"][:], lgp[:, 0, 0:16], rb_bc[:], ALU.add), [lgp, rb_bc], ["lg"])
            dv(lambda: V.reduce_max(T["mx"][:], T["lg"][:], axis=AX.X), ["lg"], ["mx"])
            dv(lambda: V.tensor_scalar(T["nmx"][:], T["mx"][:], -1.0, None, ALU.mult), ["mx"], ["nmx"])
            k.op("act", lambda: nc.scalar.activation(out=T["pe"][:], in_=T["lg"][:], func=AF.Exp,
                                                     bias=T["nmx"][:, 0:1]), r=[T["lg"], T["nmx"]], w=[T["pe"]])
            dv(lambda: V.reduce_sum(T["sum"][:], T["pe"][:], axis=AX.X), ["pe"], ["sum"])
            dv(lambda: V.reciprocal(T["rs"][:], T["sum"][:]), ["sum"], ["rs"])
            dv(lambda: V.tensor_scalar(T["pe"][:], T["pe"][:], T["rs"][:, 0:1], None, ALU.mult), ["pe", "rs"], ["pe"])
            pg = T["pe"][:].rearrange("p (g e) -> p g e", e=4)
            hi = T["hi"][:].rearrange("p (g e) -> p g e", e=2)
            lo = T["lo"][:].rearrange("p (g e) -> p g e", e=2)
            dv(lambda: V.tensor_tensor(hi, pg[:, :, 0:4:2], pg[:, :, 1:4:2], ALU.max), ["pe"], ["hi"])
            dv(lambda: V.tensor_tensor(lo, pg[:, :, 0:4:2], pg[:, :, 1:4:2], ALU.min), ["pe"], ["lo"])
            dv(lambda: V.tensor_tensor(T["m1"][:], hi[:, :, 0], hi[:, :, 1], ALU.max), ["hi"], ["m1"])
            dv(lambda: V.tensor_tensor(T["m2"][:], hi[:, :, 0], hi[:, :, 1], ALU.min), ["hi"], ["m2"])
            dv(lambda: V.tensor_tensor(T["gs"][:], lo[:, :, 0], lo[:, :, 1], ALU.max), ["lo"], ["gs"])
            dv(lambda: V.tensor_tensor(T["m2"][:], T["m2"][:], T["gs"][:], ALU.max), ["m2", "gs"], ["m2"])
            dv(lambda: V.tensor_tensor(T["gs"][:], T["m1"][:], T["m2"][:], ALU.add), ["m1", "m2"], ["gs"])
            dv(lambda: V.reduce_max(T["gm"][:], T["gs"][:], axis=AX.X), ["gs"], ["gm"])
            dv(lambda: V.tensor_scalar(T["gmask"][:], T["gs"][:], T["gm"][:, 0:1], None, ALU.is_ge),
               ["gs", "gm"], ["gmask"])
            mlv = T["ml"][:].rearrange("p (g e) -> p g e", e=4)
            penv = T["pen"][:].rearrange("p (g e) -> p g e", e=4)
            lgv = T["lg"][:].rearrange("p (g e) -> p g e", e=4)
            gmb = T["gmask"][:].unsqueeze(2).to_broadcast([128, 4, 4])
            dv(lambda: V.tensor_tensor(penv, P.ones16[:].rearrange("p (g e) -> p g e", e=4), gmb, ALU.mult),
               ["gmask", P.ones16], ["pen"])
            dv(lambda: V.tensor_scalar(T["pen"][:], T["pen"][:], -1.0, BIG, ALU.add, ALU.mult), ["pen"], ["pen"])
            dv(lambda: V.tensor_tensor(T["ml"][:], T["lg"][:], T["pen"][:], ALU.add), ["lg", "pen"], ["ml"])
            dv(lambda: V.reduce_max(T["v1"][:], T["ml"][:], axis=AX.X), ["ml"], ["v1"])
            dv(lambda: V.tensor_scalar(T["eq1"][:], T["ml"][:], T["v1"][:, 0:1], None, ALU.is_ge), ["ml", "v1"], ["eq1"])
            dv(lambda: V.scalar_tensor_tensor(T["ml2"][:], T["eq1"][:], -BIG, T["ml"][:], ALU.mult, ALU.add),
               ["eq1", "ml"], ["ml2"])
            dv(lambda: V.reduce_max(T["v2"][:], T["ml2"][:], axis=AX.X), ["ml2"], ["v2"])
            dv(lambda: V.tensor_scalar(T["eq2"][:], T["ml2"][:], T["v2"][:, 0:1], None, ALU.is_ge), ["ml2", "v2"], ["eq2"])
            dv(lambda: V.tensor_tensor(T["d"][:], T["v2"][:], T["v1"][:], ALU.subtract), ["v2", "v1"], ["d"])
            k.op("act", lambda: nc.scalar.activation(out=T["d"][:], in_=T["d"][:], func=AF.Exp), r=[T["d"]], w=[T["d"]])
            dv(lambda: V.tensor_scalar(T["w1"][:], T["d"][:], 1.0, None, ALU.add), ["d"], ["w1"])
            dv(lambda: V.reciprocal(T["w1"][:], T["w1"][:]), ["w1"], ["w1"])
            dv(lambda: V.tensor_tensor(T["w2"][:], T["d"][:], T["w1"][:], ALU.mult), ["d", "w1"], ["w2"])
            dv(lambda: V.tensor_scalar(T["t16"][:], T["eq1"][:], T["w1"][:, 0:1], None, ALU.mult), ["eq1", "w1"], ["t16"])
            dv(lambda: V.scalar_tensor_tensor(comb[:, sub, :], T["eq2"][:], T["w2"][:, 0:1], T["t16"][:],
                                              ALU.mult, ALU.add), ["eq2", "w2", "t16"], [comb])

        def load_w(e):
            u = e % 2
            k.dma("pool", wg[u][:], P.W["moe_w_gate"][li, e].rearrange("(k p) f -> p k f", p=128), w=[wg[u]])
            k.dma("pool", wu[u][:], P.W["moe_w_up"][li, e].rearrange("(k p) f -> p k f", p=128), w=[wu[u]])
            k.dma("pool", wd[u][:], P.W["moe_w_down"][li, e].rearrange("(k p) d -> p k d", p=128), w=[wd[u]])

        def prologue(bp):
            hT, comb = hTs[bp], combs[bp]
            for sub in range(NS):
                u = sub % 2
                k.gather(xs[u][:], x_src, idx[:, bp * NS + sub:bp * NS + sub + 1], r=[idx], w=[xs[u]])
                if P.moe_lvl < 1:
                    continue
                k.op("dve", lambda sub=sub, u=u: V.tensor_tensor(hf[u][:], xs[u][:], scale_bc[:], ALU.mult),
                     r=[xs[u], scale_bc], w=[hf[u]])
                k.op("dve", lambda u=u: V.tensor_tensor(hf[u][:], hf[u][:], shift_bc[:], ALU.add),
                     r=[hf[u], shift_bc], w=[hf[u]])
                for g in range(2):
                    if P.moe_lvl < 0.5:
                        continue
                    for kk in range(4):
                        kc = g * 4 + kk
                        k.op("pe", lambda g=g, kk=kk, kc=kc, u=u: nc.tensor.matmul(
                            tpf[g][:, kk, :], lhsT=hf[u][:, kc * 128:(kc + 1) * 128], rhs=identf[:],
                            start=True, stop=True), r=[hf[u], identf], w=[tpf[g]])
                    if P.moe_lvl < 0.7:
                        continue
                    k.op("act", lambda g=g, u=u: nc.scalar.copy(out=hT32[u][:, g * 4:(g + 1) * 4, :], in_=tpf[g][:]),
                         r=[tpf[g]], w=[hT32[u]])
                    if P.moe_lvl < 0.9:
                        continue
                    k.op("act", lambda g=g, sub=sub: nc.scalar.copy(
                        out=hT[:, g * 4:(g + 1) * 4, sub * 128:(sub + 1) * 128], in_=tpf[g][:]), r=[tpf[g]], w=[hT])
                lgp = tpf[0]
                if P.moe_lvl < 2:
                    continue
                for kc in range(8):
                    k.op("pe", lambda kc=kc, u=u, lgp=lgp: nc.tensor.matmul(
                        lgp[:, 0, 0:16], lhsT=hT32[u][:, kc, :], rhs=rw[:, kc, :], start=(kc == 0), stop=(kc == 7)),
                        r=[hT32[u], rw], w=[lgp], inc=(kc == 7))
                if P.moe_lvl < 3:
                    continue
                route(sub, lgp, comb)

        def experts(bp):
            hT, comb = hTs[bp], combs[bp]
            load_w(0)
            for e in range(P.moe_ne):
                u = e % 2
                if e + 1 < P.moe_ne:
                    load_w(e + 1)
                for hh in range(NH):
                    hu = (e * NH + hh) % 2
                    ts = slice(hh * 512, (hh + 1) * 512)
                    for fc in range(4):
                        pu = fc % 2
                        fs = slice(fc * 128, (fc + 1) * 128)
                        for kc in range(8):
                            k.op("pe", lambda pu=pu, kc=kc, fs=fs, u=u, ts=ts: nc.tensor.matmul(
                                Gp[pu][:], lhsT=wg[u][:, kc, fs], rhs=hT[:, kc, ts], start=(kc == 0), stop=(kc == 7)),
                                r=[wg[u], hT], w=[Gp[pu]], inc=(kc == 7))
                        for kc in range(8):
                            k.op("pe", lambda pu=pu, kc=kc, fs=fs, u=u, ts=ts: nc.tensor.matmul(
                                Up[pu][:], lhsT=wu[u][:, kc, fs], rhs=hT[:, kc, ts], start=(kc == 0), stop=(kc == 7)),
                                r=[wu[u], hT], w=[Up[pu]], inc=(kc == 7))
                        k.op("act", lambda pu=pu: nc.scalar.activation(out=sg[pu][:], in_=Gp[pu][:], func=AF.Silu),
                             r=[Gp[pu]], w=[sg[pu]])
                        k.op("dve", lambda pu=pu, fc=fc, hu=hu: V.tensor_tensor(hid[hu][:, fc, :], sg[pu][:], Up[pu][:],
                                                                               ALU.mult), r=[sg[pu], Up[pu]], w=[hid[hu]])
                    for s4 in range(4):
                        sub = hh * 4 + s4
                        for nh in range(2):
                            py = (s4 * 2 + nh) % 2
                            for fc in range(4):
                                k.op("pe", lambda py=py, fc=fc, s4=s4, nh=nh, u=u, hu=hu: nc.tensor.matmul(
                                    Yp[py][:], lhsT=hid[hu][:, fc, s4 * 128:(s4 + 1) * 128],
                                    rhs=wd[u][:, fc, nh * 512:(nh + 1) * 512], start=(fc == 0), stop=(fc == 3)),
                                    r=[hid[hu], wd[u]], w=[Yp[py]], inc=(fc == 3))
                            hs = slice(nh * 512, (nh + 1) * 512)
                            if e == 0:
                                k.op("dve", lambda py=py, sub=sub, hs=hs, e=e: V.tensor_scalar(
                                    yacc[sub][:, hs], Yp[py][:], comb[:, sub, e:e + 1], None, ALU.mult),
                                    r=[Yp[py], comb], w=[yacc[sub]])
                            else:
                                k.op("dve", lambda py=py, sub=sub, hs=hs, e=e: V.scalar_tensor_tensor(
                                    yacc[sub][:, hs], Yp[py][:], comb[:, sub, e:e + 1], yacc[sub][:, hs],
                                    ALU.mult, ALU.add), r=[Yp[py], comb, yacc[sub]], w=[yacc[sub]])

        def epilogue(bp):
            for sub in range(NS):
                u = sub % 2
                k.gather(xs[u][:], x_src, idx[:, bp * NS + sub:bp * NS + sub + 1], r=[idx], w=[xs[u]])
                k.op("dve", lambda sub=sub: V.tensor_tensor(yacc[sub][:], yacc[sub][:], gate_bc[:], ALU.mult),
                     r=[yacc[sub], gate_bc], w=[yacc[sub]])
                k.op("dve", lambda sub=sub, u=u: V.scalar_tensor_tensor(
                    yacc[sub][:], xs[u][:], ALPHA, yacc[sub][:], ALU.mult, ALU.add),
                    r=[xs[u], yacc[sub]], w=[yacc[sub]])
                layer_norm_tile(P, yacc[sub], st[u], mv[u], rstd[u], g_bc, b_bc, yo[u])
                k.scatter(x_dst, yo[u][:], idx[:, bp * NS + sub:bp * NS + sub + 1], r=[yo[u], idx])
            k.op("dve", lambda: V.tensor_scalar(idx[:, bp * NS:(bp + 1) * NS], idx[:, bp * NS:(bp + 1) * NS],
                                                float(256 * NS), None, ALU.add), r=[idx], w=[idx])

        prologue(0)

        def body(tb):
            k.replay(k.record(lambda: experts(0)), k.record(lambda: prologue(1)))
            epilogue(0)
            k.replay(k.record(lambda: experts(1)), k.record(lambda: prologue(0)))
            epilogue(1)
        k.loop(NTB // 2, body)
    k.barrier()


def phase_rope_table(P):
    nc, k, S = P.nc, P.k, P.S
    NT = S // 128
    TAB = P.scratch("rope", [S + 128, 128], F32)
    V = nc.vector
    TWO_PI = 6.283185307179586
    C1, C2 = 6.28125, TWO_PI - 6.28125
    PI_LO = 3.1415925
    with ExitStack() as es:
        pi_ = k.tile(es, "pos_i", [128, NT], I32)
        pf = k.tile(es, "pos_f", [128, NT], F32)
        inv = k.tile(es, "invf", [128, 64], F32)
        k.dma("sp", pi_[:], P.posT, w=[pi_])
        k.dma("sp", inv[:], P.C["invfreq"], w=[inv])
        k.op("dve", lambda: V.tensor_copy(pf[:], pi_[:]), r=[pi_], w=[pf])
        ang = [k.tile(es, f"ang{u}", [128, 128], F32) for u in range(2)]
        kq = [k.tile(es, f"kq{u}", [128, 128], F32) for u in range(2)]
        ki = [k.tile(es, f"ki{u}", [128, 128], I32) for u in range(2)]
        mk = [k.tile(es, f"mk{u}", [128, 128], F32) for u in range(2)]
        tb = [k.tile(es, f"tbl{u}", [128, 128], F32) for u in range(2)]
        for i in range(NT):
            u = i % 2
            a, q, qi, m, t = ang[u], kq[u], ki[u], mk[u], tb[u]
            k.op("dve", lambda a=a, i=i: V.tensor_scalar(a[:, 64:128], inv[:], pf[:, i:i + 1], None, ALU.mult),
                 r=[inv, pf], w=[a])
            k.op("dve", lambda a=a: V.tensor_scalar(a[:, 0:64], a[:, 64:128], 1.5707963267948966, None, ALU.add),
                 r=[a], w=[a])
            k.op("dve", lambda a=a, q=q: V.tensor_scalar(q[:], a[:], 1.0 / TWO_PI, None, ALU.mult), r=[a], w=[q])
            k.op("dve", lambda q=q, qi=qi: V.tensor_copy(qi[:], q[:]), r=[q], w=[qi])
            k.op("dve", lambda q=q, qi=qi: V.tensor_copy(q[:], qi[:]), r=[qi], w=[q])
            k.op("dve", lambda a=a, q=q: V.scalar_tensor_tensor(a[:], q[:], -C1, a[:], ALU.mult, ALU.add),
                 r=[a, q], w=[a])
            k.op("dve", lambda a=a, q=q: V.scalar_tensor_tensor(a[:], q[:], -C2, a[:], ALU.mult, ALU.add),
                 r=[a, q], w=[a])
            for sgn, cmp_ in ((-1.0, ALU.is_gt), (1.0, ALU.is_lt)):
                k.op("dve", lambda a=a, m=m, sgn=sgn, cmp_=cmp_: V.tensor_scalar(
                    m[:], a[:], -sgn * 3.141592653589793, None, cmp_), r=[a], w=[m])
                k.op("dve", lambda a=a, m=m, sgn=sgn: V.scalar_tensor_tensor(
                    a[:], m[:], sgn * TWO_PI, a[:], ALU.mult, ALU.add), r=[a, m], w=[a])
            k.op("dve", lambda a=a: V.tensor_scalar(a[:], a[:], PI_LO, -PI_LO, ALU.min, ALU.max), r=[a], w=[a])
            k.op("act", lambda a=a, t=t: nc.scalar.activation(out=t[:], in_=a[:], func=AF.Sin), r=[a], w=[t])
            k.dma("sp", TAB[i * 128:(i + 1) * 128, :], t[:], r=[t])
    k.barrier()


def phase_ret(P, li, x_src):
    nc, k, S = P.nc, P.k, P.S
    NT = S // 128
    MOD = P.scratch("mod", [8, 3072], F32)
    TAB = P.scratch("rope", [S + 128, 128], F32)
    G = P.scratch("retG", [S, 2048], BF16)
    j = (2 * li + 1) * 2 + 0
    V = nc.vector
    with ExitStack() as es:
        Win = k.tile(es, "Win", [128, 8, 6144], BF16)
        for part in range(4):
            k.dma("pool", Win[:, 2 * part:2 * part + 2, :],
                  P.W["ret_w_in"][li, part * 256:(part + 1) * 256, :].rearrange("(k p) n -> p k n", p=128),
                  w=[Win], key=f"d_Win{part}")
        ident = k.tile(es, "ident", [128, 128], BF16)
        k.dma("sp", ident[:], P.C["ident_bf"], w=[ident])
        shift_bc = bc_load(P, es, "shift_bc", MOD[j:j + 1, 0:1024])
        scale_bc = bc_load(P, es, "scale_bc", MOD[j:j + 1, 1024:2048])
        gn_bc = bc_load(P, es, "gn_bc", P.W["ret_gn_g"][li:li + 1, :])
        dmk = k.tile(es, "dmk", [128, 8, 128], F32)
        k.dma("sp", dmk[:], P.C["dmaskT"], w=[dmk])
        xi = k.tile(es, "xi", [128, 8], F32)
        k.dma("sp", xi[:], P.C["xi"], w=[xi])
        zs = k.tile(es, "zs", [128, 8], F32)
        k.dma("sp", zs[:], P.C["zetas"], w=[zs])
        idx = make_idx(P, es, "idx", [0, 128])
        xt = k.tile(es, "xt", [128, 1024], F32)
        hbs = [k.tile(es, f"hb{u}", [128, 1024], BF16) for u in range(2)]
        hTs = [k.tile(es, f"hT{u}", [128, 8, 128], BF16) for u in range(2)]
        css = [k.tile(es, f"cs{u}", [128, 128], F32) for u in range(2)]
        qf = k.tile(es, "qf", [128, 1024], F32)
        qr = k.tile(es, "qr", [128, 1024], F32)
        kf, kr = qf, qr
        t1 = k.tile(es, "t1", [128, 512], F32)
        t2 = k.tile(es, "t2", [128, 512], F32)
        qb = k.tile(es, "qb", [128, 1024], BF16)
        kb = k.tile(es, "kb", [128, 1024], BF16)
        kzs = [k.tile(es, f"kz{u}", [128, 1024], BF16) for u in range(2)]
        qTs = [k.tile(es, f"qT{u}", [128, 8, 128], BF16) for u in range(2)]
        kTs = [k.tile(es, f"kT{u}", [128, 8, 128], BF16) for u in range(2)]
        vbs = [k.tile(es, f"vb{u}", [128, 2048], BF16) for u in range(2)]
        sgts = [k.tile(es, f"sgt{u}", [128, 2048], BF16) for u in range(2)]
        gouts = [k.tile(es, f"gout{u}", [128, 2048], BF16) for u in range(2)]
        state = [k.tile(es, f"state{h}", [128, 256], F32) for h in range(8)]
        sbf = [k.tile(es, f"sbf{h}", [128, 256], BF16) for h in range(8)]
        Pm = [k.tile(es, f"Pm{u}", [128, 128], BF16) for u in range(2)]
        on = [k.tile(es, f"on{u}", [128, 256], F32) for u in range(2)]
        st = [k.tile(es, f"st{u}", [128, 6], F32) for u in range(2)]
        mv = [k.tile(es, f"mv{u}", [128, 2], F32) for u in range(2)]
        rstd = [k.tile(es, f"rstd{u}", [128, 1], F32) for u in range(2)]
        tp = [k.ptile(es, f"tp{u}", [128, 8, 128], BF16) for u in range(2)]
        mm = [k.ptile(es, f"mm{u}", [128, 512]) for u in range(2)]
        recA = [k.ptile(es, f"recA{u}", [128, 512]) for u in range(2)]
        recB = [k.ptile(es, f"recB{u}", [128, 512]) for u in range(2)]
        for h in range(8):
            k.op("dve", lambda h=h: V.memset(state[h][:], 0.0), w=[state[h]])
            k.op("dve", lambda h=h: V.memset(sbf[h][:], 0.0), w=[sbf[h]])

        def rotary(src, dst, cs):
            sv = src[:].rearrange("p (h t d) -> p h t d", h=8, t=2)
            dv_ = dst[:].rearrange("p (h t d) -> p h t d", h=8, t=2)
            cosb = cs[:, 0:64].unsqueeze(1).to_broadcast([128, 8, 64])
            sinb = cs[:, 64:128].unsqueeze(1).to_broadcast([128, 8, 64])
            a1 = t1[:].rearrange("p (h d) -> p h d", h=8)
            a2 = t2[:].rearrange("p (h d) -> p h d", h=8)
            k.op("dve", lambda: V.tensor_tensor(a1, sv[:, :, 0, :], cosb, ALU.mult), r=[src, cs], w=[t1])
            k.op("dve", lambda: V.tensor_tensor(a2, sv[:, :, 1, :], sinb, ALU.mult), r=[src, cs], w=[t2])
            k.op("dve", lambda: V.tensor_tensor(dv_[:, :, 0, :], a1, a2, ALU.subtract), r=[t1, t2], w=[dst])
            k.op("dve", lambda: V.tensor_tensor(a1, sv[:, :, 1, :], cosb, ALU.mult), r=[src, cs], w=[t1])
            k.op("dve", lambda: V.tensor_tensor(a2, sv[:, :, 0, :], sinb, ALU.mult), r=[src, cs], w=[t2])
            k.op("dve", lambda: V.tensor_tensor(dv_[:, :, 1, :], a1, a2, ALU.add), r=[t1, t2], w=[dst])

        def inproj(c_):
            hb, hT, cs, kz, qT, kT, vb, sgt = hbs[c_], hTs[c_], css[c_], kzs[c_], qTs[c_], kTs[c_], vbs[c_], sgts[c_]
            k.gather(xt[:], x_src, idx[:, c_:c_ + 1], r=[idx], w=[xt])
            k.gather(cs[:], TAB, idx[:, c_:c_ + 1], r=[idx], w=[cs])
            modulate(P, xt, scale_bc, shift_bc, hb)
            for kc in range(8):
                k.op("pe", lambda kc=kc: nc.tensor.transpose(tp[0][:, kc, :], hb[:, kc * 128:(kc + 1) * 128],
                                                             ident[:]), r=[hb, ident], w=[tp[0]])
            k.op("act", lambda: nc.scalar.copy(out=hT[:], in_=tp[0][:]), r=[tp[0]], w=[hT])
            def proj_chunk(n_i, n):
                m = mm[n_i % 2]
                for kc in range(8):
                    k.op("pe", lambda m=m, kc=kc, n=n: nc.tensor.matmul(
                        m[:], lhsT=hT[:, kc, :], rhs=Win[:, kc, n * 512:(n + 1) * 512],
                        start=(kc == 0), stop=(kc == 7)), r=[hT, Win], w=[m], inc=(kc == 7))
                if n < 2:
                    k.op("act", lambda m=m, n=n: nc.scalar.copy(out=qf[:, n * 512:(n + 1) * 512], in_=m[:]),
                         r=[m], w=[qf])
                elif n < 4:
                    k.op("act", lambda m=m, n=n: nc.scalar.copy(out=kf[:, (n - 2) * 512:(n - 1) * 512], in_=m[:]),
                         r=[m], w=[kf])
                elif n < 8:
                    k.op("act", lambda m=m, n=n: nc.scalar.copy(out=vb[:, (n - 4) * 512:(n - 3) * 512], in_=m[:]),
                         r=[m], w=[vb])
                else:
                    k.op("act", lambda m=m, n=n: nc.scalar.activation(
                        out=sgt[:, (n - 8) * 512:(n - 7) * 512], in_=m[:], func=AF.Silu), r=[m], w=[sgt])
            for n_i, n in enumerate([4, 5, 6, 7, 8, 9, 10, 11, 0, 1]):
                proj_chunk(n_i, n)
            rotary(qf, qr, cs)
            k.op("dve", lambda: V.tensor_tensor(qb[:].rearrange("p (h d) -> p h d", h=8),
                                                qr[:].rearrange("p (h d) -> p h d", h=8),
                                                xi[:].unsqueeze(2).to_broadcast([128, 8, 128]), ALU.mult),
                 r=[qr, xi], w=[qb])
            for n_i, n in enumerate([2, 3]):
                proj_chunk(n_i, n)
            rotary(kf, kr, cs)
            k.op("dve", lambda: V.tensor_scalar(kb[:], kr[:], 128.0 ** -0.5, None, ALU.mult), r=[kr], w=[kb])
            k.op("dve", lambda: V.tensor_tensor(kz[:].rearrange("p (h d) -> p h d", h=8),
                                                kr[:].rearrange("p (h d) -> p h d", h=8),
                                                zs[:].unsqueeze(2).to_broadcast([128, 8, 128]), ALU.mult),
                 r=[kr, zs], w=[kz])
            for h in range(8):
                k.op("pe", lambda h=h: nc.tensor.transpose(tp[0][:, h, :], qb[:, h * 128:(h + 1) * 128], ident[:]),
                     r=[qb, ident], w=[tp[0]])
            k.op("act", lambda: nc.scalar.copy(out=qT[:], in_=tp[0][:]), r=[tp[0]], w=[qT])
            for h in range(8):
                k.op("pe", lambda h=h: nc.tensor.transpose(tp[1][:, h, :], kb[:, h * 128:(h + 1) * 128], ident[:]),
                     r=[kb, ident], w=[tp[1]])
            k.op("act", lambda: nc.scalar.copy(out=kT[:], in_=tp[1][:]), r=[tp[1]], w=[kT])

        def recur(c_):
            kz, qT, kT, vb, sgt, gout = kzs[c_], qTs[c_], kTs[c_], vbs[c_], sgts[c_], gouts[c_]
            for h in range(8):
                u = h % 2
                vs = slice(h * 256, (h + 1) * 256)
                k.op("pe", lambda h=h, u=u: nc.tensor.matmul(recA[u][:, 0:128], lhsT=kT[:, h, :], rhs=qT[:, h, :],
                                                            start=True, stop=True), r=[kT, qT], w=[recA[u]])
                k.op("dve", lambda h=h, u=u: V.tensor_tensor(Pm[u][:], recA[u][:, 0:128], dmk[:, h, :], ALU.mult),
                     r=[recA[u], dmk], w=[Pm[u]])
                k.op("pe", lambda h=h, u=u, vs=vs: nc.tensor.matmul(recB[u][:, 0:256], lhsT=Pm[u][:], rhs=vb[:, vs],
                                                                   start=True, stop=False), r=[Pm[u], vb], w=[recB[u]])
                k.op("pe", lambda h=h, u=u: nc.tensor.matmul(recB[u][:, 0:256], lhsT=qT[:, h, :], rhs=sbf[h][:],
                                                            start=False, stop=True), r=[qT, sbf[h]], w=[recB[u]])
                k.op("pe", lambda h=h, u=u, vs=vs: nc.tensor.matmul(recA[u][:, 128:384],
                                                                   lhsT=kz[:, h * 128:(h + 1) * 128], rhs=vb[:, vs],
                                                                   start=True, stop=True), r=[kz, vb], w=[recA[u]])
                k.op("dve", lambda h=h, u=u: V.scalar_tensor_tensor(
                    state[h][:], state[h][:], GAMMA[h] ** 128, recA[u][:, 128:384], ALU.mult, ALU.add),
                    r=[state[h], recA[u]], w=[state[h]])
                k.op("act", lambda h=h: nc.scalar.copy(out=sbf[h][:], in_=state[h][:]), r=[state[h]], w=[sbf[h]])
                k.op("dve", lambda u=u: V.bn_stats(st[u][:], recB[u][:, 0:256]), r=[recB[u]], w=[st[u]])
                k.op("dve", lambda u=u: V.bn_aggr(mv[u][:], st[u][:]), r=[st[u]], w=[mv[u]])
                k.op("act", lambda u=u: nc.scalar.activation(out=rstd[u][:], in_=mv[u][:, 1:2], func=AF.Sqrt,
                                                             bias=P.eps_t[:, 0:1]), r=[mv[u], P.eps_t], w=[rstd[u]])
                k.op("dve", lambda u=u: V.reciprocal(rstd[u][:], rstd[u][:]), r=[rstd[u]], w=[rstd[u]])
                k.op("dve", lambda u=u: V.tensor_scalar(on[u][:], recB[u][:, 0:256], mv[u][:, 0:1], rstd[u][:, 0:1],
                                                        ALU.subtract, ALU.mult), r=[recB[u], mv[u], rstd[u]], w=[on[u]])
                k.op("dve", lambda u=u, vs=vs: V.tensor_tensor(on[u][:], on[u][:], gn_bc[:, vs], ALU.mult),
                     r=[on[u], gn_bc], w=[on[u]])
                k.op("dve", lambda u=u, vs=vs: V.tensor_tensor(gout[:, vs], on[u][:], sgt[:, vs], ALU.mult),
                     r=[on[u], sgt], w=[gout])
            k.scatter(G, gout[:], idx[:, c_:c_ + 1], r=[gout, idx])

        def bump_col(c_):
            k.op("dve", lambda: V.tensor_scalar(idx[:, c_:c_ + 1], idx[:, c_:c_ + 1], 256.0, None, ALU.add),
                 r=[idx], w=[idx])

        inproj(0)

        def body(i):
            k.replay(k.record(lambda: inproj(1)), k.record(lambda: recur(0)))
            bump_col(0)
            k.replay(k.record(lambda: inproj(0)), k.record(lambda: recur(1)))
            bump_col(1)
        k.loop(NT // 2, body)
    k.barrier()


def setup_globals(P):
    nc, k = P.nc, P.k
    def gt(name, shape, dt):
        t = Tile(name, nc.alloc_sbuf_tensor(name, list(shape), dt))
        k.tiles.append(t)
        return t
    P.one_t = gt("one_t", [128, 1], F32)
    P.eps_t = gt("eps_t", [128, 1], F32)
    P.ones16 = gt("ones16", [128, 16], F32)
    k.op("dve", lambda: nc.vector.memset(P.one_t[:], 1.0), w=[P.one_t])
    k.op("dve", lambda: nc.vector.memset(P.eps_t[:], LN_EPS), w=[P.eps_t])
    k.dma("sp", P.ones16[:], P.C["ones16"], w=[P.ones16])
    k.barrier()


def build_program(S, n_layers=DEPTH, dbg=False):
    P = Prog(S, dbg)
    setup_globals(P)
    XR = P.scratch("xr", [S + 1024, D], F32)
    phase_adaln(P)
    if n_layers > 1:
        phase_rope_table(P)
    src = P.x_in
    for l in range(n_layers):
        last = (l == n_layers - 1)
        li = l // 2
        if l % 2 == 0:
            phase_sb_inproj(P, li, src)
            phase_sb_attn(P)
            phase_post(P, P.scratch("sbC", [S, 1024], BF16), 8, True, P.W["sb_w_out"][li], 2 * l,
                       P.W["ln_g"][l, 0:1, :], P.W["ln_b"][l, 0:1, :], src, XR)
        else:
            phase_ret(P, li, src)
            phase_post(P, P.scratch("retG", [S, 2048], BF16), 16, False, P.W["ret_w_out"][li], 2 * l,
                       P.W["ln_g"][l, 0:1, :], P.W["ln_b"][l, 0:1, :], src, XR)
        src = XR
        phase_moe(P, l, XR, P.out if last else XR)
    P.k.barrier()
    return P


_CACHE = {}


def kernel(**inputs):
    S = inputs["x"].shape[1]
    B = inputs["x"].shape[0]
    if S not in _CACHE:
        _CACHE[S] = build_program(S)
    P = _CACHE[S]
    consts = make_consts()
    in_maps = []
    for b in range(B):
        m = {"x": np.ascontiguousarray(inputs["x"][b], dtype=np.float32),
             "cT": np.ascontiguousarray(np.asarray(inputs["c"][b], dtype=np.float32).reshape(8, 128).T),
             "posT": np.ascontiguousarray(np.asarray(inputs["positions"][b], dtype=np.int32).reshape(S // 128, 128).T)}
        for n in WSHAPES:
            m[n] = np.ascontiguousarray(inputs[n], dtype=np.float32)
        for n, v in consts.items():
            m["c_" + n] = v
        in_maps.append(m)
    res = run_bass_kernel_spmd(P.nc, in_maps, core_ids=list(range(B)))
    return np.stack([np.asarray(r["out"], dtype=np.float32) for r in res.results], axis=0)
```

```python
import numpy as np
import ml_dtypes
from contextlib import ExitStack
import concourse.bass as bass
import concourse.mybir as mybir
from concourse.bass_utils import run_bass_kernel_spmd

F32 = mybir.dt.float32
BF16 = mybir.dt.bfloat16
I32 = mybir.dt.int32
AF = mybir.ActivationFunctionType
ALU = mybir.AluOpType
AX = mybir.AxisListType

D = 1024
DEPTH = 4
ALPHA = (2 * DEPTH) ** 0.25
LN_EPS = 1e-5


class Aff:
    __slots__ = ("c", "k")

    def __init__(self, c=0, k=()):
        self.c = c
        self.k = tuple(k)

    def add(self, n):
        return Aff(self.c + n, self.k)

    def le(self, o):
        return self.k == o.k and self.c <= o.c


class Tile:
    def __init__(self, name, t):
        self.name = name
        self.t = t
        self.wr = None
        self.rd = {}

    def __getitem__(self, idx):
        return self.t[idx]


class K:
    ENG = ("pe", "act", "dve", "pool", "sp")

    def __init__(self, nc):
        self.nc = nc
        self.E = {"pe": nc.tensor, "act": nc.scalar, "dve": nc.vector,
                  "pool": nc.gpsimd, "sp": nc.sync}
        self.sems = {}
        self.cur = {}
        self.known = {e: {} for e in self.ENG}
        self.dirty = set()
        self.tiles = []
        self.dry = 0
        self.loops = []
        self.nloop = 0
        self.tregs = {}
        for e in ("pe", "act", "dve", "pool"):
            self._sem("e_" + e)

    def _sem(self, key):
        if key not in self.sems:
            self.sems[key] = self.nc.alloc_semaphore(key)
            self.cur[key] = Aff(0)
        return self.sems[key]

    def _treg(self, eng):
        if eng not in self.tregs:
            self.tregs[eng] = self.E[eng].alloc_register("kw_" + eng)
        return self.tregs[eng]

    def _val(self, a):
        v = a.c
        for lid, coef in a.k:
            var = [x for (l, x) in self.loops if l == lid][0]
            v = var * coef + v
        return v

    def _wait(self, eng, evs):
        for key, a in evs:
            if eng == "pe" and key == "e_pe":
                continue
            kn = self.known[eng].get(key)
            if kn is not None and a.le(kn):
                continue
            self.known[eng][key] = a
            if not self.dry:
                if a.k:
                    assert len(a.k) == 1
                    lid, coef = a.k[0]
                    var = [x for (l, x) in self.loops if l == lid][0]
                    T = self._treg(eng)
                    self.E[eng].reg_mul(T, var, coef)
                    self.E[eng].reg_add(T, T, a.c)
                    self.E[eng].wait_ge(self.sems[key], T)
                else:
                    self.E[eng].wait_ge(self.sems[key], a.c)

    def _deps(self, r, w):
        evs = []
        for t in r:
            if t.wr is not None:
                evs.append(t.wr)
        for t in w:
            if t.wr is not None:
                evs.append(t.wr)
            evs.extend(t.rd.items())
        return evs

    def _mark(self, ev, r, w):
        key, a = ev
        for t in r:
            t.rd[key] = a
        for t in w:
            t.wr = ev
            t.rd = {}

    def tile(self, es, name, shape, dt):
        self.uid = getattr(self, "uid", 0) + 1
        t = Tile(name, es.enter_context(self.nc.sbuf_tensor(f"{name}_{self.uid}", list(shape), dt)))
        self.tiles.append(t)
        return t

    def ptile(self, es, name, shape, dt=F32):
        self.uid = getattr(self, "uid", 0) + 1
        t = Tile(name, es.enter_context(self.nc.psum_tensor(f"{name}_{self.uid}", list(shape), dt)))
        self.tiles.append(t)
        return t

    def vtile(self, name):
        t = Tile(name, None)
        self.tiles.append(t)
        return t

    def op(self, eng, fn, r=(), w=(), inc=True):
        self._wait(eng, self._deps(r, w))
        if not inc:
            if not self.dry:
                fn()
            return
        key = "e_" + eng
        self.cur[key] = self.cur[key].add(1)
        self.dirty.add(key)
        if not self.dry:
            fn().then_inc(self.sems[key], 1)
        self._mark((key, self.cur[key]), r, w)

    def dma(self, q, out, in_, r=(), w=(), key=None):
        if key is None:
            key = "d_" + (w[0].name if w else r[0].name)
        self._sem(key)
        self._wait(q, self._deps(r, w))
        self.cur[key] = self.cur[key].add(16)
        self.dirty.add(key)
        if not self.dry:
            o = out() if callable(out) else out
            i = in_() if callable(in_) else in_
            self.E[q].dma_start(out=o, in_=i).then_inc(self.sems[key], 16)
        self._mark((key, self.cur[key]), r, w)

    def gather(self, out, src, idx, r=(), w=(), key=None):
        if key is None:
            key = "d_" + w[0].name
        self._sem(key)
        self._wait("pool", self._deps(r, w))
        self.cur[key] = self.cur[key].add(16)
        self.dirty.add(key)
        if not self.dry:
            self.nc.gpsimd.indirect_dma_start(
                out=out, out_offset=None, in_=src,
                in_offset=bass.IndirectOffsetOnAxis(ap=idx, axis=0)).then_inc(self.sems[key], 16)
        self._mark((key, self.cur[key]), r, w)

    def scatter(self, dst, in_, idx, r=(), w=(), key=None):
        if key is None:
            key = "d_" + r[0].name
        self._sem(key)
        self._wait("pool", self._deps(r, w))
        self.cur[key] = self.cur[key].add(16)
        self.dirty.add(key)
        if not self.dry:
            self.nc.gpsimd.indirect_dma_start(
                out=dst, out_offset=bass.IndirectOffsetOnAxis(ap=idx, axis=0), in_=in_,
                in_offset=None).then_inc(self.sems[key], 16)
        self._mark((key, self.cur[key]), r, w)

    def record(self, fn):
        rec = []
        names = ("op", "dma", "gather", "scatter")
        for nm in names:
            setattr(self, nm, (lambda nm: (lambda *a, **kw: rec.append((nm, a, kw))))(nm))
        try:
            fn()
        finally:
            for nm in names:
                delattr(self, nm)
        return rec

    def replay(self, *recs):
        pos = [0] * len(recs)
        total = sum(len(r) for r in recs)
        for _ in range(total):
            best = min((pos[i] / len(r), i) for i, r in enumerate(recs) if pos[i] < len(r))[1]
            nm, a, kw = recs[best][pos[best]]
            pos[best] += 1
            getattr(K, nm)(self, *a, **kw)

    def _clean(self):
        for t in self.tiles:
            t.wr = None
            t.rd = {}

    def barrier(self, keys=None):
        keys = sorted(self.dirty) if keys is None else sorted(keys)
        for e in self.ENG:
            for key in keys:
                self._wait(e, [(key, self.cur[key])])
        self.dirty -= set(keys)
        self._clean()

    def _release(self):
        for e in ("pe", "act", "dve", "pool"):
            h = self._sem("r_" + e)
            self.nc.sync.sem_inc(h, 1)
            self.E[e].wait_ge(h, 1)
            self.E[e].sem_clear(h)

    def _reset(self):
        for key in sorted(self.cur):
            if key.startswith("r_"):
                continue
            if self.cur[key].c:
                self.nc.sync.wait_ge(self.sems[key], self.cur[key].c)
                self.nc.sync.sem_clear(self.sems[key])
                self.cur[key] = Aff(0)
        self.dirty = set()
        self.known = {e: {} for e in self.ENG}
        self._clean()
        self._release()

    def loop_reset(self, n, body):
        assert n >= 1
        self.barrier()
        self._reset()
        with self.nc.Fori(0, n) as i:
            body(i)
            self._reset()


    def loop(self, n, body):
        assert n >= 1
        self.barrier()
        save = dict(self.cur)
        sk = {e: dict(v) for e, v in self.known.items()}
        self.dry += 1
        body(0)
        self.dry -= 1
        per = {}
        for key, a in self.cur.items():
            d = a.c - save[key].c if key in save else a.c
            if d:
                per[key] = d
        for key in list(self.cur):
            if key not in save:
                save[key] = Aff(0)
        self.cur = dict(save)
        self.known = sk
        self._clean()
        lid = self.nloop
        self.nloop += 1
        for key, d in sorted(per.items()):
            self.cur[key] = self.cur[key].add(d)
            if not self.dry:
                self.nc.sync.sem_inc(self.sems[key], d)
        for e in self.ENG:
            for key in sorted(per):
                self._wait(e, [(key, self.cur[key])])
        base = dict(self.cur)
        kn_entry = {e: dict(v) for e, v in self.known.items()}

        def set_iter(off):
            for key, d in per.items():
                b = base[key]
                self.cur[key] = Aff(b.c + off * d, b.k + ((lid, d),))

        if self.dry:
            for key, d in per.items():
                self.cur[key] = base[key].add(n * d)
            self.dirty |= set(per)
            self.barrier()
            return
        self.dry += 1
        set_iter(-1)
        self.known = {e: {} for e in self.ENG}
        body(0)
        self.dry -= 1
        with self.nc.Fori(0, n) as i:
            self.loops.append((lid, i))
            set_iter(0)
            self.known = {e: {} for e in self.ENG}
            body(i)
            self.loops.pop()
        for key, d in per.items():
            self.cur[key] = base[key].add(n * d)
        self.known = kn_entry
        self.dirty |= set(per)
        self.barrier()


GAMMA = [1.0 - 2.0 ** (-5.0 - h) for h in range(8)]


def make_consts():
    c = {}
    c["ident_bf"] = np.eye(128, dtype=np.float32).astype(ml_dtypes.bfloat16)
    c["ident_f"] = np.eye(128, dtype=np.float32)
    sp_, s_ = np.meshgrid(np.arange(128), np.arange(128), indexing="ij")
    c["tri"] = (sp_ > s_).astype(np.float32).astype(ml_dtypes.bfloat16)
    c["compl"] = (sp_ <= s_).astype(np.float32).astype(ml_dtypes.bfloat16)
    c["ntinc"] = (-(sp_ >= s_).astype(np.float32)).astype(ml_dtypes.bfloat16)
    c["nones"] = (-np.ones((128, 128), np.float32)).astype(ml_dtypes.bfloat16)
    m = np.zeros((128, 4, 512), np.float32)
    for r in range(4):
        s, t = np.meshgrid(np.arange(128), np.arange(512), indexing="ij")
        m[:, r, :] = (s + r * 128 < t)
    c["sbmask"] = m.astype(ml_dtypes.bfloat16)
    n = np.arange(128, dtype=np.float64)
    dm = np.zeros((128, 8, 128), np.float64)
    xi = np.zeros((128, 8), np.float64)
    zs = np.zeros((128, 8), np.float64)
    for h in range(8):
        g = GAMMA[h]
        dm[:, h, :] = np.where(n[None, :] >= n[:, None], g ** (-(n[:, None] + 1.0)), 0.0)
        xi[:, h] = g ** (n + 1.0)
        zs[:, h] = g ** (127.0 - n) * (128.0 ** -0.5)
    c["dmaskT"] = dm.astype(np.float32)
    c["xi"] = xi.astype(np.float32)
    c["zetas"] = zs.astype(np.float32)
    inv_freq = (1.0 / (10000.0 ** (np.arange(0, 128, 2, dtype=np.float32) / 128))).astype(np.float32)
    c["invfreq"] = np.tile(inv_freq[None, :], (128, 1)).astype(np.float32)
    c["iota"] = np.arange(128, dtype=np.int32).reshape(128, 1)
    c["ones16"] = np.ones((128, 16), np.float32)
    return c


CONST_DT = {"ident_bf": BF16, "ident_f": F32, "tri": BF16, "compl": BF16, "ntinc": BF16, "nones": BF16, "sbmask": BF16,
            "dmaskT": F32, "xi": F32, "zetas": F32, "invfreq": F32, "iota": I32, "ones16": F32}

WSHAPES = {
    "ada_w": [4, 2, 1024, 3072], "ada_b": [4, 2, 3072], "ln_g": [4, 2, 1024], "ln_b": [4, 2, 1024],
    "sb_w_in": [2, 1024, 3072], "sb_w_out": [2, 1024, 1024], "ret_w_in": [2, 1024, 6144],
    "ret_gn_g": [2, 2048], "ret_w_out": [2, 2048, 1024], "router_w": [1024, 16], "router_b": [16],
    "moe_w_gate": [4, 16, 1024, 512], "moe_w_up": [4, 16, 1024, 512], "moe_w_down": [4, 16, 512, 1024],
}


class Prog:
    def __init__(self, S, dbg=False):
        self.S = S
        self.NT = S // 128
        self.nc = nc = bass.Bass("TRN2", target_bir_lowering=False)
        self.k = K(nc)
        self.dbg = dbg
        inp = lambda name, shape, dt=F32: nc.dram_tensor(name, list(shape), dt, kind="ExternalInput").ap()
        self.x_in = inp("x", [S, D])
        self.cT = inp("cT", [128, 8])
        self.posT = inp("posT", [128, self.NT], I32)
        self.W = {n: inp(n, s) for n, s in WSHAPES.items()}
        cs = make_consts()
        self.C = {n: inp("c_" + n, cs[n].shape, CONST_DT[n]) for n in cs}
        self.out = nc.dram_tensor("out", [S, D], F32, kind="ExternalOutput").ap()
        self.scr = {}
        self.moe_ne = 16
        self.moe_lvl = 3
        self.warm_n = 0
        self.moe_ns = 8 if S >= 2048 else 4

    def scratch(self, name, shape, dt):
        if name not in self.scr:
            kind = "ExternalOutput" if self.dbg else "Internal"
            self.scr[name] = self.nc.dram_tensor("s_" + name, list(shape), dt, kind=kind).ap()
        return self.scr[name]


def bc_load(P, es, name, src_row):
    t = P.k.tile(es, name, [128, src_row.shape[-1]], F32)
    P.k.dma("sp", t[:], src_row.partition_broadcast(128), w=[t])
    return t


def phase_adaln(P):
    nc, k = P.nc, P.k
    MOD = P.scratch("mod", [8, 3072], F32)
    with ExitStack() as es:
        ct = k.tile(es, "ct", [128, 8], F32)
        sc = k.tile(es, "sc", [128, 8], F32)
        Wt = k.tile(es, "adaW", [128, 8, 3072], F32)
        bias = k.tile(es, "adab", [1, 3072], F32)
        res = k.tile(es, "adar", [1, 3072], F32)
        ps = [k.ptile(es, f"adaps{i}", [1, 512]) for i in range(2)]
        k.dma("sp", ct[:], P.cT, w=[ct])
        k.op("act", lambda: nc.scalar.activation(out=sc[:], in_=ct[:], func=AF.Silu), r=[ct], w=[sc])
        for j in range(8):
            l, s = divmod(j, 2)
            k.dma("sp", Wt[:], P.W["ada_w"][l, s].rearrange("(k p) n -> p k n", p=128), w=[Wt])
            k.dma("sp", bias[:], P.W["ada_b"][l, s:s + 1, :], w=[bias])
            for n in range(6):
                p = ps[n % 2]
                for kc in range(8):
                    k.op("pe", lambda p=p, kc=kc, n=n: nc.tensor.matmul(
                        p[0:1, :], lhsT=sc[:, kc:kc + 1], rhs=Wt[:, kc, n * 512:(n + 1) * 512],
                        start=(kc == 0), stop=(kc == 7)), r=[sc, Wt], w=[p], inc=(kc == 7))
                k.op("dve", lambda p=p, n=n: nc.vector.tensor_tensor(
                    res[0:1, n * 512:(n + 1) * 512], p[0:1, :], bias[0:1, n * 512:(n + 1) * 512], ALU.add),
                    r=[p, bias], w=[res])
            k.op("dve", lambda: nc.vector.tensor_scalar_add(res[0:1, 1024:3072], res[0:1, 1024:3072], 1.0),
                 r=[res], w=[res])
            k.dma("sp", MOD[j:j + 1, :], res[0:1, :], r=[res])
    k.barrier()


def make_idx(P, es, name, bases):
    nc, k = P.nc, P.k
    n = len(bases)
    io = k.tile(es, name + "_io", [128, 1], I32)
    k.dma("sp", io[:], P.C["iota"], w=[io])
    t = k.tile(es, name, [128, n], I32)
    for j, b in enumerate(bases):
        k.op("dve", lambda j=j, b=b: nc.vector.tensor_scalar(t[:, j:j + 1], io[:], float(b), None, ALU.add),
             r=[io], w=[t])
    return t


def bump_idx(P, t, step):
    P.k.op("dve", lambda: P.nc.vector.tensor_scalar(t[:], t[:], float(step), None, ALU.add), r=[t], w=[t])


def modulate(P, xt, scale_bc, shift_bc, out_t):
    nc, k = P.nc, P.k
    k.op("dve", lambda: nc.vector.tensor_tensor(xt[:], xt[:], scale_bc[:], ALU.mult), r=[xt, scale_bc], w=[xt])
    k.op("dve", lambda: nc.vector.tensor_tensor(out_t[:], xt[:], shift_bc[:], ALU.add),
         r=[xt, shift_bc], w=[out_t])


def phase_sb_inproj(P, li, x_src):
    nc, k, S = P.nc, P.k, P.S
    NTB = S // 512
    MOD = P.scratch("mod", [8, 3072], F32)
    A = P.scratch("sbA", [NTB * 128, 24 * 512], BF16)
    B = P.scratch("sbB", [24 * 128, S], BF16)
    j = (2 * li) * 2 + 0
    with ExitStack() as es:
        Win = k.tile(es, "Win", [128, 8, 3072], BF16)
        k.dma("pool", Win[:], P.W["sb_w_in"][li].rearrange("(k p) n -> p k n", p=128), w=[Win])
        ident = k.tile(es, "ident", [128, 128], BF16)
        k.dma("sp", ident[:], P.C["ident_bf"], w=[ident])
        shift_bc = bc_load(P, es, "shift_bc", MOD[j:j + 1, 0:1024])
        scale_bc = bc_load(P, es, "scale_bc", MOD[j:j + 1, 1024:2048])
        idx = make_idx(P, es, "idx", [sub * 128 for sub in range(4)] + [0])
        xt = [k.tile(es, f"xt{u}", [128, 1024], F32) for u in range(2)]
        hb = [k.tile(es, f"hb{u}", [128, 1024], BF16) for u in range(2)]
        hT = k.tile(es, "hT", [128, 8, 512], BF16)
        tp = [k.ptile(es, f"tp{u}", [128, 8, 128], BF16) for u in range(2)]
        mm = [k.ptile(es, f"mm{u}", [128, 512]) for u in range(4)]
        obig = k.tile(es, "obig", [128, 24, 512], BF16)

        def sub_ops(sub):
            u = sub % 2
            k.gather(xt[u][:], x_src, idx[:, sub:sub + 1], r=[idx], w=[xt[u]])
            modulate(P, xt[u], scale_bc, shift_bc, hb[u])
            for kc in range(8):
                k.op("pe", lambda u=u, kc=kc: nc.tensor.transpose(
                    tp[u][:, kc, :], hb[u][:, kc * 128:(kc + 1) * 128], ident[:]),
                    r=[hb[u], ident], w=[tp[u]])
            k.op("act", lambda u=u, sub=sub: nc.scalar.copy(
                out=hT[:, :, sub * 128:(sub + 1) * 128], in_=tp[u][:]), r=[tp[u]], w=[hT])

        def body(tb):
            k.replay(k.record(lambda: sub_ops(0)), k.record(lambda: sub_ops(1)))
            k.replay(k.record(lambda: sub_ops(2)), k.record(lambda: sub_ops(3)))
            for oc in range(24):
                m = oc % 4
                for kc in range(8):
                    k.op("pe", lambda m=m, kc=kc, oc=oc: nc.tensor.matmul(
                        mm[m][:], lhsT=Win[:, kc, oc * 128:(oc + 1) * 128], rhs=hT[:, kc, :],
                        start=(kc == 0), stop=(kc == 7)), r=[Win, hT], w=[mm[m]], inc=(kc == 7))
                sc_ = 0.125 if oc < 8 else 1.0
                if oc % 2 == 0:
                    k.op("act", lambda m=m, oc=oc, sc_=sc_: nc.scalar.activation(
                        out=obig[:, oc, :], in_=mm[m][:], func=AF.Copy, scale=sc_), r=[mm[m]], w=[obig])
                else:
                    k.op("dve", lambda m=m, oc=oc, sc_=sc_: nc.vector.tensor_scalar(
                        obig[:, oc, :], mm[m][:], sc_, None, ALU.mult), r=[mm[m]], w=[obig])
            k.scatter(A, obig[:].rearrange("p a b -> p (a b)"), idx[:, 4:5], r=[obig, idx])
            k.op("dve", lambda: nc.vector.tensor_scalar(idx[:, 0:4], idx[:, 0:4], 512.0, None, ALU.add),
                 r=[idx], w=[idx])
            k.op("dve", lambda: nc.vector.tensor_scalar(idx[:, 4:5], idx[:, 4:5], 128.0, None, ALU.add),
                 r=[idx], w=[idx])
        k.loop(NTB, body)
        Av = A.rearrange("(tb p) (oc t) -> oc p tb t", p=128, t=512)
        Bv = B.rearrange("(oc p) (tb t) -> oc p tb t", p=128, t=512)
        for oc in range(24):
            k.dma("sp" if oc % 2 == 0 else "act", Bv[oc], Av[oc], key=f"d_rl{oc % 8}")
    k.barrier()


def phase_sb_attn(P):
    nc, k, S = P.nc, P.k, P.S
    B = P.scratch("sbB", [24 * 128, S], BF16)
    OTB = P.scratch("sbOT", [1024, S], BF16)
    Cc = P.scratch("sbC", [S, 1024], BF16)
    NT = S // 128
    NC = S // 512
    with ExitStack() as es:
        tri = k.tile(es, "tri", [128, 128], BF16)
        cpl = k.tile(es, "cpl", [128, 128], BF16)
        msk = k.tile(es, "msk", [128, 4, 512], BF16)
        ident = k.tile(es, "ident", [128, 128], BF16)
        k.dma("sp", tri[:], P.C["ntinc"], w=[tri])
        k.dma("sp", cpl[:], P.C["nones"], w=[cpl])
        k.dma("sp", msk[:], P.C["sbmask"], w=[msk])
        k.dma("sp", ident[:], P.C["ident_bf"], w=[ident])
        idx = make_idx(P, es, "idx", [0, 1024, 2048, 0])
        qT = k.tile(es, "qT", [128, S], BF16)
        kT = k.tile(es, "kT", [128, S], BF16)
        vT = k.tile(es, "vT", [128, S], BF16)
        vp = k.tile(es, "vp", [128, NT, 128], BF16)
        osb = k.tile(es, "osb", [128, S], BF16)
        Z = [[k.ptile(es, f"Z{a}{u}", [128, 512]) for u in range(3)] for a in range(2)]
        SPACC = [k.tile(es, f"SPACC{a}", [128, 512], BF16) for a in range(2)]
        OTp = [k.ptile(es, f"OTp{a}", [128, 512]) for a in range(2)]
        Et = [[k.tile(es, f"E{a}{u}", [128, 512], F32) for u in range(3)] for a in range(2)]
        SPt = [[k.tile(es, f"SP{a}{u}", [128, 512], BF16) for u in range(3)] for a in range(2)]
        At = [[k.tile(es, f"A{a}{u}", [128, 512], BF16) for u in range(3)] for a in range(2)]

        def stageA(g):
            c, j, r, u, first, last = g
            qs = slice(c * 512, (c + 1) * 512)
            for a in range(2):
                pa = slice(a * 64, (a + 1) * 64)
                k.op("pe", lambda a=a, pa=pa: nc.tensor.matmul(
                    Z[a][u][:], lhsT=kT[pa, j * 128:(j + 1) * 128], rhs=qT[pa, qs],
                    start=True, stop=False, skip_group_check=True), r=[kT, qT], w=[Z[a][u]])
            for a in range(2):
                k.op("act", lambda a=a: nc.scalar.activation(
                    out=Et[a][u][:], in_=Z[a][u][:], func=AF.Exp), r=[Z[a][u]], w=[Et[a][u]])
            for a in range(2):
                k.op("act", lambda a=a: nc.scalar.activation(
                    out=SPt[a][u][:], in_=Et[a][u][:], func=AF.Ln, bias=P.one_t[:, 0:1]),
                    r=[Et[a][u], P.one_t], w=[SPt[a][u]])
                if r is not None:
                    k.op("dve", lambda a=a: nc.vector.tensor_tensor(
                        SPt[a][u][:], SPt[a][u][:], msk[:, r, :], ALU.mult), r=[SPt[a][u], msk], w=[SPt[a][u]])

        def stageB(g):
            c, j, r, u, first, last = g
            for a in range(2):
                k.op("pe", lambda a=a: nc.tensor.matmul(
                    Z[a][u][:], lhsT=tri[:], rhs=SPt[a][u][:], start=False, stop=first, skip_group_check=True),
                    r=[tri, SPt[a][u]], w=[Z[a][u]])
                if not first:
                    k.op("pe", lambda a=a: nc.tensor.matmul(
                        Z[a][u][:], lhsT=cpl[:], rhs=SPACC[a][:], start=False, stop=True, skip_group_check=True),
                        r=[cpl, SPACC[a]], w=[Z[a][u]])
            if not last:
                for a in range(2):
                    if first:
                        k.op("pool", lambda a=a: nc.gpsimd.tensor_copy(SPACC[a][:], SPt[a][u][:]),
                             r=[SPt[a][u]], w=[SPACC[a]])
                    else:
                        k.op("pool", lambda a=a: nc.gpsimd.tensor_tensor(
                            SPACC[a][:], SPACC[a][:], SPt[a][u][:], ALU.add), r=[SPACC[a], SPt[a][u]], w=[SPACC[a]])
            for a in range(2):
                k.op("act", lambda a=a: nc.scalar.activation(
                    out=At[a][u][:], in_=Z[a][u][:], func=AF.Exp), r=[Z[a][u]], w=[At[a][u]])
                if r is not None:
                    k.op("dve", lambda a=a: nc.vector.tensor_tensor(
                        At[a][u][:], At[a][u][:], msk[:, r, :], ALU.mult), r=[At[a][u], msk], w=[At[a][u]])
            for a in range(2):
                k.op("pe", lambda a=a: nc.tensor.matmul(
                    OTp[a][:], lhsT=vp[:, j, :], rhs=At[a][u][:], start=first, stop=True, skip_group_check=True),
                    r=[vp, At[a][u]], w=[OTp[a]])

        def evac(c):
            k.op("act", lambda: nc.scalar.copy(out=osb[0:64, c * 512:(c + 1) * 512], in_=OTp[0][0:64, :]),
                 r=[OTp[0]], w=[osb])
            k.op("dve", lambda: nc.vector.tensor_copy(osb[64:128, c * 512:(c + 1) * 512], OTp[1][64:128, :]),
                 r=[OTp[1]], w=[osb])

        def pair_body(hp):
            k.gather(qT[:], B, idx[:, 0:1], r=[idx], w=[qT])
            k.gather(kT[:], B, idx[:, 1:2], r=[idx], w=[kT])
            k.gather(vT[:], B, idx[:, 2:3], r=[idx], w=[vT])
            for g in range(NT // 8):
                a = g % 2
                tpv = Z[a][0][:].bitcast(BF16).rearrange("p (j d) -> p j d", d=128)
                for jj in range(8):
                    j = g * 8 + jj
                    k.op("pe", lambda tpv=tpv, jj=jj, j=j: nc.tensor.transpose(
                        tpv[:, jj, :], vT[:, j * 128:(j + 1) * 128], ident[:]), r=[vT, ident], w=[Z[a][0]])
                k.op("act", lambda tpv=tpv, g=g: nc.scalar.copy(out=vp[:, g * 8:(g + 1) * 8, :], in_=tpv),
                     r=[Z[a][0]], w=[vp])
            groups = []
            for c in range(NC):
                js = [(4 * c + 3, 3), (4 * c + 2, 2), (4 * c + 1, 1), (4 * c, 0)] + \
                     [(j, None) for j in range(4 * c - 1, -1, -1)]
                for t, (j, r) in enumerate(js):
                    groups.append((c, j, r, len(groups) % 3, t == 0, t == len(js) - 1))
            for w_ in range(P.warm_n):
                k.op("pe", lambda: nc.tensor.matmul(Z[0][2][:], lhsT=tri[:], rhs=msk[:, 0, :], start=True, stop=True,
                                                    skip_group_check=True), r=[tri, msk], w=[Z[0][2]],
                     inc=(w_ == P.warm_n - 1))
            stageA(groups[0])
            stageA(groups[1])
            for n, g in enumerate(groups):
                if n + 2 < len(groups):
                    stageA(groups[n + 2])
                stageB(g)
                if g[5]:
                    evac(g[0])
            k.scatter(OTB, osb[:], idx[:, 3:4], r=[osb, idx])
            bump_idx(P, idx, 128)
        k.loop(8, pair_body)
        Ov = OTB.rearrange("(kc p) (i t) -> kc p i t", p=128, t=128)
        Cv = Cc.rearrange("(i p) (kc t) -> kc p i t", p=128, t=128)
        for kc in range(8):
            k.dma("sp" if kc % 2 == 0 else "act", Cv[kc], Ov[kc], key=f"d_rl{kc}")
    k.barrier()


def layer_norm_tile(P, r, st, mv, rstd, g_bc, b_bc, out_t):
    nc, k = P.nc, P.k
    for hh in range(2):
        k.op("dve", lambda hh=hh: nc.vector.bn_stats(st[:, hh, :], r[:, hh * 512:(hh + 1) * 512]),
             r=[r], w=[st])
    k.op("dve", lambda: nc.vector.bn_aggr(mv[:], st[:].rearrange("p a b -> p (a b)")), r=[st], w=[mv])
    k.op("act", lambda: nc.scalar.activation(out=rstd[:], in_=mv[:, 1:2], func=AF.Sqrt, bias=P.eps_t[:, 0:1]),
         r=[mv, P.eps_t], w=[rstd])
    k.op("dve", lambda: nc.vector.reciprocal(rstd[:], rstd[:]), r=[rstd], w=[rstd])
    k.op("dve", lambda: nc.vector.tensor_scalar(r[:], r[:], mv[:, 0:1], rstd[:, 0:1], ALU.subtract, ALU.mult),
         r=[r, mv, rstd], w=[r])
    k.op("dve", lambda: nc.vector.tensor_tensor(r[:], r[:], g_bc[:], ALU.mult), r=[r, g_bc], w=[r])
    k.op("dve", lambda: nc.vector.tensor_tensor(out_t[:], r[:], b_bc[:], ALU.add), r=[r, b_bc], w=[out_t])


def phase_post(P, A, KC, fm, w_out, modj, lng, lnb, x_src, x_dst):
    nc, k, S = P.nc, P.k, P.S
    MOD = P.scratch("mod", [8, 3072], F32)
    with ExitStack() as es:
        Wo = k.tile(es, "Wo", [128, KC, 1024], BF16)
        k.dma("pool", Wo[:], w_out.rearrange("(k p) n -> p k n", p=128), w=[Wo])
        gate_bc = bc_load(P, es, "gate_bc", MOD[modj:modj + 1, 2048:3072])
        g_bc = bc_load(P, es, "g_bc", lng)
        b_bc = bc_load(P, es, "b_bc", lnb)
        ident = k.tile(es, "ident", [128, 128], BF16)
        k.dma("sp", ident[:], P.C["ident_bf"], w=[ident])
        U = 2
        idx = make_idx(P, es, "idx", [u * 128 for u in range(U)])
        at = [k.tile(es, f"at{u}", [128, KC * 128], BF16) for u in range(U)]
        if not fm:
            gt = [k.tile(es, f"gt{u}", [128, KC * 128], BF16) for u in range(U)]
            tp = [k.ptile(es, f"tp{u}", [128, 8, 128], BF16) for u in range(U)]
        xt = [k.tile(es, f"xt{u}", [128, 1024], F32) for u in range(U)]
        rt = [k.tile(es, f"rt{u}", [128, 1024], F32) for u in range(U)]
        yo = [k.tile(es, f"yo{u}", [128, 1024], F32) for u in range(U)]
        st = [k.tile(es, f"st{u}", [128, 2, 6], F32) for u in range(U)]
        mv = [k.tile(es, f"mv{u}", [128, 2], F32) for u in range(U)]
        rstd = [k.tile(es, f"rstd{u}", [128, 1], F32) for u in range(U)]
        yp = [[k.ptile(es, f"yp{u}{h}", [128, 512]) for h in range(2)] for u in range(U)]

        def tile_ops(u):
            k.gather(xt[u][:], x_src, idx[:, u:u + 1], r=[idx], w=[xt[u]])
            if fm:
                k.gather(at[u][:], A, idx[:, u:u + 1], r=[idx], w=[at[u]])
            else:
                k.gather(gt[u][:], A, idx[:, u:u + 1], r=[idx], w=[gt[u]])
                for g8 in range(KC // 8):
                    for kc in range(8):
                        kk = g8 * 8 + kc
                        k.op("pe", lambda u=u, kc=kc, kk=kk: nc.tensor.transpose(
                            tp[u][:, kc, :], gt[u][:, kk * 128:(kk + 1) * 128], ident[:]),
                            r=[gt[u], ident], w=[tp[u]])
                    k.op("act", lambda u=u, g8=g8: nc.scalar.copy(
                        out=at[u][:, g8 * 1024:(g8 + 1) * 1024].rearrange("p (a b) -> p a b", b=128),
                        in_=tp[u][:]), r=[tp[u]], w=[at[u]])
            for h in range(2):
                for kc in range(KC):
                    k.op("pe", lambda u=u, h=h, kc=kc: nc.tensor.matmul(
                        yp[u][h][:], lhsT=at[u][:, kc * 128:(kc + 1) * 128],
                        rhs=Wo[:, kc, h * 512:(h + 1) * 512],
                        start=(kc == 0), stop=(kc == KC - 1)), r=[at[u], Wo], w=[yp[u][h]], inc=(kc == KC - 1))
            for h in range(2):
                hs = slice(h * 512, (h + 1) * 512)
                k.op("dve", lambda u=u, h=h, hs=hs: nc.vector.tensor_tensor(
                    rt[u][:, hs], yp[u][h][:], gate_bc[:, hs], ALU.mult), r=[yp[u][h], gate_bc], w=[rt[u]])
            k.op("dve", lambda u=u: nc.vector.scalar_tensor_tensor(
                rt[u][:], xt[u][:], ALPHA, rt[u][:], ALU.mult, ALU.add), r=[xt[u], rt[u]], w=[rt[u]])
            layer_norm_tile(P, rt[u], st[u], mv[u], rstd[u], g_bc, b_bc, yo[u])
            k.scatter(x_dst, yo[u][:], idx[:, u:u + 1], r=[yo[u], idx])

        def body(i):
            k.replay(*[k.record(lambda u=u: tile_ops(u)) for u in range(U)])
            bump_idx(P, idx, 128 * U)
        k.loop(S // (128 * U), body)
    k.barrier()


def phase_moe(P, li, x_src, x_dst):
    nc, k, S = P.nc, P.k, P.S
    NS = P.moe_ns
    NH = NS // 4
    NTB = S // (128 * NS)
    MOD = P.scratch("mod", [8, 3072], F32)
    j = (2 * li + 1)
    BIG = 1.0e30
    with ExitStack() as es:
        shift_bc = bc_load(P, es, "shift_bc", MOD[j:j + 1, 0:1024])
        scale_bc = bc_load(P, es, "scale_bc", MOD[j:j + 1, 1024:2048])
        gate_bc = bc_load(P, es, "gate_bc", MOD[j:j + 1, 2048:3072])
        g_bc = bc_load(P, es, "g_bc", P.W["ln_g"][li, 1:2, :])
        b_bc = bc_load(P, es, "b_bc", P.W["ln_b"][li, 1:2, :])
        rb_bc = bc_load(P, es, "rb_bc", P.W["router_b"].rearrange("(o e) -> o e", o=1))
        rw = k.tile(es, "rw", [128, 8, 16], F32)
        k.dma("sp", rw[:], P.W["router_w"].rearrange("(k p) e -> p k e", p=128), w=[rw])
        identf = k.tile(es, "identf", [128, 128], F32)
        k.dma("sp", identf[:], P.C["ident_f"], w=[identf])
        idx = make_idx(P, es, "idx", [sub * 128 for sub in range(2 * NS)])
        xs = [k.tile(es, f"xs{s_}", [128, 1024], F32) for s_ in range(2)]
        hf = [k.tile(es, f"hf{u}", [128, 1024], F32) for u in range(2)]
        hT32 = [k.tile(es, f"hT32{u}", [128, 8, 128], F32) for u in range(2)]
        hTs = [k.tile(es, f"hT{u}", [128, 8, 128 * NS], BF16) for u in range(2)]
        combs = [k.tile(es, f"comb{u}", [128, NS, 16], F32) for u in range(2)]
        yacc = [k.tile(es, f"yacc{s_}", [128, 1024], F32) for s_ in range(NS)]
        wg = [k.tile(es, f"wg{u}", [128, 8, 512], BF16) for u in range(2)]
        wu = [k.tile(es, f"wu{u}", [128, 8, 512], BF16) for u in range(2)]
        wd = [k.tile(es, f"wd{u}", [128, 4, 1024], BF16) for u in range(2)]
        sg = [k.tile(es, f"sg{u}", [128, 512], F32) for u in range(2)]
        hid = [k.tile(es, f"hid{u}", [128, 4, 512], BF16) for u in range(2)]
        sm = {n: k.tile(es, "r_" + n, [128, w_], F32) for n, w_ in
              [("lg", 16), ("mx", 1), ("nmx", 1), ("pe", 16), ("sum", 1), ("rs", 1), ("hi", 8), ("lo", 8),
               ("m1", 4), ("m2", 4), ("gs", 4), ("gm", 1), ("gmask", 4), ("ml", 16), ("pen", 16), ("v1", 1),
               ("eq1", 16), ("ml2", 16), ("v2", 1), ("eq2", 16), ("d", 1), ("w1", 1), ("w2", 1), ("t16", 16)]}
        st = [k.tile(es, f"st{u}", [128, 2, 6], F32) for u in range(2)]
        mv = [k.tile(es, f"mv{u}", [128, 2], F32) for u in range(2)]
        rstd = [k.tile(es, f"rstd{u}", [128, 1], F32) for u in range(2)]
        yo = [k.tile(es, f"yo{u}", [128, 1024], F32) for u in range(2)]
        tpf = [k.ptile(es, f"tpf{u}", [128, 4, 128], F32) for u in range(2)]
        Gp = [k.ptile(es, f"Gp{u}", [128, 512]) for u in range(2)]
        Up = [k.ptile(es, f"Up{u}", [128, 512]) for u in range(2)]
        Yp = [k.ptile(es, f"Yp{u}", [128, 512]) for u in range(2)]
        V = nc.vector

        def route(sub, lgp, comb):
            T = sm
            def dv(fn, r, w):
                k.op("dve", fn, r=[T[x] if isinstance(x, str) else x for x in r],
                     w=[T[x] if isinstance(x, str) else x for x in w])
            dv(lambda: V.tensor_tensor(T["lg"][:], lgp[:, 0, 0:16], rb_bc[:], ALU.add), [lgp, rb_bc], ["lg"])
            dv(lambda: V.reduce_max(T["mx"][:], T["lg"][:], axis=AX.X), ["lg"], ["mx"])
            dv(lambda: V.tensor_scalar(T["nmx"][:], T["mx"][:], -1.0, None, ALU.mult), ["mx"], ["nmx"])
            k.op("act", lambda: nc.scalar.activation(out=T["pe"][:], in_=T["lg"][:], func=AF.Exp,
                                                     bias=T["nmx"][:, 0:1]), r=[T["lg"], T["nmx"]], w=[T["pe"]])
            dv(lambda: V.reduce_sum(T["sum"][:], T["pe"][:], axis=AX.X), ["pe"], ["sum"])
            dv(lambda: V.reciprocal(T["rs"][:], T["sum"][:]), ["sum"], ["rs"])
            dv(lambda: V.tensor_scalar(T["pe"][:], T["pe"][:], T["rs"][:, 0:1], None, ALU.mult), ["pe", "rs"], ["pe"])
            pg = T["pe"][:].rearrange("p (g e) -> p g e", e=4)
            hi = T["hi"][:].rearrange("p (g e) -> p g e", e=2)
            lo = T["lo"][:].rearrange("p (g e) -> p g e", e=2)
            dv(lambda: V.tensor_tensor(hi, pg[:, :, 0:4:2], pg[:, :, 1:4:2], ALU.max), ["pe"], ["hi"])
            dv(lambda: V.tensor_tensor(lo, pg[:, :, 0:4:2], pg[:, :, 1:4:2], ALU.min), ["pe"], ["lo"])
            dv(lambda: V.tensor_tensor(T["m1"][:], hi[:, :, 0], hi[:, :, 1], ALU.max), ["hi"], ["m1"])
            dv(lambda: V.tensor_tensor(T["m2"][:], hi[:, :, 0], hi[:, :, 1], ALU.min), ["hi"], ["m2"])
            dv(lambda: V.tensor_tensor(T["gs"][:], lo[:, :, 0], lo[:, :, 1], ALU.max), ["lo"], ["gs"])
            dv(lambda: V.tensor_tensor(T["m2"][:], T["m2"][:], T["gs"][:], ALU.max), ["m2", "gs"], ["m2"])
            dv(lambda: V.tensor_tensor(T["gs"][:], T["m1"][:], T["m2"][:], ALU.add), ["m1", "m2"], ["gs"])
            dv(lambda: V.reduce_max(T["gm"][:], T["gs"][:], axis=AX.X), ["gs"], ["gm"])
            dv(lambda: V.tensor_scalar(T["gmask"][:], T["gs"][:], T["gm"][:, 0:1], None, ALU.is_ge),
               ["gs", "gm"], ["gmask"])
            mlv = T["ml"][:].rearrange("p (g e) -> p g e", e=4)
            penv = T["pen"][:].rearrange("p (g e) -> p g e", e=4)
            lgv = T["lg"][:].rearrange("p (g e) -> p g e", e=4)
            gmb = T["gmask"][:].unsqueeze(2).to_broadcast([128, 4, 4])
            dv(lambda: V.tensor_tensor(penv, P.ones16[:].rearrange("p (g e) -> p g e", e=4), gmb, ALU.mult),
               ["gmask", P.ones16], ["pen"])
            dv(lambda: V.tensor_scalar(T["pen"][:], T["pen"][:], -1.0, BIG, ALU.add, ALU.mult), ["pen"], ["pen"])
            dv(lambda: V.tensor_tensor(T["ml"][:], T["lg"][:], T["pen"][:], ALU.add), ["lg", "pen"], ["ml"])
            dv(lambda: V.reduce_max(T["v1"][:], T["ml"][:], axis=AX.X), ["ml"], ["v1"])
            dv(lambda: V.tensor_scalar(T["eq1"][:], T["ml"][:], T["v1"][:, 0:1], None, ALU.is_ge), ["ml", "v1"], ["eq1"])
            dv(lambda: V.scalar_tensor_tensor(T["ml2"][:], T["eq1"][:], -BIG, T["ml"][:], ALU.mult, ALU.add),
               ["eq1", "ml"], ["ml2"])
            dv(lambda: V.reduce_max(T["v2"][:], T["ml2"][:], axis=AX.X), ["ml2"], ["v2"])
            dv(lambda: V.tensor_scalar(T["eq2"][:], T["ml2"][:], T["v2"][:, 0:1], None, ALU.is_ge), ["ml2", "v2"], ["eq2"])
            dv(lambda: V.tensor_tensor(T["d"][:], T["v2"][:], T["v1"][:], ALU.subtract), ["v2", "v1"], ["d"])
            k.op("act", lambda: nc.scalar.activation(out=T["d"][:], in_=T["d"][:], func=AF.Exp), r=[T["d"]], w=[T["d"]])
            dv(lambda: V.tensor_scalar(T["w1"][:], T["d"][:], 1.0, None, ALU.add), ["d"], ["w1"])
            dv(lambda: V.reciprocal(T["w1"][:], T["w1"][:]), ["w1"], ["w1"])
            dv(lambda: V.tensor_tensor(T["w2"][:], T["d"][:], T["w1"][:], ALU.mult), ["d", "w1"], ["w2"])
            dv(lambda: V.tensor_scalar(T["t16"][:], T["eq1"][:], T["w1"][:, 0:1], None, ALU.mult), ["eq1", "w1"], ["t16"])
            dv(lambda: V.scalar_tensor_tensor(comb[:, sub, :], T["eq2"][:], T["w2"][:, 0:1], T["t16"][:],
                                              ALU.mult, ALU.add), ["eq2", "w2", "t16"], [comb])

        def load_w(e):
            u = e % 2
            k.dma("pool", wg[u][:], P.W["moe_w_gate"][li, e].rearrange("(k p) f -> p k f", p=128), w=[wg[u]])
            k.dma("pool", wu[u][:], P.W["moe_w_up"][li, e].rearrange("(k p) f -> p k f", p=128), w=[wu[u]])
            k.dma("pool", wd[u][:], P.W["moe_w_down"][li, e].rearrange("(k p) d -> p k d", p=128), w=[wd[u]])

        def prologue(bp):
            hT, comb = hTs[bp], combs[bp]
            for sub in range(NS):
                u = sub % 2
                k.gather(xs[u][:], x_src, idx[:, bp * NS + sub:bp * NS + sub + 1], r=[idx], w=[xs[u]])
                if P.moe_lvl < 1:
                    continue
                k.op("dve", lambda sub=sub, u=u: V.tensor_tensor(hf[u][:], xs[u][:], scale_bc[:], ALU.mult),
                     r=[xs[u], scale_bc], w=[hf[u]])
                k.op("dve", lambda u=u: V.tensor_tensor(hf[u][:], hf[u][:], shift_bc[:], ALU.add),
                     r=[hf[u], shift_bc], w=[hf[u]])
                for g in range(2):
                    if P.moe_lvl < 0.5:
                        continue
                    for kk in range(4):
                        kc = g * 4 + kk
                        k.op("pe", lambda g=g, kk=kk, kc=kc, u=u: nc.tensor.matmul(
                            tpf[g][:, kk, :], lhsT=hf[u][:, kc * 128:(kc + 1) * 128], rhs=identf[:],
                            start=True, stop=True), r=[hf[u], identf], w=[tpf[g]])
                    if P.moe_lvl < 0.7:
                        continue
                    k.op("act", lambda g=g, u=u: nc.scalar.copy(out=hT32[u][:, g * 4:(g + 1) * 4, :], in_=tpf[g][:]),
                         r=[tpf[g]], w=[hT32[u]])
                    if P.moe_lvl < 0.9:
                        continue
                    k.op("act", lambda g=g, sub=sub: nc.scalar.copy(
                        out=hT[:, g * 4:(g + 1) * 4, sub * 128:(sub + 1) * 128], in_=tpf[g][:]), r=[tpf[g]], w=[hT])
                lgp = tpf[0]
                if P.moe_lvl < 2:
                    continue
                for kc in range(8):
                    k.op("pe", lambda kc=kc, u=u, lgp=lgp: nc.tensor.matmul(
                        lgp[:, 0, 0:16], lhsT=hT32[u][:, kc, :], rhs=rw[:, kc, :], start=(kc == 0), stop=(kc == 7)),
                        r=[hT32[u], rw], w=[lgp], inc=(kc == 7))
                if P.moe_lvl < 3:
                    continue
                route(sub, lgp, comb)

        def experts(bp):
            hT, comb = hTs[bp], combs[bp]
            load_w(0)
            for e in range(P.moe_ne):
                u = e % 2
                if e + 1 < P.moe_ne:
                    load_w(e + 1)
                for hh in range(NH):
                    hu = (e * NH + hh) % 2
                    ts = slice(hh * 512, (hh + 1) * 512)
                    for fc in range(4):
                        pu = fc % 2
                        fs = slice(fc * 128, (fc + 1) * 128)
                        for kc in range(8):
                            k.op("pe", lambda pu=pu, kc=kc, fs=fs, u=u, ts=ts: nc.tensor.matmul(
                                Gp[pu][:], lhsT=wg[u][:, kc, fs], rhs=hT[:, kc, ts], start=(kc == 0), stop=(kc == 7)),
                                r=[wg[u], hT], w=[Gp[pu]], inc=(kc == 7))
                        for kc in range(8):
                            k.op("pe", lambda pu=pu, kc=kc, fs=fs, u=u, ts=ts: nc.tensor.matmul(
                                Up[pu][:], lhsT=wu[u][:, kc, fs], rhs=hT[:, kc, ts], start=(kc == 0), stop=(kc == 7)),
                                r=[wu[u], hT], w=[Up[pu]], inc=(kc == 7))
                        k.op("act", lambda pu=pu: nc.scalar.activation(out=sg[pu][:], in_=Gp[pu][:], func=AF.Silu),
                             r=[Gp[pu]], w=[sg[pu]])
                        k.op("dve", lambda pu=pu, fc=fc, hu=hu: V.tensor_tensor(hid[hu][:, fc, :], sg[pu][:], Up[pu][:],
                                                                               ALU.mult), r=[sg[pu], Up[pu]], w=[hid[hu]])
                    for s4 in range(4):
                        sub = hh * 4 + s4
                        for nh in range(2):
                            py = (s4 * 2 + nh) % 2
                            for fc in range(4):
                                k.op("pe", lambda py=py, fc=fc, s4=s4, nh=nh, u=u, hu=hu: nc.tensor.matmul(
                                    Yp[py][:], lhsT=hid[hu][:, fc, s4 * 128:(s4 + 1) * 128],
                                    rhs=wd[u][:, fc, nh * 512:(nh + 1) * 512], start=(fc == 0), stop=(fc == 3)),
                                    r=[hid[hu], wd[u]], w=[Yp[py]], inc=(fc == 3))
                            hs = slice(nh * 512, (nh + 1) * 512)
                            if e == 0:
                                k.op("dve", lambda py=py, sub=sub, hs=hs, e=e: V.tensor_scalar(
                                    yacc[sub][:, hs], Yp[py][:], comb[:, sub, e:e + 1], None, ALU.mult),
                                    r=[Yp[py], comb], w=[yacc[sub]])
                            else:
                                k.op("dve", lambda py=py, sub=sub, hs=hs, e=e: V.scalar_tensor_tensor(
                                    yacc[sub][:, hs], Yp[py][:], comb[:, sub, e:e + 1], yacc[sub][:, hs],
                                    ALU.mult, ALU.add), r=[Yp[py], comb, yacc[sub]], w=[yacc[sub]])

        def epilogue(bp):
            def ep_sub(sub):
                u = sub % 2
                k.gather(xs[u][:], x_src, idx[:, bp * NS + sub:bp * NS + sub + 1], r=[idx], w=[xs[u]])
                k.op("dve", lambda sub=sub: V.tensor_tensor(yacc[sub][:], yacc[sub][:], gate_bc[:], ALU.mult),
                     r=[yacc[sub], gate_bc], w=[yacc[sub]])
                k.op("dve", lambda sub=sub, u=u: V.scalar_tensor_tensor(
                    yacc[sub][:], xs[u][:], ALPHA, yacc[sub][:], ALU.mult, ALU.add),
                    r=[xs[u], yacc[sub]], w=[yacc[sub]])
                layer_norm_tile(P, yacc[sub], st[u], mv[u], rstd[u], g_bc, b_bc, yo[u])
                k.scatter(x_dst, yo[u][:], idx[:, bp * NS + sub:bp * NS + sub + 1], r=[yo[u], idx])
            for s2 in range(0, NS, 2):
                k.replay(k.record(lambda: ep_sub(s2)), k.record(lambda: ep_sub(s2 + 1)))
            k.op("dve", lambda: V.tensor_scalar(idx[:, bp * NS:(bp + 1) * NS], idx[:, bp * NS:(bp + 1) * NS],
                                                float(256 * NS), None, ALU.add), r=[idx], w=[idx])

        prologue(0)

        def body(tb):
            k.replay(k.record(lambda: experts(0)), k.record(lambda: prologue(1)))
            epilogue(0)
            k.replay(k.record(lambda: experts(1)), k.record(lambda: prologue(0)))
            epilogue(1)
        k.loop(NTB // 2, body)
    k.barrier()


def phase_rope_table(P):
    nc, k, S = P.nc, P.k, P.S
    NT = S // 128
    TAB = P.scratch("rope", [S + 128, 128], F32)
    V = nc.vector
    TWO_PI = 6.283185307179586
    C1, C2 = 6.28125, TWO_PI - 6.28125
    PI_LO = 3.1415925
    with ExitStack() as es:
        pi_ = k.tile(es, "pos_i", [128, NT], I32)
        pf = k.tile(es, "pos_f", [128, NT], F32)
        inv = k.tile(es, "invf", [128, 64], F32)
        k.dma("sp", pi_[:], P.posT, w=[pi_])
        k.dma("sp", inv[:], P.C["invfreq"], w=[inv])
        k.op("dve", lambda: V.tensor_copy(pf[:], pi_[:]), r=[pi_], w=[pf])
        ang = [k.tile(es, f"ang{u}", [128, 128], F32) for u in range(2)]
        kq = [k.tile(es, f"kq{u}", [128, 128], F32) for u in range(2)]
        ki = [k.tile(es, f"ki{u}", [128, 128], I32) for u in range(2)]
        mk = [k.tile(es, f"mk{u}", [128, 128], F32) for u in range(2)]
        tb = [k.tile(es, f"tbl{u}", [128, 128], F32) for u in range(2)]
        for i in range(NT):
            u = i % 2
            a, q, qi, m, t = ang[u], kq[u], ki[u], mk[u], tb[u]
            k.op("dve", lambda a=a, i=i: V.tensor_scalar(a[:, 64:128], inv[:], pf[:, i:i + 1], None, ALU.mult),
                 r=[inv, pf], w=[a])
            k.op("dve", lambda a=a: V.tensor_scalar(a[:, 0:64], a[:, 64:128], 1.5707963267948966, None, ALU.add),
                 r=[a], w=[a])
            k.op("dve", lambda a=a, q=q: V.tensor_scalar(q[:], a[:], 1.0 / TWO_PI, None, ALU.mult), r=[a], w=[q])
            k.op("dve", lambda q=q, qi=qi: V.tensor_copy(qi[:], q[:]), r=[q], w=[qi])
            k.op("dve", lambda q=q, qi=qi: V.tensor_copy(q[:], qi[:]), r=[qi], w=[q])
            k.op("dve", lambda a=a, q=q: V.scalar_tensor_tensor(a[:], q[:], -C1, a[:], ALU.mult, ALU.add),
                 r=[a, q], w=[a])
            k.op("dve", lambda a=a, q=q: V.scalar_tensor_tensor(a[:], q[:], -C2, a[:], ALU.mult, ALU.add),
                 r=[a, q], w=[a])
            for sgn, cmp_ in ((-1.0, ALU.is_gt), (1.0, ALU.is_lt)):
                k.op("dve", lambda a=a, m=m, sgn=sgn, cmp_=cmp_: V.tensor_scalar(
                    m[:], a[:], -sgn * 3.141592653589793, None, cmp_), r=[a], w=[m])
                k.op("dve", lambda a=a, m=m, sgn=sgn: V.scalar_tensor_tensor(
                    a[:], m[:], sgn * TWO_PI, a[:], ALU.mult, ALU.add), r=[a, m], w=[a])
            k.op("dve", lambda a=a: V.tensor_scalar(a[:], a[:], PI_LO, -PI_LO, ALU.min, ALU.max), r=[a], w=[a])
            k.op("act", lambda a=a, t=t: nc.scalar.activation(out=t[:], in_=a[:], func=AF.Sin), r=[a], w=[t])
            k.dma("sp", TAB[i * 128:(i + 1) * 128, :], t[:], r=[t])
    k.barrier()


def phase_ret(P, li, x_src):
    nc, k, S = P.nc, P.k, P.S
    NT = S // 128
    MOD = P.scratch("mod", [8, 3072], F32)
    TAB = P.scratch("rope", [S + 128, 128], F32)
    G = P.scratch("retG", [S, 2048], BF16)
    j = (2 * li + 1) * 2 + 0
    V = nc.vector
    with ExitStack() as es:
        Win = k.tile(es, "Win", [128, 8, 6144], BF16)
        for part in range(4):
            k.dma("pool", Win[:, 2 * part:2 * part + 2, :],
                  P.W["ret_w_in"][li, part * 256:(part + 1) * 256, :].rearrange("(k p) n -> p k n", p=128),
                  w=[Win], key=f"d_Win{part}")
        ident = k.tile(es, "ident", [128, 128], BF16)
        k.dma("sp", ident[:], P.C["ident_bf"], w=[ident])
        shift_bc = bc_load(P, es, "shift_bc", MOD[j:j + 1, 0:1024])
        scale_bc = bc_load(P, es, "scale_bc", MOD[j:j + 1, 1024:2048])
        gn_bc = bc_load(P, es, "gn_bc", P.W["ret_gn_g"][li:li + 1, :])
        dmk = k.tile(es, "dmk", [128, 8, 128], F32)
        k.dma("sp", dmk[:], P.C["dmaskT"], w=[dmk])
        xi = k.tile(es, "xi", [128, 8], F32)
        k.dma("sp", xi[:], P.C["xi"], w=[xi])
        zs = k.tile(es, "zs", [128, 8], F32)
        k.dma("sp", zs[:], P.C["zetas"], w=[zs])
        idx = make_idx(P, es, "idx", [0, 128])
        xt = k.tile(es, "xt", [128, 1024], F32)
        hbs = [k.tile(es, f"hb{u}", [128, 1024], BF16) for u in range(2)]
        hTs = [k.tile(es, f"hT{u}", [128, 8, 128], BF16) for u in range(2)]
        css = [k.tile(es, f"cs{u}", [128, 128], F32) for u in range(2)]
        qf = k.tile(es, "qf", [128, 1024], F32)
        qr = k.tile(es, "qr", [128, 1024], F32)
        kf, kr = qf, qr
        t1 = k.tile(es, "t1", [128, 512], F32)
        t2 = k.tile(es, "t2", [128, 512], F32)
        qb = k.tile(es, "qb", [128, 1024], BF16)
        kb = k.tile(es, "kb", [128, 1024], BF16)
        kzs = [k.tile(es, f"kz{u}", [128, 1024], BF16) for u in range(2)]
        qTs = [k.tile(es, f"qT{u}", [128, 8, 128], BF16) for u in range(2)]
        kTs = [k.tile(es, f"kT{u}", [128, 8, 128], BF16) for u in range(2)]
        vbs = [k.tile(es, f"vb{u}", [128, 2048], BF16) for u in range(2)]
        sgts = [k.tile(es, f"sgt{u}", [128, 2048], BF16) for u in range(2)]
        gouts = [k.tile(es, f"gout{u}", [128, 2048], BF16) for u in range(2)]
        state = [k.tile(es, f"state{h}", [128, 256], F32) for h in range(8)]
        sbf = [k.tile(es, f"sbf{h}", [128, 256], BF16) for h in range(8)]
        Pm = [k.tile(es, f"Pm{u}", [128, 128], BF16) for u in range(2)]
        on = [k.tile(es, f"on{u}", [128, 256], F32) for u in range(2)]
        st = [k.tile(es, f"st{u}", [128, 6], F32) for u in range(2)]
        mv = [k.tile(es, f"mv{u}", [128, 2], F32) for u in range(2)]
        rstd = [k.tile(es, f"rstd{u}", [128, 1], F32) for u in range(2)]
        tp = [k.ptile(es, f"tp{u}", [128, 8, 128], BF16) for u in range(2)]
        mm = [k.ptile(es, f"mm{u}", [128, 512]) for u in range(2)]
        recA = [k.ptile(es, f"recA{u}", [128, 512]) for u in range(2)]
        recB = [k.ptile(es, f"recB{u}", [128, 512]) for u in range(2)]
        for h in range(8):
            k.op("dve", lambda h=h: V.memset(state[h][:], 0.0), w=[state[h]])
            k.op("dve", lambda h=h: V.memset(sbf[h][:], 0.0), w=[sbf[h]])

        def rotary(src, dst, cs):
            sv = src[:].rearrange("p (h t d) -> p h t d", h=8, t=2)
            dv_ = dst[:].rearrange("p (h t d) -> p h t d", h=8, t=2)
            cosb = cs[:, 0:64].unsqueeze(1).to_broadcast([128, 8, 64])
            sinb = cs[:, 64:128].unsqueeze(1).to_broadcast([128, 8, 64])
            a1 = t1[:].rearrange("p (h d) -> p h d", h=8)
            a2 = t2[:].rearrange("p (h d) -> p h d", h=8)
            k.op("dve", lambda: V.tensor_tensor(a1, sv[:, :, 0, :], cosb, ALU.mult), r=[src, cs], w=[t1])
            k.op("dve", lambda: V.tensor_tensor(a2, sv[:, :, 1, :], sinb, ALU.mult), r=[src, cs], w=[t2])
            k.op("dve", lambda: V.tensor_tensor(dv_[:, :, 0, :], a1, a2, ALU.subtract), r=[t1, t2], w=[dst])
            k.op("dve", lambda: V.tensor_tensor(a1, sv[:, :, 1, :], cosb, ALU.mult), r=[src, cs], w=[t1])
            k.op("dve", lambda: V.tensor_tensor(a2, sv[:, :, 0, :], sinb, ALU.mult), r=[src, cs], w=[t2])
            k.op("dve", lambda: V.tensor_tensor(dv_[:, :, 1, :], a1, a2, ALU.add), r=[t1, t2], w=[dst])

        def inproj(c_):
            hb, hT, cs, kz, qT, kT, vb, sgt = hbs[c_], hTs[c_], css[c_], kzs[c_], qTs[c_], kTs[c_], vbs[c_], sgts[c_]
            k.gather(xt[:], x_src, idx[:, c_:c_ + 1], r=[idx], w=[xt])
            k.gather(cs[:], TAB, idx[:, c_:c_ + 1], r=[idx], w=[cs])
            modulate(P, xt, scale_bc, shift_bc, hb)
            for kc in range(8):
                k.op("pe", lambda kc=kc: nc.tensor.transpose(tp[0][:, kc, :], hb[:, kc * 128:(kc + 1) * 128],
                                                             ident[:]), r=[hb, ident], w=[tp[0]])
            k.op("act", lambda: nc.scalar.copy(out=hT[:], in_=tp[0][:]), r=[tp[0]], w=[hT])
            def proj_chunk(n_i, n):
                m = mm[n_i % 2]
                for kc in range(8):
                    k.op("pe", lambda m=m, kc=kc, n=n: nc.tensor.matmul(
                        m[:], lhsT=hT[:, kc, :], rhs=Win[:, kc, n * 512:(n + 1) * 512],
                        start=(kc == 0), stop=(kc == 7)), r=[hT, Win], w=[m], inc=(kc == 7))
                if n < 2:
                    k.op("act", lambda m=m, n=n: nc.scalar.copy(out=qf[:, n * 512:(n + 1) * 512], in_=m[:]),
                         r=[m], w=[qf])
                elif n < 4:
                    k.op("act", lambda m=m, n=n: nc.scalar.copy(out=kf[:, (n - 2) * 512:(n - 1) * 512], in_=m[:]),
                         r=[m], w=[kf])
                elif n < 8:
                    k.op("act", lambda m=m, n=n: nc.scalar.copy(out=vb[:, (n - 4) * 512:(n - 3) * 512], in_=m[:]),
                         r=[m], w=[vb])
                else:
                    k.op("act", lambda m=m, n=n: nc.scalar.activation(
                        out=sgt[:, (n - 8) * 512:(n - 7) * 512], in_=m[:], func=AF.Silu), r=[m], w=[sgt])
            for n_i, n in enumerate([4, 5, 6, 7, 8, 9, 10, 11, 0, 1]):
                proj_chunk(n_i, n)
            rotary(qf, qr, cs)
            k.op("dve", lambda: V.tensor_tensor(qb[:].rearrange("p (h d) -> p h d", h=8),
                                                qr[:].rearrange("p (h d) -> p h d", h=8),
                                                xi[:].unsqueeze(2).to_broadcast([128, 8, 128]), ALU.mult),
                 r=[qr, xi], w=[qb])
            for n_i, n in enumerate([2, 3]):
                proj_chunk(n_i, n)
            rotary(kf, kr, cs)
            k.op("dve", lambda: V.tensor_scalar(kb[:], kr[:], 128.0 ** -0.5, None, ALU.mult), r=[kr], w=[kb])
            k.op("dve", lambda: V.tensor_tensor(kz[:].rearrange("p (h d) -> p h d", h=8),
                                                kr[:].rearrange("p (h d) -> p h d", h=8),
                                                zs[:].unsqueeze(2).to_broadcast([128, 8, 128]), ALU.mult),
                 r=[kr, zs], w=[kz])
            for h in range(8):
                k.op("pe", lambda h=h: nc.tensor.transpose(tp[0][:, h, :], qb[:, h * 128:(h + 1) * 128], ident[:]),
                     r=[qb, ident], w=[tp[0]])
            k.op("act", lambda: nc.scalar.copy(out=qT[:], in_=tp[0][:]), r=[tp[0]], w=[qT])
            for h in range(8):
                k.op("pe", lambda h=h: nc.tensor.transpose(tp[1][:, h, :], kb[:, h * 128:(h + 1) * 128], ident[:]),
                     r=[kb, ident], w=[tp[1]])
            k.op("act", lambda: nc.scalar.copy(out=kT[:], in_=tp[1][:]), r=[tp[1]], w=[kT])

        def recur(c_):
            kz, qT, kT, vb, sgt, gout = kzs[c_], qTs[c_], kTs[c_], vbs[c_], sgts[c_], gouts[c_]
            for h in range(8):
                u = h % 2
                vs = slice(h * 256, (h + 1) * 256)
                k.op("pe", lambda h=h, u=u: nc.tensor.matmul(recA[u][:, 0:128], lhsT=kT[:, h, :], rhs=qT[:, h, :],
                                                            start=True, stop=True), r=[kT, qT], w=[recA[u]])
                k.op("dve", lambda h=h, u=u: V.tensor_tensor(Pm[u][:], recA[u][:, 0:128], dmk[:, h, :], ALU.mult),
                     r=[recA[u], dmk], w=[Pm[u]])
                k.op("pe", lambda h=h, u=u, vs=vs: nc.tensor.matmul(recB[u][:, 0:256], lhsT=Pm[u][:], rhs=vb[:, vs],
                                                                   start=True, stop=False), r=[Pm[u], vb], w=[recB[u]])
                k.op("pe", lambda h=h, u=u: nc.tensor.matmul(recB[u][:, 0:256], lhsT=qT[:, h, :], rhs=sbf[h][:],
                                                            start=False, stop=True), r=[qT, sbf[h]], w=[recB[u]])
                k.op("pe", lambda h=h, u=u, vs=vs: nc.tensor.matmul(recA[u][:, 128:384],
                                                                   lhsT=kz[:, h * 128:(h + 1) * 128], rhs=vb[:, vs],
                                                                   start=True, stop=True), r=[kz, vb], w=[recA[u]])
                k.op("dve", lambda h=h, u=u: V.scalar_tensor_tensor(
                    state[h][:], state[h][:], GAMMA[h] ** 128, recA[u][:, 128:384], ALU.mult, ALU.add),
                    r=[state[h], recA[u]], w=[state[h]])
                k.op("act", lambda h=h: nc.scalar.copy(out=sbf[h][:], in_=state[h][:]), r=[state[h]], w=[sbf[h]])
                k.op("dve", lambda u=u: V.bn_stats(st[u][:], recB[u][:, 0:256]), r=[recB[u]], w=[st[u]])
                k.op("dve", lambda u=u: V.bn_aggr(mv[u][:], st[u][:]), r=[st[u]], w=[mv[u]])
                k.op("act", lambda u=u: nc.scalar.activation(out=rstd[u][:], in_=mv[u][:, 1:2], func=AF.Sqrt,
                                                             bias=P.eps_t[:, 0:1]), r=[mv[u], P.eps_t], w=[rstd[u]])
                k.op("dve", lambda u=u: V.reciprocal(rstd[u][:], rstd[u][:]), r=[rstd[u]], w=[rstd[u]])
                k.op("dve", lambda u=u: V.tensor_scalar(on[u][:], recB[u][:, 0:256], mv[u][:, 0:1], rstd[u][:, 0:1],
                                                        ALU.subtract, ALU.mult), r=[recB[u], mv[u], rstd[u]], w=[on[u]])
                k.op("dve", lambda u=u, vs=vs: V.tensor_tensor(on[u][:], on[u][:], gn_bc[:, vs], ALU.mult),
                     r=[on[u], gn_bc], w=[on[u]])
                k.op("dve", lambda u=u, vs=vs: V.tensor_tensor(gout[:, vs], on[u][:], sgt[:, vs], ALU.mult),
                     r=[on[u], sgt], w=[gout])
            k.scatter(G, gout[:], idx[:, c_:c_ + 1], r=[gout, idx])

        def bump_col(c_):
            k.op("dve", lambda: V.tensor_scalar(idx[:, c_:c_ + 1], idx[:, c_:c_ + 1], 256.0, None, ALU.add),
                 r=[idx], w=[idx])

        inproj(0)

        def body(i):
            k.replay(k.record(lambda: inproj(1)), k.record(lambda: recur(0)))
            bump_col(0)
            k.replay(k.record(lambda: inproj(0)), k.record(lambda: recur(1)))
            bump_col(1)
        k.loop(NT // 2, body)
    k.barrier()


def setup_globals(P):
    nc, k = P.nc, P.k
    def gt(name, shape, dt):
        t = Tile(name, nc.alloc_sbuf_tensor(name, list(shape), dt))
        k.tiles.append(t)
        return t
    P.one_t = gt("one_t", [128, 1], F32)
    P.eps_t = gt("eps_t", [128, 1], F32)
    P.ones16 = gt("ones16", [128, 16], F32)
    k.op("dve", lambda: nc.vector.memset(P.one_t[:], 1.0), w=[P.one_t])
    k.op("dve", lambda: nc.vector.memset(P.eps_t[:], LN_EPS), w=[P.eps_t])
    k.dma("sp", P.ones16[:], P.C["ones16"], w=[P.ones16])
    k.barrier()


def build_program(S, n_layers=DEPTH, dbg=False):
    P = Prog(S, dbg)
    setup_globals(P)
    XR = P.scratch("xr", [S + 1024, D], F32)
    phase_adaln(P)
    if n_layers > 1:
        phase_rope_table(P)
    src = P.x_in
    for l in range(n_layers):
        last = (l == n_layers - 1)
        li = l // 2
        if l % 2 == 0:
            phase_sb_inproj(P, li, src)
            phase_sb_attn(P)
            phase_post(P, P.scratch("sbC", [S, 1024], BF16), 8, True, P.W["sb_w_out"][li], 2 * l,
                       P.W["ln_g"][l, 0:1, :], P.W["ln_b"][l, 0:1, :], src, XR)
        else:
            phase_ret(P, li, src)
            phase_post(P, P.scratch("retG", [S, 2048], BF16), 16, False, P.W["ret_w_out"][li], 2 * l,
                       P.W["ln_g"][l, 0:1, :], P.W["ln_b"][l, 0:1, :], src, XR)
        src = XR
        phase_moe(P, l, XR, P.out if last else XR)
    P.k.barrier()
    return P


_CACHE = {}


def kernel(**inputs):
    S = inputs["x"].shape[1]
    B = inputs["x"].shape[0]
    if S not in _CACHE:
        _CACHE[S] = build_program(S)
    P = _CACHE[S]
    consts = make_consts()
    in_maps = []
    for b in range(B):
        m = {"x": np.ascontiguousarray(inputs["x"][b], dtype=np.float32),
             "cT": np.ascontiguousarray(np.asarray(inputs["c"][b], dtype=np.float32).reshape(8, 128).T),
             "posT": np.ascontiguousarray(np.asarray(inputs["positions"][b], dtype=np.int32).reshape(S // 128, 128).T)}
        for n in WSHAPES:
            m[n] = np.ascontiguousarray(inputs[n], dtype=np.float32)
        for n, v in consts.items():
            m["c_" + n] = v
        in_maps.append(m)
    res = run_bass_kernel_spmd(P.nc, in_maps, core_ids=list(range(B)))
    return np.stack([np.asarray(r["out"], dtype=np.float32) for r in res.results], axis=0)
```
